# Optimizing a Trainium2 kernel written in Bass

```python
import jax, jax.numpy as jnp
from jax import lax
import numpy as np

D_MODEL = 1024
BATCH = 2
SEQ = 8192
DEPTH = 2

GRID_W = 64
NA_HEADS = 16
NA_HEAD_DIM = D_MODEL // NA_HEADS
NA_WIN_H = 8
NA_WIN_W = 16
MLA_HEADS = 16
MLA_Q_LORA = D_MODEL // 4
MLA_KV_LORA = D_MODEL // 8
MLA_NOPE = 64
MLA_ROPE = 32
MLA_V = 64
ROPE_THETA = 10000.0
Q_BLOCK = 128
FFN_DIM = 2816
N_EXPERTS = 8
TOP_K = 2
EXPERT_DIM = 1792
NORM_EPS = 1e-6

kernel_name = "hybrid_na_mla_moe_adaln_encoder"


def _rmsnorm(x):
    xf = x.astype(jnp.float32)
    y = xf * lax.rsqrt(jnp.mean(xf * xf, axis=-1, keepdims=True) + NORM_EPS)
    return y.astype(x.dtype)


def _modulate(x, shift, scale):
    return _rmsnorm(x) * (1 + scale[:, None, :]) + shift[:, None, :]


def _swiglu(h, w_gu, w_down):
    gate, up = jnp.split(h @ w_gu, 2, axis=-1)
    return (jax.nn.silu(gate) * up) @ w_down


def _rope_2d_tables(seq_len):
    t = jnp.arange(seq_len)
    row = (t // GRID_W).astype(jnp.float32)
    col = (t % GRID_W).astype(jnp.float32)
    n = MLA_ROPE // 4
    inv = ROPE_THETA ** (-jnp.arange(n, dtype=jnp.float32) / n)
    ang = jnp.concatenate([row[:, None] * inv, col[:, None] * inv], axis=-1)
    return jnp.cos(ang), jnp.sin(ang)


def _apply_rope(x, cos, sin):
    x1 = x[..., 0::2].astype(jnp.float32)
    x2 = x[..., 1::2].astype(jnp.float32)
    out = jnp.stack([x1 * cos - x2 * sin, x1 * sin + x2 * cos], axis=-1)
    return out.reshape(x.shape).astype(x.dtype)


def _neighborhood_attention(h, w_qkv, rpb, w_o):
    B, S, D = h.shape
    rows = S // GRID_W
    kh = min(NA_WIN_H, rows)
    kw = min(NA_WIN_W, GRID_W)
    qkv = (h @ w_qkv).reshape(B, rows, GRID_W, 3, NA_HEADS, NA_HEAD_DIM)
    q = qkv[:, :, :, 0] * (NA_HEAD_DIM ** -0.5)
    k = qkv[:, :, :, 1]
    v = qkv[:, :, :, 2]
    cols = jnp.arange(GRID_W)
    col_idx = jnp.clip(cols - kw // 2, 0, GRID_W - kw)[:, None] + jnp.arange(kw)[None, :]
    dc_idx = col_idx - cols[:, None] + NA_WIN_W - 1

    def row_block(r):
        rs = jnp.clip(r - kh // 2, 0, rows - kh)
        q_r = lax.dynamic_index_in_dim(q, r, axis=1, keepdims=False)
        k_g = lax.dynamic_slice_in_dim(k, rs, kh, axis=1)[:, :, col_idx]
        v_g = lax.dynamic_slice_in_dim(v, rs, kh, axis=1)[:, :, col_idx]
        dr_idx = rs + jnp.arange(kh) - r + NA_WIN_H - 1
        bias = rpb[:, dr_idx[None, :, None], dc_idx[:, None, :]]
        s = jnp.einsum('bqhd,bkqjhd->bhqkj', q_r, k_g).astype(jnp.float32) + bias.astype(jnp.float32)
        p = jax.nn.softmax(s.reshape(B, NA_HEADS, GRID_W, kh * kw), axis=-1)
        p = p.reshape(s.shape).astype(v.dtype)
        return jnp.einsum('bhqkj,bkqjhd->bqhd', p, v_g)

    o = lax.map(row_block, jnp.arange(rows))
    o = jnp.moveaxis(o, 0, 1).reshape(B, S, D)
    return o @ w_o


def _mla(h, w_down, q_norm, w_uq, kv_norm, w_ukv, w_o):
    B, S, D = h.shape
    down = h @ w_down
    c_q = _rmsnorm(down[..., :MLA_Q_LORA]) * q_norm
    c_kv = _rmsnorm(down[..., MLA_Q_LORA:MLA_Q_LORA + MLA_KV_LORA]) * kv_norm
    k_rope = down[..., MLA_Q_LORA + MLA_KV_LORA:]
    q = (c_q @ w_uq).reshape(B, S, MLA_HEADS, MLA_NOPE + MLA_ROPE)
    kv = (c_kv @ w_ukv).reshape(B, S, MLA_HEADS, MLA_NOPE + MLA_V)
    k_nope, v = kv[..., :MLA_NOPE], kv[..., MLA_NOPE:]
    cos, sin = _rope_2d_tables(S)
    q_nope = q[..., :MLA_NOPE]
    q_rope = _apply_rope(q[..., MLA_NOPE:], cos[:, None, :], sin[:, None, :])
    k_rope = _apply_rope(k_rope, cos, sin)
    scale = (MLA_NOPE + MLA_ROPE) ** -0.5
    nb = S // Q_BLOCK

    def to_blocks(t):
        return jnp.moveaxis(t.reshape(B, nb, Q_BLOCK, *t.shape[2:]), 1, 0)

    def q_block(blk):
        qn, qr = blk
        s = (jnp.einsum('bqhd,bkhd->bhqk', qn, k_nope)
             + jnp.einsum('bqhr,bkr->bhqk', qr, k_rope)).astype(jnp.float32) * scale
        p = jax.nn.softmax(s, axis=-1).astype(v.dtype)
        return jnp.einsum('bhqk,bkhd->bqhd', p, v)

    o = lax.map(q_block, (to_blocks(q_nope), to_blocks(q_rope)))
    o = jnp.moveaxis(o, 0, 1).reshape(B, S, MLA_HEADS * MLA_V)
    return o @ w_o


def _moe(h, w_router, w_gu, w_down):
    B, S, D = h.shape
    hf = h.reshape(B * S, D)
    probs = jax.nn.softmax((hf @ w_router).astype(jnp.float32), axis=-1)
    vals, idx = lax.top_k(probs, TOP_K)
    wts = vals / jnp.sum(vals, axis=-1, keepdims=True)
    combine = jnp.sum(jax.nn.one_hot(idx, N_EXPERTS, dtype=jnp.float32) * wts[..., None], axis=1)
    combine = combine.astype(h.dtype)
    y = jnp.zeros_like(hf)
    for e in range(N_EXPERTS):
        y = y + combine[:, e:e + 1] * _swiglu(hf, w_gu[e], w_down[e])
    return y.reshape(B, S, D)


def setup_inputs(seed: int = 0) -> dict:
    key = jax.random.key(seed)
    ks = iter(jax.random.split(key, 32))
    n_even = (DEPTH + 1) // 2
    n_odd = DEPTH // 2
    D = D_MODEL

    def nrm(shape, fan_in, mult=1.0):
        return jax.random.normal(next(ks), shape, jnp.float32) * (mult * fan_in ** -0.5)

    return {
        "x": jax.random.normal(next(ks), (BATCH, SEQ, D), jnp.float32),
        "c": jax.random.normal(next(ks), (BATCH, D), jnp.float32),
        "w_ada": nrm((DEPTH, D, 6 * D), D, 0.5),
        "b_ada": 0.02 * jax.random.normal(next(ks), (DEPTH, 6 * D), jnp.float32),
        "na_w_qkv": nrm((n_even, D, 3 * D), D),
        "na_rpb": 0.1 * jax.random.normal(next(ks), (n_even, NA_HEADS, 2 * NA_WIN_H - 1, 2 * NA_WIN_W - 1), jnp.float32),
        "na_w_o": nrm((n_even, D, D), D),
        "ffn_w_gu": nrm((n_even, D, 2 * FFN_DIM), D),
        "ffn_w_down": nrm((n_even, FFN_DIM, D), FFN_DIM),
        "mla_w_down": nrm((n_odd, D, MLA_Q_LORA + MLA_KV_LORA + MLA_ROPE), D),
        "mla_q_norm": 1.0 + 0.01 * jax.random.normal(next(ks), (n_odd, MLA_Q_LORA), jnp.float32),
        "mla_w_uq": nrm((n_odd, MLA_Q_LORA, MLA_HEADS * (MLA_NOPE + MLA_ROPE)), MLA_Q_LORA),
        "mla_kv_norm": 1.0 + 0.01 * jax.random.normal(next(ks), (n_odd, MLA_KV_LORA), jnp.float32),
        "mla_w_ukv": nrm((n_odd, MLA_KV_LORA, MLA_HEADS * (MLA_NOPE + MLA_V)), MLA_KV_LORA),
        "mla_w_o": nrm((n_odd, MLA_HEADS * MLA_V, D), MLA_HEADS * MLA_V),
        "moe_w_router": nrm((n_odd, D, N_EXPERTS), D),
        "moe_w_gu": nrm((n_odd, N_EXPERTS, D, 2 * EXPERT_DIM), D),
        "moe_w_down": nrm((n_odd, N_EXPERTS, EXPERT_DIM, D), EXPERT_DIM),
        "final_norm": 1.0 + 0.01 * jax.random.normal(next(ks), (D,), jnp.float32),
    }


def reference(x, c, w_ada, b_ada, na_w_qkv, na_rpb, na_w_o, ffn_w_gu, ffn_w_down,
              mla_w_down, mla_q_norm, mla_w_uq, mla_kv_norm, mla_w_ukv, mla_w_o,
              moe_w_router, moe_w_gu, moe_w_down, final_norm):
    c_act = jax.nn.silu(c)
    for i in range(DEPTH):
        j = i // 2
        mod = c_act @ w_ada[i] + b_ada[i]
        sh_a, sc_a, g_a, sh_f, sc_f, g_f = jnp.split(mod, 6, axis=-1)
        h = _modulate(x, sh_a, sc_a)
        if i % 2 == 0:
            m = _neighborhood_attention(h, na_w_qkv[j], na_rpb[j], na_w_o[j])
        else:
            m = _mla(h, mla_w_down[j], mla_q_norm[j], mla_w_uq[j], mla_kv_norm[j], mla_w_ukv[j], mla_w_o[j])
        x = x + g_a[:, None, :] * m
        h = _modulate(x, sh_f, sc_f)
        if i % 2 == 0:
            f = _swiglu(h, ffn_w_gu[j], ffn_w_down[j])
        else:
            f = _moe(h, moe_w_router[j], moe_w_gu[j], moe_w_down[j])
        x = x + g_f[:, None, :] * f
    return _rmsnorm(x) * final_norm
```

```python
import contextlib
import numpy as np
import concourse.bass as bass
import concourse.mybir as mybir
from concourse.bass_utils import run_bass_kernel_spmd

F32 = mybir.dt.float32
BF16 = mybir.dt.bfloat16
ALU = mybir.AluOpType
AF = mybir.ActivationFunctionType

ENGS = ("pe", "act", "dve", "pool", "sp")
SAME_ENGINE_SYNC = True

D = 1024
T = 2048
TE = 2560
S = 8192
NC = 8
EPS = 1e-6
FFN = 2816
NEXP = 8
EDIM = 1792
NEG = -30000.0
ARENA_WORDS = 50 * 1024


class Prog:
    def __init__(self, nc):
        self.nc = nc
        self.ops = []
        self.last_writer = {}
        self.readers = {}
        self.pending_bar = {}
        self.last_on_eng = {}
        self.last_dma_on_key = {}

    def add(self, eng, fn, reads=(), writes=(), key=None, inc=16):
        i = len(self.ops)
        deps = set()
        for r in reads:
            w = self.last_writer.get(r)
            if w is not None:
                deps.add(w)
        for w_ in writes:
            w = self.last_writer.get(w_)
            if w is not None:
                deps.add(w)
            deps.update(self.readers.get(w_, ()))
        if eng in self.pending_bar:
            deps.update(self.pending_bar.pop(eng))
        deps.discard(i)
        for r in reads:
            self.readers.setdefault(r, []).append(i)
        for w_ in writes:
            self.last_writer[w_] = i
            self.readers[w_] = []
        self.ops.append(dict(eng=eng, fn=fn, deps=deps, key=key, inc=inc))
        self.last_on_eng[eng] = i
        if key is not None:
            self.last_dma_on_key[key] = i
        return i

    def barrier(self):
        b = set(self.last_on_eng.values()) | set(self.last_dma_on_key.values())
        for e in ENGS:
            self.pending_bar[e] = set(b) | self.pending_bar.get(e, set())

    def emit(self, final_wait_eng="sp"):
        nc = self.nc
        ops = self.ops
        n = len(ops)
        fin = set(self.last_on_eng.values()) | set(self.last_dma_on_key.values())
        ops.append(dict(eng=final_wait_eng, fn=None, deps=fin, key=None, inc=0))
        waited_eng = {}
        waited_key = {}
        needed = [False] * (n + 1)
        for i, op in enumerate(ops):
            e = op["eng"]
            best_eng = {}
            best_key = {}
            for d in op["deps"]:
                od = ops[d]
                if od["key"] is not None:
                    k = od["key"]
                    if d > best_key.get(k, -1):
                        best_key[k] = d
                else:
                    se = od["eng"]
                    if se == e and (se == "pe" or not SAME_ENGINE_SYNC):
                        continue
                    if d > best_eng.get(se, -1):
                        best_eng[se] = d
            fdeps = []
            for se, d in best_eng.items():
                if waited_eng.get((e, se), -1) >= d:
                    continue
                waited_eng[(e, se)] = d
                fdeps.append(d)
            for k, d in best_key.items():
                if waited_key.get((e, k), -1) >= d:
                    continue
                waited_key[(e, k)] = d
                fdeps.append(d)
            op["fdeps"] = fdeps
            for d in fdeps:
                needed[d] = True
        seq = {e: 0 for e in ENGS}
        keycnt = {}
        for i, op in enumerate(ops):
            if op["key"] is not None:
                k = op["key"]
                keycnt[k] = keycnt.get(k, 0) + op["inc"]
                op["semval"] = keycnt[k]
            elif needed[i]:
                seq[op["eng"]] += 1
                op["semval"] = seq[op["eng"]]
        self.stats = dict(n_ops=n, n_sig=dict(seq), n_keys=len(keycnt))
        running = {}
        for i, op in enumerate(ops):
            waits = []
            for d in op["fdeps"]:
                od = ops[d]
                if od["key"] is not None:
                    waits.append(("key", od["key"], running[od["key"]]))
                else:
                    waits.append(("eng", od["eng"], od["semval"]))
            op["waits"] = waits
            op["sig"] = needed[i]
            if op["key"] is not None:
                running[op["key"]] = op["semval"]
        sems = {}
        with contextlib.ExitStack() as st:
            for e in ENGS:
                sems[("eng", e)] = st.enter_context(nc.semaphore("s_" + e))
            for k in keycnt:
                sems[("key", k)] = st.enter_context(nc.semaphore("k_" + k))
            block = st.enter_context(nc.Block())
            by_eng = {e: [op for op in ops if op["eng"] == e] for e in ENGS}

            def run(eh, e):
                for op in by_eng[e]:
                    for (kind, name, val) in op["waits"]:
                        eh.wait_ge(sems[(kind, name)], val)
                    if op["fn"] is None:
                        continue
                    ins = op["fn"](eh)
                    if op["key"] is not None:
                        ins.then_inc(sems[("key", op["key"])], op["inc"])
                    elif op["sig"]:
                        ins.then_inc(sems[("eng", e)], 1)

            @block.tensor
            def _(eh):
                run(eh, "pe")

            @block.scalar
            def _(eh):
                run(eh, "act")

            @block.vector
            def _(eh):
                run(eh, "dve")

            @block.gpsimd
            def _(eh):
                run(eh, "pool")

            @block.sync
            def _(eh):
                run(eh, "sp")


class Arena:
    def __init__(self, nc, nwords):
        self.t = nc.alloc_sbuf_tensor("arena", [128, nwords], F32)
        self.n = nwords
        self.off = 0
        self.peak = 0

    def alloc(self, shape, dtype):
        nel = int(np.prod(shape))
        nb = nel * (4 if dtype == F32 else 2)
        nw = (nb + 3) // 4
        nw = (nw + 7) // 8 * 8
        assert self.off + nw <= self.n, f"arena overflow {self.off}+{nw}>{self.n}"
        ap = self.t[:, self.off:self.off + nw]
        self.off += nw
        self.peak = max(self.peak, self.off)
        if dtype != F32:
            ap = ap.bitcast(dtype)
        ap = ap[:, 0:nel]
        if len(shape) == 2:
            ap = ap.rearrange("p (a b) -> p a b", a=shape[0], b=shape[1])
        elif len(shape) == 3:
            ap = ap.rearrange("p (a b c) -> p a b c", a=shape[0], b=shape[1], c=shape[2])
        elif len(shape) == 4:
            ap = ap.rearrange("p (a b c d) -> p a b c d", a=shape[0], b=shape[1], c=shape[2], d=shape[3])
        return ap

    def mark(self):
        return self.off

    def reset(self, m):
        self.off = m


class Ctx:
    pass


def na_tiles(lr):
    if lr < 4:
        return list(range(lr // 2, 6))
    if lr <= 28:
        return list(range(lr // 2, (lr + 7) // 2 + 1))
    return list(range(14, (lr + 7) // 2 + 1))


NA_SETS = [4, 5, 0, 1, 2, 3, 29, 30, 31]


def na_boff(lr):
    offs = {}
    o = 0
    for s in NA_SETS:
        offs[s] = o
        o += len(na_tiles(s))
    if lr < 4 or lr > 28:
        return offs[lr]
    return offs[4] if lr % 2 == 0 else offs[5]


NA_NBLK = sum(len(na_tiles(s)) for s in NA_SETS)


def host_na_bias(rpb, q):
    out = np.empty((16, 128, NA_NBLK * 64), np.float32)
    kk = np.arange(128)
    c = np.arange(64)
    cs = np.clip(c - 8, 0, 48)
    o = 0
    for s in NA_SETS:
        r = 32 * q + s
        rs = min(max(r - 4, 0), 120)
        for t in na_tiles(s):
            kr = 2 * t + kk // 64
            kcol = kk % 64
            krow = 32 * q - 4 + kr
            vrow = (krow >= rs) & (krow < rs + 8)
            vcol = (kcol[:, None] >= cs[None, :]) & (kcol[:, None] < cs[None, :] + 16)
            valid = vrow[:, None] & vcol
            dr = np.clip(krow - r + 7, 0, 14)
            dc = np.clip(kcol[:, None] - c[None, :] + 15, 0, 30)
            vals = rpb[:, dr[:, None], dc]
            out[:, :, o * 64:(o + 1) * 64] = np.where(valid[None], vals, np.float32(NEG))
            o += 1
    return out


def host_rope_tables(q):
    lr = np.arange(32)
    row = (32 * q + lr).astype(np.float32)
    col = np.arange(64).astype(np.float32)
    n = 8
    inv = (np.float32(10000.0) ** (-np.arange(n, dtype=np.float32) / np.float32(n))).astype(np.float32)
    rowang = row[:, None] * inv[None, :]
    colang = col[:, None] * inv[None, :]
    ang = np.zeros((32, 64, 16), np.float32)
    ang[:, :, 0:8] = rowang[:, None, :]
    ang[:, :, 8:16] = colang[None, :, :]
    ang = ang.reshape(2048, 16)
    cos = np.cos(ang).astype(np.float32)
    sin = np.sin(ang).astype(np.float32)
    C = np.repeat(cos.T, 2, axis=0)
    Sn = np.repeat(sin.T, 2, axis=0)
    Cf = np.zeros((128, 2048), np.float32)
    Sf = np.zeros((128, 2048), np.float32)
    for base in (0, 64):
        Cf[base:base + 32] = C
        Sf[base:base + 32] = Sn
    return Cf, Sf


def bank(K, b):
    return K.ps[:, b * 512:(b + 1) * 512]


def mm(K, out, lhsT, rhs, start, stop, reads, writes):
    K.P.add("pe", lambda e: e.matmul(out, lhsT, rhs, start=start, stop=stop), reads=reads, writes=writes)


def setup_consts(K):
    P, A = K.P, K.A
    K.onesD = A.alloc([128], F32)
    K.ones256 = A.alloc([128], F32)
    K.ones128 = A.alloc([128], F32)
    K.ident = A.alloc([128], F32)
    K.onespad = A.alloc([2, 128], BF16)
    P.add("dve", lambda e: e.memset(K.onesD, 1.0 / 1024), writes=["consts"])
    P.add("dve", lambda e: e.memset(K.ones256, 1.0 / 256), writes=["consts"])
    P.add("dve", lambda e: e.memset(K.ones128, 1.0 / 128), writes=["consts"])
    P.add("dve", lambda e: e.memset(K.onespad, 0.0), writes=["consts"])
    P.add("dve", lambda e: e.memset(K.onespad[:, 0, 0:64], 1.0), writes=["consts"])
    P.add("dve", lambda e: e.memset(K.onespad[:, 1, 64:128], 1.0), writes=["consts"])
    P.add("sp", lambda e: e.dma_start(out=K.ident, in_=K.din["ident"]), writes=["ident"], key="ident")
    K.cact = A.alloc([8], F32)
    K.mod = [A.alloc([48], F32), A.alloc([48], F32)]
    K.bada = [A.alloc([48], F32), A.alloc([48], F32)]
    K.norm_ctr = 0


def adaln(K, layers):
    P, A = K.P, K.A
    m0 = A.mark()
    wa = [A.alloc([8, 768], F32), A.alloc([8, 768], F32)]
    P.add("sp", lambda e: e.dma_start(out=K.cact, in_=K.din["cT"]), writes=["cact"], key="cact")
    P.add("act", lambda e: e.activation(K.cact, K.cact, AF.Silu), reads=["cact"], writes=["cact"])
    cnt = 0
    for li in layers:
        P.add("sp", lambda e, li=li: e.dma_start(out=K.bada[li], in_=K.din["b_ada"][li]), writes=[f"bada{li}"], key=f"bada{li}")
        wv = K.din["w_ada"][li].rearrange("(kc p) n -> p kc n", p=128)
        for jb in range(8):
            buf = wa[cnt % 2]
            res = f"wa{cnt % 2}"
            cnt += 1
            P.add("sp", lambda e, buf=buf, jb=jb, wv=wv: e.dma_start(out=buf, in_=wv[:, :, jb * 768:(jb + 1) * 768]),
                  writes=[res], key=res)
            for j in range(6):
                col = jb * 6 + j
                for kc in range(8):
                    mm(K, bank(K, 0)[:, col:col + 1], buf[:, kc, j * 128:(j + 1) * 128], K.cact[:, kc:kc + 1],
                       kc == 0, kc == 7, [res, "cact"], ["ps0"])
        mod = K.mod[li]
        P.add("dve", lambda e, mod=mod, li=li: e.tensor_tensor(mod, bank(K, 0)[:, 0:48], K.bada[li], ALU.add),
              reads=["ps0", f"bada{li}"], writes=[f"mod{li}"])
        P.add("dve", lambda e, mod=mod: e.tensor_scalar_add(mod[:, 8:16], mod[:, 8:16], 1.0), reads=[f"mod{li}"], writes=[f"mod{li}"])
        P.add("dve", lambda e, mod=mod: e.tensor_scalar_add(mod[:, 32:40], mod[:, 32:40], 1.0), reads=[f"mod{li}"], writes=[f"mod{li}"])
    P.barrier()
    A.reset(m0)


def alloc_norm_scratch(K):
    A = K.A
    K.sq = [A.alloc([512], F32), A.alloc([512], F32)]
    K.rs = [A.alloc([512], F32), A.alloc([512], F32)]
    K.ntmp = [A.alloc([512], F32), A.alloc([512], F32)]


def modulate_block(K, src_fn, src_res, n, mod, modres, sc0, sh0, dst_fn, dst_res_fn, nb0):
    P = K.P
    i = K.norm_ctr
    K.norm_ctr += 1
    psb = nb0 + (i % 2)
    for c in range(8):
        sq = K.sq[c % 2]
        P.add("pool", lambda e, sq=sq, c=c: e.tensor_tensor(sq[:, :n], src_fn(c), src_fn(c), ALU.mult),
              reads=[src_res(c)], writes=[f"sq{c % 2}"])
        mm(K, bank(K, psb)[:, :n], K.onesD, sq[:, :n], c == 0, c == 7, [f"sq{c % 2}", "consts"], [f"ps{psb}"])
    rs = K.rs[i % 2]
    rr = f"rs{i % 2}"
    P.add("act", lambda e: e.activation(rs[:, :n], bank(K, psb)[:, :n], AF.Sqrt, bias=EPS, scale=1.0),
          reads=[f"ps{psb}"], writes=[rr])
    P.add("dve", lambda e: e.reciprocal(rs[:, :n], rs[:, :n]), reads=[rr], writes=[rr])
    for c in range(8):
        if mod is not None:
            tmp = K.ntmp[c % 2]
            tr = f"ntmp{c % 2}"
            P.add("dve", lambda e, c=c, tmp=tmp: e.scalar_tensor_tensor(tmp[:, :n], src_fn(c), mod[:, sc0 + c:sc0 + c + 1], rs[:, :n],
                                                                        ALU.mult, ALU.mult),
                  reads=[src_res(c), rr, modres], writes=[tr])
            P.add("act", lambda e, c=c, tmp=tmp: e.activation(dst_fn(c), tmp[:, :n], AF.Identity, bias=mod[:, sh0 + c:sh0 + c + 1], scale=1.0),
                  reads=[tr, modres], writes=[dst_res_fn(c)])
        else:
            P.add("dve", lambda e, c=c: e.scalar_tensor_tensor(dst_fn(c), src_fn(c), K.fscale[:, c:c + 1], rs[:, :n],
                                                               ALU.mult, ALU.mult),
                  reads=[src_res(c), rr, "fscale"], writes=[dst_res_fn(c)])


def load_x(K, src_dram, tok0, dres=None):
    P = K.P
    v = src_dram.rearrange("(c p) t -> p c t", p=128)
    for c in range(8):
        P.add("sp", lambda e, c=c: e.dma_start(out=K.x[:, c, :], in_=v[:, c, tok0:tok0 + T]),
              reads=([f"{dres}{c}"] if dres else []), writes=[f"x{c}_{tb}" for tb in range(4)], key=f"x{c}")


def xres(c, tb):
    return f"x{c}_{tb}"


def proj_residual(K, w_dram, inT, in_res, mod, modres, g0):
    P, A = K.P, K.A
    m0 = A.mark()
    wo = A.alloc([8, 1024], BF16)
    wv = w_dram.rearrange("(kc p) n -> p kc n", p=128)
    for hf in range(2):
        P.add("pool", lambda e, hf=hf: e.dma_start(out=wo[:, hf * 4:(hf + 1) * 4, :], in_=wv[:, hf * 4:(hf + 1) * 4, :]),
              writes=[f"wo{hf}"], key=f"wo{hf}")
    k = 0
    for tb in range(4):
        for oc in range(8):
            b = 4 + (k % 2)
            k += 1
            for kc in range(8):
                mm(K, bank(K, b), wo[:, kc, oc * 128:(oc + 1) * 128], inT[:, kc, tb * 512:(tb + 1) * 512],
                   kc == 0, kc == 7, [f"wo{kc // 4}", in_res(kc)], [f"ps{b}"])
            P.add("dve", lambda e, b=b, oc=oc, tb=tb: e.scalar_tensor_tensor(
                K.x[:, oc, tb * 512:(tb + 1) * 512], bank(K, b), mod[:, g0 + oc:g0 + oc + 1],
                K.x[:, oc, tb * 512:(tb + 1) * 512], ALU.mult, ALU.add),
                reads=[f"ps{b}", xres(oc, tb), modres], writes=[xres(oc, tb)])
    P.barrier()
    A.reset(m0)


def swiglu_stream(K, hT, hres, groups, mod, modres, g0, comb_fn=None):
    P, A = K.P, K.A
    NF = max(g["nf"] for g in groups)
    Wg = [A.alloc([8, NF * 128], BF16) for _ in range(2)]
    Wu = [A.alloc([8, NF * 128], BF16) for _ in range(2)]
    Wd = [A.alloc([NF, 1024], BF16) for _ in range(2)]
    act = [A.alloc([NF, 512], BF16) for _ in range(2)]
    t1 = [A.alloc([512], BF16) for _ in range(2)]
    t2 = [A.alloc([512], BF16) for _ in range(2)]
    steps = [(gi, tb) for gi in range(len(groups)) for tb in range(4)]
    loaded = set()
    ctr = dict(gu=0, y=0)

    def ensure_loaded(gi):
        if gi in loaded or gi >= len(groups):
            return
        loaded.add(gi)
        par = gi % 2
        groups[gi]["load"](K, Wg[par], Wu[par], Wd[par], par)

    def emit_gu(si):
        gi, tb = steps[si]
        g = groups[gi]
        par = gi % 2
        assert gi in loaded
        ab = act[si % 2]
        ar = f"act{si % 2}"
        comb = comb_fn(g, tb) if comb_fn is not None else None
        for fi in range(g["nf"]):
            k = ctr["gu"]
            ctr["gu"] += 1
            bg = 0 + (k % 2)
            bu = 2 + (k % 2)
            for kc in range(8):
                mm(K, bank(K, bg), Wg[par][:, kc, fi * 128:(fi + 1) * 128], hT[:, kc, tb * 512:(tb + 1) * 512],
                   kc == 0, kc == 7, [f"Wg{par}", hres(kc)], [f"ps{bg}"])
            for kc in range(8):
                mm(K, bank(K, bu), Wu[par][:, kc, fi * 128:(fi + 1) * 128], hT[:, kc, tb * 512:(tb + 1) * 512],
                   kc == 0, kc == 7, [f"Wu{par}", hres(kc)], [f"ps{bu}"])
            tt = t1[k % 2]
            tr = f"t1_{k % 2}"
            P.add("act", lambda e, tt=tt, bg=bg: e.activation(tt, bank(K, bg), AF.Silu), reads=[f"ps{bg}"], writes=[tr])
            src, sr = tt, tr
            if comb is not None:
                cap, cres = comb
                t2b = t2[k % 2]
                t2r = f"t2_{k % 2}"
                P.add("pool", lambda e, t2b=t2b, tt=tt, cap=cap: e.tensor_tensor(t2b, tt, cap, ALU.mult),
                      reads=[tr, cres], writes=[t2r])
                src, sr = t2b, t2r
            P.add("dve", lambda e, ab=ab, fi=fi, src=src, bu=bu: e.tensor_tensor(ab[:, fi, :], src, bank(K, bu), ALU.mult),
                  reads=[sr, f"ps{bu}"], writes=[ar])

    def emit_down(si):
        gi, tb = steps[si]
        g = groups[gi]
        par = gi % 2
        ab = act[si % 2]
        ar = f"act{si % 2}"
        nf = g["nf"]
        for oc in range(8):
            k = ctr["y"]
            ctr["y"] += 1
            b = 4 + (k % 2)
            for fi in range(nf):
                mm(K, bank(K, b), Wd[par][:, fi, oc * 128:(oc + 1) * 128], ab[:, fi, :], fi == 0, fi == nf - 1,
                   [f"Wd{par}", ar], [f"ps{b}"])
            P.add("dve", lambda e, b=b, oc=oc, tb=tb: e.scalar_tensor_tensor(
                K.x[:, oc, tb * 512:(tb + 1) * 512], bank(K, b), mod[:, g0 + oc:g0 + oc + 1],
                K.x[:, oc, tb * 512:(tb + 1) * 512], ALU.mult, ALU.add),
                reads=[f"ps{b}", xres(oc, tb), modres], writes=[xres(oc, tb)])

    ensure_loaded(0)
    ensure_loaded(1)
    emit_gu(0)
    for si in range(len(steps)):
        if si + 1 < len(steps):
            emit_gu(si + 1)
        emit_down(si)
        if steps[si][1] == 3:
            ensure_loaded(steps[si][0] + 2)


def phase_A(K):
    P, A, din = K.P, K.A, K.din
    setup_consts(K)
    adaln(K, [0, 1])
    mod0, mod1 = K.mod
    mA = A.mark()
    hT = A.alloc([8, TE], BF16)
    OT = A.alloc([8, T], BF16)
    m1 = A.mark()
    alloc_norm_scratch(K)
    xs = [A.alloc([8, 512], F32), A.alloc([8, 512], F32)]
    xv = din["xT"].rearrange("(c p) t -> p c t", p=128)

    def hres(c, lo, hi):
        return [f"h{c}_{tb}" for tb in range(lo // 512, (hi - 1) // 512 + 1)]

    for tb in range(5):
        buf = xs[tb % 2]
        P.add("sp", lambda e, buf=buf, tb=tb: e.dma_start(out=buf, in_=xv[:, :, tb * 512:(tb + 1) * 512]),
              writes=[f"xs{tb % 2}"], key=f"xs{tb % 2}")
        modulate_block(K, lambda c, buf=buf: buf[:, c, :], lambda c, tb=tb: f"xs{tb % 2}", 512, mod0, "mod0", 8, 0,
                       lambda c, tb=tb: hT[:, c, tb * 512:(tb + 1) * 512], lambda c, tb=tb: f"h{c}_{tb}", 6)
    P.barrier()
    A.reset(m1)
    wqkv = [A.alloc([3, 8, 128], BF16) for _ in range(2)]
    qT = [A.alloc([T], BF16) for _ in range(2)]
    kT = [A.alloc([TE], BF16) for _ in range(2)]
    Vp = [A.alloc([20, 2, 128], BF16) for _ in range(2)]
    biasb = [A.alloc([NA_NBLK * 64], F32) for _ in range(2)]
    stmp = [A.alloc([384], F32) for _ in range(3)]
    pT = [A.alloc([384], BF16) for _ in range(3)]
    rden = [A.alloc([512], F32) for _ in range(2)]
    for par in range(2):
        P.add("pool", lambda e, par=par: e.memset(Vp[par], 1.0), writes=[f"Vp{par}"])
    wv = din["na_w_qkv"].rearrange("(kc p) n -> p kc n", p=128)
    nm = ["wq", "wk", "wv"]
    PB = 7
    for p in range(8):
        par = p % 2
        for s in range(3):
            P.add("pool", lambda e, par=par, s=s, p=p: e.dma_start(out=wqkv[par][:, s], in_=wv[:, :, s * 1024 + p * 128:s * 1024 + (p + 1) * 128]),
                  writes=[f"{nm[s]}{par}"], key=f"{nm[s]}{par}")
        for h2 in range(2):
            h = 2 * p + h2
            P.add("sp", lambda e, h2=h2, h=h: e.dma_start(out=biasb[h2], in_=din["na_bias"][h]), writes=[f"bias{h2}"], key=f"bias{h2}")
        for tb in range(4):
            lo = 256 + tb * 512
            for kc in range(8):
                mm(K, bank(K, PB), wqkv[par][:, 0, kc, :], hT[:, kc, lo:lo + 512], kc == 0, kc == 7,
                   [f"wq{par}"] + hres(kc, lo, lo + 512), [f"ps{PB}"])
            P.add("act", lambda e, par=par, tb=tb: e.copy(qT[par][:, tb * 512:(tb + 1) * 512], bank(K, PB)),
                  reads=[f"ps{PB}"], writes=[f"qT{par}"])
        for tb in range(5):
            lo = tb * 512
            for kc in range(8):
                mm(K, bank(K, PB), wqkv[par][:, 1, kc, :], hT[:, kc, lo:lo + 512], kc == 0, kc == 7,
                   [f"wk{par}"] + hres(kc, lo, lo + 512), [f"ps{PB}"])
            P.add("act", lambda e, par=par, tb=tb: e.copy(kT[par][:, tb * 512:(tb + 1) * 512], bank(K, PB)),
                  reads=[f"ps{PB}"], writes=[f"kT{par}"])
        for g in range(5):
            for j in range(4):
                t = g * 4 + j
                for kc in range(8):
                    mm(K, bank(K, PB)[:, j * 128:(j + 1) * 128], hT[:, kc, t * 128:(t + 1) * 128], wqkv[par][:, 2, kc, :],
                       kc == 0, kc == 7, [f"wv{par}"] + hres(kc, t * 128, (t + 1) * 128), [f"ps{PB}"])
            pv = bank(K, PB).rearrange("p (t c) -> p t c", c=128)
            P.add("dve", lambda e, par=par, g=g, pv=pv: e.tensor_copy(Vp[par][:, g * 4:(g + 1) * 4, 0, 0:64], pv[:, :, 0:64]),
                  reads=[f"ps{PB}"], writes=[f"Vp{par}"])
            P.add("dve", lambda e, par=par, g=g, pv=pv: e.tensor_copy(Vp[par][:, g * 4:(g + 1) * 4, 1, 64:128], pv[:, :, 64:128]),
                  reads=[f"ps{PB}"], writes=[f"Vp{par}"])
        steps = [(lr, h2) for lr in range(32) for h2 in range(2)]

        def emit_S(si, par=par):
            lr, h2 = steps[si]
            sb = si % 3
            for j, t in enumerate(na_tiles(lr)):
                mm(K, bank(K, sb)[:, j * 64:(j + 1) * 64], kT[par][h2 * 64:(h2 + 1) * 64, t * 128:(t + 1) * 128],
                   qT[par][h2 * 64:(h2 + 1) * 64, lr * 64:(lr + 1) * 64], True, True, [f"kT{par}", f"qT{par}"], [f"ps{sb}"])

        def emit_soft(si):
            lr, h2 = steps[si]
            sb = si % 3
            nt = len(na_tiles(lr))
            bo = na_boff(lr)
            tmp = stmp[si % 3]
            pt = pT[si % 3]
            P.add("dve", lambda e: e.scalar_tensor_tensor(tmp[:, :nt * 64], bank(K, sb)[:, :nt * 64], 0.125,
                                                          biasb[h2][:, bo * 64:(bo + nt) * 64], ALU.mult, ALU.add),
                  reads=[f"ps{sb}", f"bias{h2}"], writes=[f"stmp{si % 3}"])
            P.add("act", lambda e: e.activation(pt[:, :nt * 64], tmp[:, :nt * 64], AF.Exp),
                  reads=[f"stmp{si % 3}"], writes=[f"pT{si % 3}"])

        def emit_PV(si, par=par, p=p):
            lr, h2 = steps[si]
            grp, slot = lr // 8, lr % 8
            ob = 3 + (grp % 2) + 2 * h2
            pt = pT[si % 3]
            tiles = na_tiles(lr)
            for j, t in enumerate(tiles):
                mm(K, bank(K, ob)[:, slot * 64:(slot + 1) * 64], Vp[par][:, t, h2, :], pt[:, j * 64:(j + 1) * 64],
                   j == 0, j == len(tiles) - 1, [f"Vp{par}", f"pT{si % 3}"], [f"ps{ob}"])
            if slot == 7:
                rd = rden[h2]
                rr = f"rden{h2}"
                o_lo, o_hi = (0, 64) if h2 == 0 else (64, 128)
                d_lo, d_hi = (64, 128) if h2 == 0 else (0, 64)
                P.add("dve", lambda e: e.reciprocal(rd[d_lo:d_hi, :], bank(K, ob)[d_lo:d_hi, :]), reads=[f"ps{ob}"], writes=[rr])
                P.add("dve", lambda e: e.tensor_copy(rd[o_lo:o_hi, :], rd[d_lo:d_hi, :]), reads=[rr], writes=[rr])
                P.add("dve", lambda e: e.tensor_tensor(OT[o_lo:o_hi, p, grp * 512:(grp + 1) * 512], bank(K, ob)[o_lo:o_hi, :],
                                                       rd[o_lo:o_hi, :], ALU.mult),
                      reads=[f"ps{ob}", rr], writes=[f"OT{p}"])

        emit_S(0)
        emit_S(1)
        for si in range(len(steps)):
            emit_soft(si)
            if si + 2 < len(steps):
                emit_S(si + 2)
            emit_PV(si)
    P.barrier()
    A.reset(mA)
    OT2 = A.alloc([8, TE], BF16)
    OT = A.alloc([8, T], BF16)
    K.x = A.alloc([8, T], F32)
    load_x(K, din["xT"], 256)
    proj_residual(K, din["na_w_o"], OT, lambda kc: f"OT{kc}", mod0, "mod0", 16)
    A.reset(mA)
    hT2 = A.alloc([8, T], BF16)
    skip = A.alloc([8, TE - T], BF16)
    skip2 = A.alloc([8, T], BF16)
    K.x = A.alloc([8, T], F32)
    alloc_norm_scratch(K)
    for tb in range(4):
        modulate_block(K, lambda c, tb=tb: K.x[:, c, tb * 512:(tb + 1) * 512], lambda c, tb=tb: xres(c, tb), 512, mod0, "mod0", 32, 24,
                       lambda c, tb=tb: hT2[:, c, tb * 512:(tb + 1) * 512], lambda c: f"h2_{c}", 6)
    gu = din["ffn_w_gu"].rearrange("(kc p) n -> p kc n", p=128)
    dn = din["ffn_w_down"].rearrange("(f p) n -> p f n", p=128)
    groups = []
    for gi in range(11):
        def load(K, Wg, Wu, Wd, par, gi=gi):
            K.P.add("pool", lambda e: e.dma_start(out=Wg[:, :, 0:256], in_=gu[:, :, gi * 256:(gi + 1) * 256]), writes=[f"Wg{par}"], key=f"Wg{par}")
            K.P.add("pool", lambda e: e.dma_start(out=Wu[:, :, 0:256], in_=gu[:, :, FFN + gi * 256:FFN + (gi + 1) * 256]), writes=[f"Wu{par}"], key=f"Wu{par}")
            K.P.add("pool", lambda e: e.dma_start(out=Wd[:, 0:2, :], in_=dn[:, gi * 2:(gi + 1) * 2, :]), writes=[f"Wd{par}"], key=f"Wd{par}")
        groups.append(dict(load=load, nf=2))
    swiglu_stream(K, hT2, lambda kc: f"h2_{kc}", groups, mod0, "mod0", 40)
    P.barrier()
    A.reset(mA)
    K.mA = mA
    hT3 = A.alloc([8, T], BF16)
    K.hT3 = hT3
    K.cq_region = A.alloc([2, T], BF16)
    K.OTmark = A.mark()
    K.OTreg = A.alloc([8, T], BF16)
    K.xmark = A.mark()
    K.x = A.alloc([8, T], F32)
    alloc_norm_scratch(K)
    for tb in range(4):
        modulate_block(K, lambda c, tb=tb: K.x[:, c, tb * 512:(tb + 1) * 512], lambda c, tb=tb: xres(c, tb), 512, mod1, "mod1", 8, 0,
                       lambda c, tb=tb: hT3[:, c, tb * 512:(tb + 1) * 512], lambda c: f"h3_{c}", 6)
    xo = din["x1"].rearrange("(c p) t -> p c t", p=128)
    for c in range(8):
        P.add("sp", lambda e, c=c: e.dma_start(out=xo[:, c, :], in_=K.x[:, c, :]), reads=[xres(c, tb) for tb in range(4)],
              writes=[f"x1d{c}"], key=f"x{c}")
    mla_latent(K, hT3, lambda kc: f"h3_{kc}")


def mla_latent(K, hT3, hres):
    P, A, din = K.P, K.A, K.din
    wd = A.alloc([8, 160], BF16)
    wsw = A.alloc([8, 32], BF16)
    kvn = A.alloc([1], F32)
    ropeC = A.alloc([T], F32)
    ropeS = A.alloc([T], F32)
    dkv = A.alloc([512], F32)
    sqb = A.alloc([512], F32)
    rsb = A.alloc([512], F32)
    lat = A.alloc([T], F32)
    krot = A.alloc([T], F32)
    ktmp = A.alloc([512], F32)
    wv = din["mla_w_down"].rearrange("(kc p) n -> p kc n", p=128)
    P.add("pool", lambda e: e.dma_start(out=wd, in_=wv[:, :, 256:416]), writes=["wd"], key="wd")
    P.add("sp", lambda e: e.dma_start(out=kvn, in_=din["kv_norm"]), writes=["kvn"], key="kvn")
    P.add("sp", lambda e: e.dma_start(out=ropeC, in_=din["ropeC"]), writes=["ropeC"], key="ropeC")
    P.add("sp", lambda e: e.dma_start(out=ropeS, in_=din["ropeS"]), writes=["ropeS"], key="ropeS")
    wr = wd[:, :, 128:160].rearrange("p k (i two) -> p k i two", two=2)
    ws = wsw.rearrange("p k (i two) -> p k i two", two=2)
    P.add("dve", lambda e: e.tensor_scalar_mul(ws[:, :, :, 0], wr[:, :, :, 1], -1.0), reads=["wd"], writes=["wsw"])
    P.add("dve", lambda e: e.tensor_copy(ws[:, :, :, 1], wr[:, :, :, 0]), reads=["wd"], writes=["wsw"])
    for tb in range(4):
        sl = slice(tb * 512, (tb + 1) * 512)
        for kc in range(8):
            mm(K, bank(K, 0), wd[:, kc, 0:128], hT3[:, kc, sl], kc == 0, kc == 7, ["wd", hres(kc)], ["ps0"])
        for kc in range(8):
            mm(K, bank(K, 1)[0:32, :], wd[:, kc, 128:160], hT3[:, kc, sl], kc == 0, kc == 7, ["wd", hres(kc)], ["ps1"])
        for kc in range(8):
            mm(K, bank(K, 2)[0:32, :], wsw[:, kc, :], hT3[:, kc, sl], kc == 0, kc == 7, ["wsw", hres(kc)], ["ps2"])
        P.add("act", lambda e: e.copy(dkv, bank(K, 0)), reads=["ps0"], writes=["dkv"])
        P.add("pool", lambda e: e.tensor_tensor(sqb, dkv, dkv, ALU.mult), reads=["dkv"], writes=["sqb"])
        mm(K, bank(K, 3), K.ones128, sqb, True, True, ["sqb", "consts"], ["ps3"])
        P.add("act", lambda e: e.activation(rsb, bank(K, 3), AF.Sqrt, bias=EPS, scale=1.0), reads=["ps3"], writes=["rsb"])
        P.add("dve", lambda e: e.reciprocal(rsb, rsb), reads=["rsb"], writes=["rsb"])
        P.add("dve", lambda e, sl=sl: e.scalar_tensor_tensor(lat[:, sl], dkv, kvn[:, 0:1], rsb, ALU.mult, ALU.mult),
              reads=["dkv", "rsb", "kvn"], writes=["lat"])
        P.add("dve", lambda e, sl=sl: e.tensor_tensor(krot[0:32, sl], bank(K, 1)[0:32, :], ropeC[0:32, sl], ALU.mult),
              reads=["ps1", "ropeC"], writes=["krot"])
        P.add("dve", lambda e, sl=sl: e.tensor_tensor(ktmp[0:32, :], bank(K, 2)[0:32, :], ropeS[0:32, sl], ALU.mult),
              reads=["ps2", "ropeS"], writes=["ktmp"])
        P.add("pool", lambda e, sl=sl: e.tensor_tensor(krot[0:32, sl], krot[0:32, sl], ktmp[0:32, :], ALU.add),
              reads=["krot", "ktmp"], writes=["krot"])
    P.add("sp", lambda e: e.dma_start(out=din["lat"][0:128, :], in_=lat), reads=["lat"], writes=["latd0"], key="lat")
    P.add("sp", lambda e: e.dma_start(out=din["lat"][128:160, :], in_=krot[0:32, :]), reads=["krot"], writes=["latd1"], key="krot")


def phase_B_prologue_unfused(K):
    P, A, din = K.P, K.A, K.din
    setup_consts(K)
    adaln(K, [1])
    mA = A.mark()
    K.mA = mA
    K.hT3 = A.alloc([8, T], BF16)
    K.cq_region = A.alloc([2, T], BF16)
    K.OTmark = A.mark()
    K.OTreg = A.alloc([8, T], BF16)
    K.xmark = A.mark()
    K.x = A.alloc([8, T], F32)
    load_x(K, din["x1"], 0)
    alloc_norm_scratch(K)
    for tb in range(4):
        modulate_block(K, lambda c, tb=tb: K.x[:, c, tb * 512:(tb + 1) * 512], lambda c, tb=tb: xres(c, tb), 512, K.mod[1], "mod1", 8, 0,
                       lambda c, tb=tb: K.hT3[:, c, tb * 512:(tb + 1) * 512], lambda c: f"h3_{c}", 6)
    P.barrier()


def phase_B(K):
    P, A, din = K.P, K.A, K.din
    mod1 = K.mod[1]
    mB = K.mA
    oT = K.OTreg
    cqT = K.cq_region
    hT3 = K.hT3
    m1 = K.xmark
    A.reset(m1)
    wdq = A.alloc([8, 256], BF16)
    qn = A.alloc([2], F32)
    dq = [A.alloc([512], F32), A.alloc([512], F32)]
    sqq = [A.alloc([512], F32), A.alloc([512], F32)]
    rsq = A.alloc([512], F32)
    wv = din["mla_w_down"].rearrange("(kc p) n -> p kc n", p=128)
    P.add("pool", lambda e: e.dma_start(out=wdq, in_=wv[:, :, 0:256]), writes=["wdq"], key="wdq")
    P.add("sp", lambda e: e.dma_start(out=qn, in_=din["q_norm"]), writes=["qn"], key="qn")
    for tb in range(4):
        sl = slice(tb * 512, (tb + 1) * 512)
        for j in range(2):
            for kc in range(8):
                mm(K, bank(K, j), wdq[:, kc, j * 128:(j + 1) * 128], hT3[:, kc, sl], kc == 0, kc == 7, ["wdq", f"h3_{kc}"], [f"ps{j}"])
            P.add("act", lambda e, j=j: e.copy(dq[j], bank(K, j)), reads=[f"ps{j}"], writes=[f"dq{j}"])
            P.add("pool", lambda e, j=j: e.tensor_tensor(sqq[j], dq[j], dq[j], ALU.mult), reads=[f"dq{j}"], writes=[f"sqq{j}"])
            mm(K, bank(K, 2), K.ones256, sqq[j], j == 0, j == 1, [f"sqq{j}", "consts"], ["ps2"])
        P.add("act", lambda e: e.activation(rsq, bank(K, 2), AF.Sqrt, bias=EPS, scale=1.0), reads=["ps2"], writes=["rsq"])
        P.add("dve", lambda e: e.reciprocal(rsq, rsq), reads=["rsq"], writes=["rsq"])
        for j in range(2):
            P.add("dve", lambda e, j=j, sl=sl: e.scalar_tensor_tensor(cqT[:, j, sl], dq[j], qn[:, j:j + 1], rsq, ALU.mult, ALU.mult),
                  reads=[f"dq{j}", "rsq", "qn"], writes=["cqT"])
    P.barrier()
    A.reset(m1)
    save_off = A.off
    A.off = K.mA
    ckvT = A.alloc([S], BF16)
    kh0 = A.alloc([S], BF16)
    A.off = save_off
    khT = [kh0, A.alloc([S], BF16)]
    Vh = [A.alloc([64, 128], BF16) for _ in range(2)]
    qhT = [A.alloc([T], BF16) for _ in range(2)]
    wuq = A.alloc([2, 1536], BF16)
    wuqsw = A.alloc([2, 16, 96], BF16)
    wukv = A.alloc([2048], BF16)
    ropeC = A.alloc([T], F32)
    ropeS = A.alloc([T], F32)
    pT = [A.alloc([1024], BF16) for _ in range(3)]
    rt1 = [A.alloc([512], F32) for _ in range(2)]
    rt2 = [A.alloc([512], F32) for _ in range(2)]
    rd = [A.alloc([512], F32) for _ in range(2)]
    otmp = [A.alloc([512], BF16) for _ in range(2)]
    la = din["latall"]
    bsel = A.alloc([2], F32)
    P.add("sp", lambda e: e.dma_start(out=bsel, in_=din["bsel"]), writes=["bsel"], key="bsel")
    CH = 512
    stA = [A.alloc([CH], F32) for _ in range(2)]
    stB = [A.alloc([CH], F32) for _ in range(2)]
    for par in range(2):
        P.add("pool", lambda e, par=par: e.memset(Vh[par][:, :, 64:128], 1.0), writes=[f"Vh1_{par}"])
    nch = T // CH
    it = 0
    for i in range(4):
        for j in range(nch):
            sa, sb_ = stA[it % 2], stB[it % 2]
            ra, rb = f"stA{it % 2}", f"stB{it % 2}"
            it += 1
            r0, r1 = i * 160, (4 + i) * 160
            cs = slice(j * CH, (j + 1) * CH)
            ks = slice(i * T + j * CH, i * T + (j + 1) * CH)
            P.add("sp", lambda e, sa=sa, r0=r0, cs=cs: e.dma_start(out=sa[0:128, :], in_=la[r0:r0 + 128, cs]), reads=["latall"], writes=[ra], key=ra)
            P.add("sp", lambda e, sb_=sb_, r1=r1, cs=cs: e.dma_start(out=sb_[0:128, :], in_=la[r1:r1 + 128, cs]), reads=["latall"], writes=[rb], key=rb)
            P.add("dve", lambda e, sa=sa: e.tensor_scalar_mul(sa, sa, bsel[:, 0:1]), reads=[ra, "bsel"], writes=[ra])
            P.add("dve", lambda e, sa=sa, sb_=sb_, ks=ks: e.scalar_tensor_tensor(ckvT[:, ks], sb_, bsel[:, 1:2], sa, ALU.mult, ALU.add),
                  reads=[ra, rb, "bsel"], writes=[f"ckv{i}"])
    for i in range(4):
        for j in range(nch):
            sa, sb_ = stA[it % 2], stB[it % 2]
            ra, rb = f"stA{it % 2}", f"stB{it % 2}"
            it += 1
            r0, r1 = i * 160 + 128, (4 + i) * 160 + 128
            cs = slice(j * CH, (j + 1) * CH)
            ks = slice(i * T + j * CH, i * T + (j + 1) * CH)
            P.add("sp", lambda e, sa=sa, r0=r0, cs=cs: e.dma_start(out=sa[64:96, :], in_=la[r0:r0 + 32, cs]), reads=["latall"], writes=[ra], key=ra)
            P.add("sp", lambda e, sb_=sb_, r1=r1, cs=cs: e.dma_start(out=sb_[64:96, :], in_=la[r1:r1 + 32, cs]), reads=["latall"], writes=[rb], key=rb)
            P.add("pool", lambda e, sa=sa: e.tensor_scalar_mul(sa[64:96, :], sa[64:96, :], bsel[64:96, 0:1]), reads=[ra, "bsel"], writes=[ra])
            for par in range(2):
                P.add("dve", lambda e, sa=sa, sb_=sb_, ks=ks, par=par: e.scalar_tensor_tensor(
                    khT[par][64:96, ks], sb_[64:96, :], bsel[64:96, 1:2], sa[64:96, :], ALU.mult, ALU.add),
                    reads=[ra, rb, "bsel"], writes=[f"khr{par}"])
    uq = din["mla_w_uq"].rearrange("(kc p) n -> p kc n", p=128)
    P.add("pool", lambda e: e.dma_start(out=wuq, in_=uq), writes=["wuq"], key="wuq")
    P.add("pool", lambda e: e.dma_start(out=wukv, in_=din["mla_w_ukv"]), writes=["wukv"], key="wukv")
    P.add("sp", lambda e: e.dma_start(out=ropeC, in_=din["ropeC"]), writes=["ropeC"], key="ropeC")
    P.add("sp", lambda e: e.dma_start(out=ropeS, in_=din["ropeS"]), writes=["ropeS"], key="ropeS")
    P.add("pool", lambda e: e.memset(wuqsw, 0.0), writes=["wuqsw"])
    wq4 = wuq.rearrange("p k (h f) -> p k h f", h=16)
    src = wq4[:, :, :, 64:96].rearrange("p k h (i two) -> p k h i two", two=2)
    dst = wuqsw[:, :, :, 64:96].rearrange("p k h (i two) -> p k h i two", two=2)
    for kc in range(2):
        P.add("dve", lambda e, kc=kc: e.tensor_scalar_mul(dst[:, kc, :, :, 0], src[:, kc, :, :, 1], -1.0), reads=["wuq", "wuqsw"], writes=["wuqsw"])
        P.add("dve", lambda e, kc=kc: e.tensor_copy(dst[:, kc, :, :, 1], src[:, kc, :, :, 0]), reads=["wuq", "wuqsw"], writes=["wuqsw"])
    SCALE = float(96 ** -0.5)
    PBK = [6, 7]
    pctr = [0]

    def pbank():
        b = PBK[pctr[0] % 2]
        pctr[0] += 1
        return b

    def emit_proj(h):
        par = h % 2
        for kb in range(16):
            b = pbank()
            mm(K, bank(K, b)[0:64, :], wukv[:, h * 128:h * 128 + 64], ckvT[:, kb * 512:(kb + 1) * 512], True, True,
               ["wukv", f"ckv{kb // 4}"], [f"ps{b}"])
            P.add("dve", lambda e, b=b, kb=kb, par=par: e.tensor_copy(khT[par][0:64, kb * 512:(kb + 1) * 512], bank(K, b)[0:64, :]),
                  reads=[f"ps{b}"], writes=[f"kh{par}_{kb}"])
        for g in range(8):
            b = pbank()
            for j in range(8):
                kt = g * 8 + j
                mm(K, bank(K, b)[:, j * 64:(j + 1) * 64], ckvT[:, kt * 128:(kt + 1) * 128], wukv[:, h * 128 + 64:h * 128 + 128], True, True,
                   ["wukv", f"ckv{kt // 16}"], [f"ps{b}"])
            pv = bank(K, b).rearrange("p (t c) -> p t c", c=64)
            P.add("dve", lambda e, g=g, par=par, pv=pv: e.tensor_copy(Vh[par][:, g * 8:(g + 1) * 8, 0:64], pv),
                  reads=[f"ps{b}"], writes=[f"Vh{par}_{g}"])
        for tb in range(4):
            sl = slice(tb * 512, (tb + 1) * 512)
            b1 = pbank()
            b2 = pbank()
            for kc in range(2):
                mm(K, bank(K, b1)[0:96, :], wuq[:, kc, h * 96:(h + 1) * 96], cqT[:, kc, sl], kc == 0, kc == 1, ["wuq", "cqT"], [f"ps{b1}"])
            for kc in range(2):
                mm(K, bank(K, b2)[0:96, :], wuqsw[:, kc, h, :], cqT[:, kc, sl], kc == 0, kc == 1, ["wuqsw", "cqT"], [f"ps{b2}"])
            i = tb % 2
            P.add("dve", lambda e, b1=b1, sl=sl, par=par: e.tensor_copy(qhT[par][0:64, sl], bank(K, b1)[0:64, :]),
                  reads=[f"ps{b1}"], writes=[f"qh{par}"])
            P.add("dve", lambda e, b1=b1, sl=sl, i=i: e.tensor_tensor(rt1[i][64:96, :], bank(K, b1)[64:96, :], ropeC[64:96, sl], ALU.mult),
                  reads=[f"ps{b1}", "ropeC"], writes=[f"rt1_{i}"])
            P.add("dve", lambda e, b2=b2, sl=sl, i=i: e.tensor_tensor(rt2[i][64:96, :], bank(K, b2)[64:96, :], ropeS[64:96, sl], ALU.mult),
                  reads=[f"ps{b2}", "ropeS"], writes=[f"rt2_{i}"])
            P.add("pool", lambda e, sl=sl, i=i, par=par: e.tensor_tensor(qhT[par][64:96, sl], rt1[i][64:96, :], rt2[i][64:96, :], ALU.add),
                  reads=[f"rt1_{i}", f"rt2_{i}"], writes=[f"qh{par}"])

    sctr = [0]

    def emit_S(h, qb, k2):
        par = h % 2
        si = sctr[0]
        sctr[0] += 1
        b0 = (si % 2) * 2
        for j in range(2):
            kt = k2 * 2 + j
            mm(K, bank(K, b0 + j), khT[par][0:96, kt * 128:(kt + 1) * 128], qhT[par][0:96, qb * 512:(qb + 1) * 512], True, True,
               [f"kh{par}_{kt // 4}", f"khr{par}", f"qh{par}"], [f"ps{b0 + j}"])
        return si

    def emit_exp_pv(h, qb, k2, si):
        par = h % 2
        b0 = (si % 2) * 2
        pt = pT[si % 3]
        ob = 4 + ((h * 4 + qb) % 2)
        P.add("act", lambda e: e.activation(pt, K.ps[:, b0 * 512:(b0 + 2) * 512], AF.Exp, scale=SCALE),
              reads=[f"ps{b0}", f"ps{b0 + 1}"], writes=[f"pT{si % 3}"])
        for j in range(2):
            kt = k2 * 2 + j
            mm(K, bank(K, ob), Vh[par][:, kt, :], pt[:, j * 512:(j + 1) * 512], kt == 0, kt == 63,
               [f"Vh{par}_{kt // 8}", f"Vh1_{par}", f"pT{si % 3}"], [f"ps{ob}"])
        if k2 == 31:
            i = (h * 4 + qb) % 2
            c = h // 2
            sl = slice(qb * 512, (qb + 1) * 512)
            P.add("dve", lambda e: e.reciprocal(rd[i][64:128, :], bank(K, ob)[64:128, :]), reads=[f"ps{ob}"], writes=[f"rd{i}"])
            P.add("dve", lambda e: e.tensor_copy(rd[i][0:64, :], rd[i][64:128, :]), reads=[f"rd{i}"], writes=[f"rd{i}"])
            if par == 0:
                P.add("dve", lambda e: e.tensor_tensor(oT[0:64, c, sl], bank(K, ob)[0:64, :], rd[i][0:64, :], ALU.mult),
                      reads=[f"ps{ob}", f"rd{i}"], writes=[f"oT{c}"])
            else:
                P.add("dve", lambda e: e.tensor_tensor(otmp[i][0:64, :], bank(K, ob)[0:64, :], rd[i][0:64, :], ALU.mult),
                      reads=[f"ps{ob}", f"rd{i}"], writes=[f"otmp{i}"])
                P.add("dve", lambda e: e.tensor_copy(oT[64:128, c, sl], otmp[i][0:64, :]), reads=[f"otmp{i}"], writes=[f"oT{c}"])

    emit_proj(0)
    seq = [(h, qb, k2) for h in range(16) for qb in range(4) for k2 in range(32)]
    pend = emit_S(*seq[0])
    for idx, (h, qb, k2) in enumerate(seq):
        nxt = None
        if idx + 1 < len(seq):
            if seq[idx + 1][1:] == (0, 0) and False:
                pass
            nxt = emit_S(*seq[idx + 1])
        emit_exp_pv(h, qb, k2, pend)
        pend = nxt
        if qb == 1 and k2 == 31 and h + 1 < 16:
            emit_proj(h + 1)
    P.barrier()
    A.reset(m1)
    K.x = A.alloc([8, T], F32)
    load_x(K, din["x1"], 0, dres="x1d")
    proj_residual(K, din["mla_w_o"], oT, lambda kc: f"oT{kc}", mod1, "mod1", 16)
    A.reset(mB)
    hT4 = A.alloc([8, T], BF16)
    skip = A.alloc([2, T], BF16)
    skip2 = A.alloc([8, T], BF16)
    assert A.mark() == K.xmark
    K.x = A.alloc([8, T], F32)
    mX = A.mark()
    alloc_norm_scratch(K)
    for tb in range(4):
        modulate_block(K, lambda c, tb=tb: K.x[:, c, tb * 512:(tb + 1) * 512], lambda c, tb=tb: xres(c, tb), 512, mod1, "mod1", 32, 24,
                       lambda c, tb=tb: hT4[:, c, tb * 512:(tb + 1) * 512], lambda c: f"h4_{c}", 6)
    save_off = A.off
    A.off = K.OTmark
    wr = A.alloc([8, 8], BF16)
    lg = A.alloc([16, 8], F32)
    eq = A.alloc([16, 8], F32)
    l2 = A.alloc([16, 8], F32)
    ex = A.alloc([16, 8], F32)
    comb = A.alloc([16, 8], F32)
    mx1 = A.alloc([16], F32)
    mx2 = A.alloc([16], F32)
    ssum = A.alloc([16], F32)
    onesF = A.alloc([128], F32)
    cm = [A.alloc([128], F32) for _ in range(2)]
    cbc = [A.alloc([T], F32) for _ in range(2)]
    assert A.off <= K.xmark
    A.off = save_off
    P.add("dve", lambda e: e.memset(onesF, 1.0), writes=["onesF"])
    P.add("pool", lambda e: e.dma_start(out=wr, in_=din["moe_w_router"].rearrange("(kc p) n -> p kc n", p=128)), writes=["wr"], key="wr")
    for tt in range(16):
        for kc in range(8):
            mm(K, bank(K, 6)[:, tt * 8:(tt + 1) * 8], hT4[:, kc, tt * 128:(tt + 1) * 128], wr[:, kc, :], kc == 0, kc == 7,
               ["wr", f"h4_{kc}"], ["ps6"])
    lgf = lg.rearrange("p a b -> p (a b)")
    P.add("dve", lambda e: e.tensor_copy(lgf, bank(K, 6)[:, 0:128]), reads=["ps6"], writes=["lg"])
    X = mybir.AxisListType.X

    def bc(v):
        return v.unsqueeze(2).to_broadcast([128, 16, 8])

    P.add("dve", lambda e: e.tensor_reduce(mx1, lg, X, ALU.max), reads=["lg"], writes=["mx1"])
    P.add("dve", lambda e: e.tensor_tensor(eq, lg, bc(mx1), ALU.is_equal), reads=["lg", "mx1"], writes=["eq"])
    P.add("dve", lambda e: e.scalar_tensor_tensor(l2, eq, -1e30, lg, ALU.mult, ALU.add), reads=["eq", "lg"], writes=["l2"])
    P.add("dve", lambda e: e.tensor_reduce(mx2, l2, X, ALU.max), reads=["l2"], writes=["mx2"])
    P.add("dve", lambda e: e.tensor_tensor(eq, lg, bc(mx2), ALU.is_ge), reads=["lg", "mx2", "eq"], writes=["eq"])
    P.add("dve", lambda e: e.tensor_tensor(l2, lg, bc(mx1), ALU.subtract), reads=["lg", "mx1", "l2"], writes=["l2"])
    P.add("act", lambda e: e.activation(ex, l2, AF.Exp), reads=["l2"], writes=["ex"])
    P.add("dve", lambda e: e.tensor_tensor(ex, ex, eq, ALU.mult), reads=["ex", "eq"], writes=["ex"])
    P.add("dve", lambda e: e.tensor_reduce(ssum, ex, X, ALU.add), reads=["ex"], writes=["ssum"])
    P.add("dve", lambda e: e.reciprocal(ssum, ssum), reads=["ssum"], writes=["ssum"])
    P.add("dve", lambda e: e.tensor_tensor(comb, ex, bc(ssum), ALU.mult), reads=["ex", "ssum"], writes=["comb"])

    def make_cbc(ex_):
        buf = cbc[ex_ % 2]
        res = f"cbc{ex_ % 2}"
        for tt in range(16):
            cmb = cm[tt % 2]
            P.add("dve", lambda e, cmb=cmb, tt=tt: e.tensor_scalar_mul(cmb, onesF, comb[:, tt, ex_:ex_ + 1]),
                  reads=["comb", "onesF"], writes=[f"cm{tt % 2}"])
            mm(K, bank(K, 7)[:, (tt % 4) * 128:(tt % 4 + 1) * 128], cmb, K.ident, True, True, [f"cm{tt % 2}", "ident"], ["ps7"])
            if tt % 4 == 3:
                t0 = tt - 3
                P.add("act", lambda e, t0=t0: e.copy(buf[:, t0 * 128:(t0 + 4) * 128], bank(K, 7)), reads=["ps7"], writes=[res])

    gu = din["moe_w_gu"]
    dn = din["moe_w_down"]
    groups = []
    for ex_ in range(NEXP):
        guv = gu[ex_].rearrange("(kc p) n -> p kc n", p=128)
        dnv = dn[ex_].rearrange("(f p) n -> p f n", p=128)
        for gj in range(7):
            def load(K, Wg, Wu, Wd, par, guv=guv, dnv=dnv, gj=gj, ex_=ex_):
                if gj == 0:
                    make_cbc(ex_)
                K.P.add("pool", lambda e: e.dma_start(out=Wg[:, :, 0:256], in_=guv[:, :, gj * 256:(gj + 1) * 256]), writes=[f"Wg{par}"], key=f"Wg{par}")
                K.P.add("pool", lambda e: e.dma_start(out=Wu[:, :, 0:256], in_=guv[:, :, EDIM + gj * 256:EDIM + (gj + 1) * 256]), writes=[f"Wu{par}"], key=f"Wu{par}")
                K.P.add("pool", lambda e: e.dma_start(out=Wd[:, 0:2, :], in_=dnv[:, gj * 2:(gj + 1) * 2, :]), writes=[f"Wd{par}"], key=f"Wd{par}")
            groups.append(dict(load=load, nf=2, ex=ex_))

    def comb_fn(g, tb):
        e_ = g["ex"]
        return cbc[e_ % 2][:, tb * 512:(tb + 1) * 512], f"cbc{e_ % 2}"

    swiglu_stream(K, hT4, lambda kc: f"h4_{kc}", groups, mod1, "mod1", 40, comb_fn=comb_fn)
    P.barrier()
    A.reset(mX)
    alloc_norm_scratch(K)
    K.fscale = A.alloc([8], F32)
    P.add("sp", lambda e: e.dma_start(out=K.fscale, in_=din["final_norm"]), writes=["fscale"], key="fscale")
    ost = [A.alloc([8, 512], F32) for _ in range(2)]
    ov = din["out"].rearrange("(c p) t -> p c t", p=128)
    for tb in range(4):
        ob_ = ost[tb % 2]
        modulate_block(K, lambda c, tb=tb: K.x[:, c, tb * 512:(tb + 1) * 512], lambda c, tb=tb: xres(c, tb), 512, None, None, 0, 0,
                       lambda c, ob_=ob_: ob_[:, c, :], lambda c, tb=tb: f"ost{tb % 2}", 6)
        P.add("sp", lambda e, ob_=ob_, tb=tb: e.dma_start(out=ov[:, :, tb * 512:(tb + 1) * 512], in_=ob_), reads=[f"ost{tb % 2}"], key=f"ost{tb % 2}")


FUSED = False


def build_A():
    nc = bass.Bass("TRN2", target_bir_lowering=False)
    K = Ctx()
    K.nc = nc
    K.P = Prog(nc)
    K.A = Arena(nc, ARENA_WORDS)
    K.ps = nc.alloc_psum_tensor("ps", [128, 4096], F32)

    def inp(name, shape):
        return nc.dram_tensor(name, list(shape), F32, kind="ExternalInput").ap()

    def outp(name, shape):
        return nc.dram_tensor(name, list(shape), F32, kind="ExternalOutput").ap()

    K.din = dict(
        xT=inp("xT", (D, TE)), cT=inp("cT", (128, 8)), w_ada=inp("w_ada", (2, D, 6 * D)), b_ada=inp("b_ada", (2, 128, 48)),
        ident=inp("ident", (128, 128)), na_w_qkv=inp("na_w_qkv", (D, 3 * D)), na_bias=inp("na_bias", (16, 128, NA_NBLK * 64)),
        na_w_o=inp("na_w_o", (D, D)), ffn_w_gu=inp("ffn_w_gu", (D, 2 * FFN)), ffn_w_down=inp("ffn_w_down", (FFN, D)),
        mla_w_down=inp("mla_w_down", (D, 416)), kv_norm=inp("kv_norm", (128, 1)),
        ropeC=inp("ropeC", (128, T)), ropeS=inp("ropeS", (128, T)),
        x1=outp("x1", (D, T)), lat=outp("lat", (160, T)),
    )
    phase_A(K)
    K.P.emit()
    print("A stats", K.P.stats, "arena peak words", K.A.peak)
    return nc


def build_B():
    nc = bass.Bass("TRN2", target_bir_lowering=False)
    K = Ctx()
    K.nc = nc
    K.P = Prog(nc)
    K.A = Arena(nc, ARENA_WORDS)
    K.ps = nc.alloc_psum_tensor("ps", [128, 4096], F32)

    def inp(name, shape):
        return nc.dram_tensor(name, list(shape), F32, kind="ExternalInput").ap()

    def outp(name, shape):
        return nc.dram_tensor(name, list(shape), F32, kind="ExternalOutput").ap()

    K.din = dict(
        x1=inp("x1", (D, T)), latall=inp("latall", (NC * 160, T)), cT=inp("cT", (128, 8)), w_ada=inp("w_ada", (2, D, 6 * D)),
        b_ada=inp("b_ada", (2, 128, 48)), ident=inp("ident", (128, 128)), mla_w_down=inp("mla_w_down", (D, 416)),
        ropeC=inp("ropeC", (128, T)), ropeS=inp("ropeS", (128, T)), bsel=inp("bsel", (128, 2)),
        q_norm=inp("q_norm", (128, 2)), mla_w_uq=inp("mla_w_uq", (256, 1536)), mla_w_ukv=inp("mla_w_ukv", (128, 2048)),
        mla_w_o=inp("mla_w_o", (D, D)), moe_w_router=inp("moe_w_router", (D, 8)), moe_w_gu=inp("moe_w_gu", (NEXP, D, 2 * EDIM)),
        moe_w_down=inp("moe_w_down", (NEXP, EDIM, D)), final_norm=inp("final_norm", (128, 8)),
        out=outp("out", (D, T)),
    )
    phase_B_prologue_unfused(K)
    phase_B(K)
    K.P.emit()
    print("B stats", K.P.stats, "arena peak words", K.A.peak)
    return nc


def build_AB():
    nc = bass.Bass("TRN2", target_bir_lowering=False)
    K = Ctx()
    K.nc = nc
    K.P = Prog(nc)
    K.A = Arena(nc, ARENA_WORDS)
    K.ps = nc.alloc_psum_tensor("ps", [128, 4096], F32)

    def inp(name, shape):
        return nc.dram_tensor(name, list(shape), F32, kind="ExternalInput").ap()

    def outp(name, shape):
        return nc.dram_tensor(name, list(shape), F32, kind="ExternalOutput").ap()

    x1d = nc.dram_tensor("x1d", [D, T], F32)
    latd = nc.dram_tensor("latd", [160, T], F32)
    latall = nc.dram_tensor("latall", [NC * 160, T], F32)
    K.din = dict(
        xT=inp("xT", (D, TE)), cT=inp("cT", (128, 8)), w_ada=inp("w_ada", (2, D, 6 * D)), b_ada=inp("b_ada", (2, 128, 48)),
        ident=inp("ident", (128, 128)), na_w_qkv=inp("na_w_qkv", (D, 3 * D)), na_bias=inp("na_bias", (16, 128, NA_NBLK * 64)),
        na_w_o=inp("na_w_o", (D, D)), ffn_w_gu=inp("ffn_w_gu", (D, 2 * FFN)), ffn_w_down=inp("ffn_w_down", (FFN, D)),
        mla_w_down=inp("mla_w_down", (D, 416)), kv_norm=inp("kv_norm", (128, 1)),
        ropeC=inp("ropeC", (128, T)), ropeS=inp("ropeS", (128, T)), bsel=inp("bsel", (128, 2)),
        q_norm=inp("q_norm", (128, 2)), mla_w_uq=inp("mla_w_uq", (256, 1536)), mla_w_ukv=inp("mla_w_ukv", (128, 2048)),
        mla_w_o=inp("mla_w_o", (D, D)), moe_w_router=inp("moe_w_router", (D, 8)), moe_w_gu=inp("moe_w_gu", (NEXP, D, 2 * EDIM)),
        moe_w_down=inp("moe_w_down", (NEXP, EDIM, D)), final_norm=inp("final_norm", (128, 8)),
        x1=x1d.ap(), lat=latd.ap(), latall=latall.ap(),
        out=outp("out", (D, T)),
    )
    phase_A(K)
    K.P.barrier()
    K.P.add("pool", lambda e: e.collective_compute("AllGather", ALU.bypass, replica_groups=[list(range(NC))],
                                                   ins=[latd.ap().opt()], outs=[latall.ap().opt()]),
            reads=["latd0", "latd1"], writes=["latall"], key="cc", inc=1)
    phase_B(K)
    K.P.emit()
    print("AB stats", K.P.stats, "arena peak words", K.A.peak)
    return nc


def host_common(inputs):
    x = np.asarray(inputs["x"], np.float32)
    c = np.asarray(inputs["c"], np.float32)
    per_core = []
    for core in range(NC):
        b, q = core // 4, core % 4
        r0 = 32 * q - 4
        xe = np.zeros((TE, D), np.float32)
        lo, hi = max(r0, 0), min(r0 + 40, 128)
        xe[(lo - r0) * 64:(hi - r0) * 64] = x[b, lo * 64:hi * 64]
        Cf, Sf = host_rope_tables(q)
        per_core.append(dict(
            xT=np.ascontiguousarray(xe.T),
            cT=np.ascontiguousarray(c[b].reshape(8, 128).T),
            ropeC=Cf, ropeS=Sf,
        ))
    shared = dict(
        w_ada=np.asarray(inputs["w_ada"], np.float32),
        b_ada=np.ascontiguousarray(np.asarray(inputs["b_ada"], np.float32).reshape(2, 48, 128).transpose(0, 2, 1)),
        ident=np.eye(128, dtype=np.float32),
    )
    return per_core, shared


_CACHE = {}


def kernel(**inputs):
    per_core, shared = host_common(inputs)
    rpb = np.asarray(inputs["na_rpb"], np.float32)[0]
    bias_q = [host_na_bias(rpb, q) for q in range(4)]
    if FUSED and "AB" not in _CACHE:
        _CACHE["AB"] = build_AB()
    if not FUSED and "A" not in _CACHE:
        _CACHE["A"] = build_A()
        _CACHE["B"] = build_B()
    f32 = lambda k: np.asarray(inputs[k], np.float32)[0]
    common = dict(
        na_w_qkv=f32("na_w_qkv"), na_w_o=f32("na_w_o"), ffn_w_gu=f32("ffn_w_gu"), ffn_w_down=f32("ffn_w_down"),
        mla_w_down=f32("mla_w_down"), kv_norm=f32("mla_kv_norm").reshape(128, 1),
        q_norm=np.ascontiguousarray(f32("mla_q_norm").reshape(2, 128).T),
        mla_w_uq=f32("mla_w_uq"), mla_w_ukv=f32("mla_w_ukv"), mla_w_o=f32("mla_w_o"), moe_w_router=f32("moe_w_router"),
        moe_w_gu=f32("moe_w_gu"), moe_w_down=f32("moe_w_down"),
        final_norm=np.ascontiguousarray(np.asarray(inputs["final_norm"], np.float32).reshape(8, 128).T),
    )
    in_maps = []
    for core in range(NC):
        b = core // 4
        m = dict(per_core[core])
        m.update(shared)
        m.update(common)
        m["na_bias"] = bias_q[core % 4]
        bs = np.zeros((128, 2), np.float32)
        bs[:, b] = 1.0
        m["bsel"] = bs
        in_maps.append(m)
    if FUSED:
        res = run_bass_kernel_spmd(_CACHE["AB"], in_maps, core_ids=list(range(NC))).results
    else:
        keysA = ("xT", "cT", "w_ada", "b_ada", "ident", "na_w_qkv", "na_bias", "na_w_o", "ffn_w_gu", "ffn_w_down",
                 "mla_w_down", "kv_norm", "ropeC", "ropeS")
        resA = run_bass_kernel_spmd(_CACHE["A"], [{k: m[k] for k in keysA} for m in in_maps], core_ids=list(range(NC))).results
        if inputs.get("_debugA") is not None:
            return resA
        latall = np.ascontiguousarray(np.concatenate([resA[c]["lat"] for c in range(NC)], axis=0))
        keysB = ("cT", "w_ada", "b_ada", "ident", "mla_w_down", "ropeC", "ropeS", "bsel", "q_norm", "mla_w_uq", "mla_w_ukv",
                 "mla_w_o", "moe_w_router", "moe_w_gu", "moe_w_down", "final_norm")
        mapsB = []
        for c in range(NC):
            mb = {k: in_maps[c][k] for k in keysB}
            mb["x1"] = resA[c]["x1"]
            mb["latall"] = latall
            mapsB.append(mb)
        res = run_bass_kernel_spmd(_CACHE["B"], mapsB, core_ids=list(range(NC))).results
    out = np.empty((2, S, D), np.float32)
    for core in range(NC):
        b, q = core // 4, core % 4
        out[b, q * T:(q + 1) * T, :] = res[core]["out"].T
    return out
```

```python
import contextlib
import numpy as np
import concourse.bass as bass
import concourse.mybir as mybir
from concourse.bass_utils import run_bass_kernel_spmd

F32 = mybir.dt.float32
BF16 = mybir.dt.bfloat16
ALU = mybir.AluOpType
AF = mybir.ActivationFunctionType

ENGS = ("pe", "act", "dve", "pool", "sp")
SAME_ENGINE_SYNC = True

D = 1024
T = 2048
TE = 2560
S = 8192
NC = 8
EPS = 1e-6
FFN = 2816
NEXP = 8
EDIM = 1792
NEG = -30000.0
ARENA_WORDS = 50 * 1024


class Prog:
    def __init__(self, nc):
        self.nc = nc
        self.ops = []
        self.last_writer = {}
        self.readers = {}
        self.pending_bar = {}
        self.last_on_eng = {}
        self.last_dma_on_key = {}

    def add(self, eng, fn, reads=(), writes=(), key=None, inc=16):
        i = len(self.ops)
        deps = set()
        for r in reads:
            w = self.last_writer.get(r)
            if w is not None:
                deps.add(w)
        for w_ in writes:
            w = self.last_writer.get(w_)
            if w is not None:
                deps.add(w)
            deps.update(self.readers.get(w_, ()))
        if eng in self.pending_bar:
            deps.update(self.pending_bar.pop(eng))
        deps.discard(i)
        for r in reads:
            self.readers.setdefault(r, []).append(i)
        for w_ in writes:
            self.last_writer[w_] = i
            self.readers[w_] = []
        self.ops.append(dict(eng=eng, fn=fn, deps=deps, key=key, inc=inc))
        self.last_on_eng[eng] = i
        if key is not None:
            self.last_dma_on_key[key] = i
        return i

    def barrier(self):
        b = set(self.last_on_eng.values()) | set(self.last_dma_on_key.values())
        for e in ENGS:
            self.pending_bar[e] = set(b) | self.pending_bar.get(e, set())

    def emit(self, final_wait_eng="sp"):
        nc = self.nc
        ops = self.ops
        n = len(ops)
        fin = set(self.last_on_eng.values()) | set(self.last_dma_on_key.values())
        ops.append(dict(eng=final_wait_eng, fn=None, deps=fin, key=None, inc=0))
        waited_eng = {}
        waited_key = {}
        needed = [False] * (n + 1)
        for i, op in enumerate(ops):
            e = op["eng"]
            best_eng = {}
            best_key = {}
            for d in op["deps"]:
                od = ops[d]
                if od["key"] is not None:
                    k = od["key"]
                    if d > best_key.get(k, -1):
                        best_key[k] = d
                else:
                    se = od["eng"]
                    if se == e and (se == "pe" or not SAME_ENGINE_SYNC):
                        continue
                    if d > best_eng.get(se, -1):
                        best_eng[se] = d
            fdeps = []
            for se, d in best_eng.items():
                if waited_eng.get((e, se), -1) >= d:
                    continue
                waited_eng[(e, se)] = d
                fdeps.append(d)
            for k, d in best_key.items():
                if waited_key.get((e, k), -1) >= d:
                    continue
                waited_key[(e, k)] = d
                fdeps.append(d)
            op["fdeps"] = fdeps
            for d in fdeps:
                needed[d] = True
        seq = {e: 0 for e in ENGS}
        keycnt = {}
        for i, op in enumerate(ops):
            if op["key"] is not None:
                k = op["key"]
                keycnt[k] = keycnt.get(k, 0) + op["inc"]
                op["semval"] = keycnt[k]
            elif needed[i]:
                seq[op["eng"]] += 1
                op["semval"] = seq[op["eng"]]
        self.stats = dict(n_ops=n, n_sig=dict(seq), n_keys=len(keycnt))
        running = {}
        for i, op in enumerate(ops):
            waits = []
            for d in op["fdeps"]:
                od = ops[d]
                if od["key"] is not None:
                    waits.append(("key", od["key"], running[od["key"]]))
                else:
                    waits.append(("eng", od["eng"], od["semval"]))
            op["waits"] = waits
            op["sig"] = needed[i]
            if op["key"] is not None:
                running[op["key"]] = op["semval"]
        sems = {}
        with contextlib.ExitStack() as st:
            for e in ENGS:
                sems[("eng", e)] = st.enter_context(nc.semaphore("s_" + e))
            for k in keycnt:
                sems[("key", k)] = st.enter_context(nc.semaphore("k_" + k))
            block = st.enter_context(nc.Block())
            by_eng = {e: [op for op in ops if op["eng"] == e] for e in ENGS}

            def run(eh, e):
                for op in by_eng[e]:
                    for (kind, name, val) in op["waits"]:
                        eh.wait_ge(sems[(kind, name)], val)
                    if op["fn"] is None:
                        continue
                    ins = op["fn"](eh)
                    if op["key"] is not None:
                        ins.then_inc(sems[("key", op["key"])], op["inc"])
                    elif op["sig"]:
                        ins.then_inc(sems[("eng", e)], 1)

            @block.tensor
            def _(eh):
                run(eh, "pe")

            @block.scalar
            def _(eh):
                run(eh, "act")

            @block.vector
            def _(eh):
                run(eh, "dve")

            @block.gpsimd
            def _(eh):
                run(eh, "pool")

            @block.sync
            def _(eh):
                run(eh, "sp")


class Arena:
    def __init__(self, nc, nwords):
        self.t = nc.alloc_sbuf_tensor("arena", [128, nwords], F32)
        self.n = nwords
        self.off = 0
        self.peak = 0

    def alloc(self, shape, dtype):
        nel = int(np.prod(shape))
        nb = nel * (4 if dtype == F32 else 2)
        nw = (nb + 3) // 4
        nw = (nw + 7) // 8 * 8
        assert self.off + nw <= self.n, f"arena overflow {self.off}+{nw}>{self.n}"
        ap = self.t[:, self.off:self.off + nw]
        self.off += nw
        self.peak = max(self.peak, self.off)
        if dtype != F32:
            ap = ap.bitcast(dtype)
        ap = ap[:, 0:nel]
        if len(shape) == 2:
            ap = ap.rearrange("p (a b) -> p a b", a=shape[0], b=shape[1])
        elif len(shape) == 3:
            ap = ap.rearrange("p (a b c) -> p a b c", a=shape[0], b=shape[1], c=shape[2])
        elif len(shape) == 4:
            ap = ap.rearrange("p (a b c d) -> p a b c d", a=shape[0], b=shape[1], c=shape[2], d=shape[3])
        return ap

    def mark(self):
        return self.off

    def reset(self, m):
        self.off = m


class Ctx:
    pass


def na_tiles(lr):
    if lr < 4:
        return list(range(lr // 2, 6))
    if lr <= 28:
        return list(range(lr // 2, (lr + 7) // 2 + 1))
    return list(range(14, (lr + 7) // 2 + 1))


NA_SETS = [4, 5, 0, 1, 2, 3, 29, 30, 31]


def na_boff(lr):
    offs = {}
    o = 0
    for s in NA_SETS:
        offs[s] = o
        o += len(na_tiles(s))
    if lr < 4 or lr > 28:
        return offs[lr]
    return offs[4] if lr % 2 == 0 else offs[5]


NA_NBLK = sum(len(na_tiles(s)) for s in NA_SETS)


def host_na_bias(rpb, q):
    out = np.empty((16, 128, NA_NBLK * 64), np.float32)
    kk = np.arange(128)
    c = np.arange(64)
    cs = np.clip(c - 8, 0, 48)
    o = 0
    for s in NA_SETS:
        r = 32 * q + s
        rs = min(max(r - 4, 0), 120)
        for t in na_tiles(s):
            kr = 2 * t + kk // 64
            kcol = kk % 64
            krow = 32 * q - 4 + kr
            vrow = (krow >= rs) & (krow < rs + 8)
            vcol = (kcol[:, None] >= cs[None, :]) & (kcol[:, None] < cs[None, :] + 16)
            valid = vrow[:, None] & vcol
            dr = np.clip(krow - r + 7, 0, 14)
            dc = np.clip(kcol[:, None] - c[None, :] + 15, 0, 30)
            vals = rpb[:, dr[:, None], dc]
            out[:, :, o * 64:(o + 1) * 64] = np.where(valid[None], vals, np.float32(NEG))
            o += 1
    return out


def host_rope_tables(q):
    lr = np.arange(32)
    row = (32 * q + lr).astype(np.float32)
    col = np.arange(64).astype(np.float32)
    n = 8
    inv = (np.float32(10000.0) ** (-np.arange(n, dtype=np.float32) / np.float32(n))).astype(np.float32)
    rowang = row[:, None] * inv[None, :]
    colang = col[:, None] * inv[None, :]
    ang = np.zeros((32, 64, 16), np.float32)
    ang[:, :, 0:8] = rowang[:, None, :]
    ang[:, :, 8:16] = colang[None, :, :]
    ang = ang.reshape(2048, 16)
    cos = np.cos(ang).astype(np.float32)
    sin = np.sin(ang).astype(np.float32)
    C = np.repeat(cos.T, 2, axis=0)
    Sn = np.repeat(sin.T, 2, axis=0)
    Cf = np.zeros((128, 2048), np.float32)
    Sf = np.zeros((128, 2048), np.float32)
    for base in (0, 64):
        Cf[base:base + 32] = C
        Sf[base:base + 32] = Sn
    return Cf, Sf


def bank(K, b):
    return K.ps[:, b * 512:(b + 1) * 512]


def mm(K, out, lhsT, rhs, start, stop, reads, writes):
    K.P.add("pe", lambda e: e.matmul(out, lhsT, rhs, start=start, stop=stop), reads=reads, writes=writes)


def setup_consts(K):
    P, A = K.P, K.A
    K.onesD = A.alloc([128], F32)
    K.ones256 = A.alloc([128], F32)
    K.ones128 = A.alloc([128], F32)
    K.ident = A.alloc([128], F32)
    K.onespad = A.alloc([2, 128], BF16)
    P.add("dve", lambda e: e.memset(K.onesD, 1.0 / 1024), writes=["consts"])
    P.add("dve", lambda e: e.memset(K.ones256, 1.0 / 256), writes=["consts"])
    P.add("dve", lambda e: e.memset(K.ones128, 1.0 / 128), writes=["consts"])
    P.add("dve", lambda e: e.memset(K.onespad, 0.0), writes=["consts"])
    P.add("dve", lambda e: e.memset(K.onespad[:, 0, 0:64], 1.0), writes=["consts"])
    P.add("dve", lambda e: e.memset(K.onespad[:, 1, 64:128], 1.0), writes=["consts"])
    P.add("sp", lambda e: e.dma_start(out=K.ident, in_=K.din["ident"]), writes=["ident"], key="ident")
    K.cact = A.alloc([8], F32)
    K.mod = [A.alloc([48], F32), A.alloc([48], F32)]
    K.bada = [A.alloc([48], F32), A.alloc([48], F32)]
    K.norm_ctr = 0


def adaln(K, layers):
    P, A = K.P, K.A
    m0 = A.mark()
    wa = [A.alloc([8, 768], F32), A.alloc([8, 768], F32)]
    P.add("sp", lambda e: e.dma_start(out=K.cact, in_=K.din["cT"]), writes=["cact"], key="cact")
    P.add("act", lambda e: e.activation(K.cact, K.cact, AF.Silu), reads=["cact"], writes=["cact"])
    cnt = 0
    for li in layers:
        P.add("sp", lambda e, li=li: e.dma_start(out=K.bada[li], in_=K.din["b_ada"][li]), writes=[f"bada{li}"], key=f"bada{li}")
        wv = K.din["w_ada"][li].rearrange("(kc p) n -> p kc n", p=128)
        for jb in range(8):
            buf = wa[cnt % 2]
            res = f"wa{cnt % 2}"
            cnt += 1
            P.add("sp", lambda e, buf=buf, jb=jb, wv=wv: e.dma_start(out=buf, in_=wv[:, :, jb * 768:(jb + 1) * 768]),
                  writes=[res], key=res)
            for j in range(6):
                col = jb * 6 + j
                for kc in range(8):
                    mm(K, bank(K, 0)[:, col:col + 1], buf[:, kc, j * 128:(j + 1) * 128], K.cact[:, kc:kc + 1],
                       kc == 0, kc == 7, [res, "cact"], ["ps0"])
        mod = K.mod[li]
        P.add("dve", lambda e, mod=mod, li=li: e.tensor_tensor(mod, bank(K, 0)[:, 0:48], K.bada[li], ALU.add),
              reads=["ps0", f"bada{li}"], writes=[f"mod{li}"])
        P.add("dve", lambda e, mod=mod: e.tensor_scalar_add(mod[:, 8:16], mod[:, 8:16], 1.0), reads=[f"mod{li}"], writes=[f"mod{li}"])
        P.add("dve", lambda e, mod=mod: e.tensor_scalar_add(mod[:, 32:40], mod[:, 32:40], 1.0), reads=[f"mod{li}"], writes=[f"mod{li}"])
    P.barrier()
    A.reset(m0)


def alloc_norm_scratch(K):
    A = K.A
    K.sq = [A.alloc([512], F32), A.alloc([512], F32)]
    K.rs = [A.alloc([512], F32), A.alloc([512], F32)]
    K.ntmp = [A.alloc([512], F32), A.alloc([512], F32)]


def modulate_block(K, src_fn, src_res, n, mod, modres, sc0, sh0, dst_fn, dst_res_fn, nb0):
    P = K.P
    i = K.norm_ctr
    K.norm_ctr += 1
    psb = nb0 + (i % 2)
    for c in range(8):
        sq = K.sq[c % 2]
        P.add("pool", lambda e, sq=sq, c=c: e.tensor_tensor(sq[:, :n], src_fn(c), src_fn(c), ALU.mult),
              reads=[src_res(c)], writes=[f"sq{c % 2}"])
        mm(K, bank(K, psb)[:, :n], K.onesD, sq[:, :n], c == 0, c == 7, [f"sq{c % 2}", "consts"], [f"ps{psb}"])
    rs = K.rs[i % 2]
    rr = f"rs{i % 2}"
    P.add("act", lambda e: e.activation(rs[:, :n], bank(K, psb)[:, :n], AF.Sqrt, bias=EPS, scale=1.0),
          reads=[f"ps{psb}"], writes=[rr])
    P.add("dve", lambda e: e.reciprocal(rs[:, :n], rs[:, :n]), reads=[rr], writes=[rr])
    for c in range(8):
        if mod is not None:
            tmp = K.ntmp[c % 2]
            tr = f"ntmp{c % 2}"
            P.add("dve", lambda e, c=c, tmp=tmp: e.scalar_tensor_tensor(tmp[:, :n], src_fn(c), mod[:, sc0 + c:sc0 + c + 1], rs[:, :n],
                                                                        ALU.mult, ALU.mult),
                  reads=[src_res(c), rr, modres], writes=[tr])
            P.add("act", lambda e, c=c, tmp=tmp: e.activation(dst_fn(c), tmp[:, :n], AF.Identity, bias=mod[:, sh0 + c:sh0 + c + 1], scale=1.0),
                  reads=[tr, modres], writes=[dst_res_fn(c)])
        else:
            P.add("dve", lambda e, c=c: e.scalar_tensor_tensor(dst_fn(c), src_fn(c), K.fscale[:, c:c + 1], rs[:, :n],
                                                               ALU.mult, ALU.mult),
                  reads=[src_res(c), rr, "fscale"], writes=[dst_res_fn(c)])


def load_x(K, src_dram, tok0, dres=None):
    P = K.P
    v = src_dram.rearrange("(c p) t -> p c t", p=128)
    for c in range(8):
        P.add("sp", lambda e, c=c: e.dma_start(out=K.x[:, c, :], in_=v[:, c, tok0:tok0 + T]),
              reads=([f"{dres}{c}"] if dres else []), writes=[f"x{c}_{tb}" for tb in range(4)], key=f"x{c}")


def xres(c, tb):
    return f"x{c}_{tb}"


def proj_residual(K, w_dram, inT, in_res, mod, modres, g0):
    P, A = K.P, K.A
    m0 = A.mark()
    wo = A.alloc([8, 1024], BF16)
    wv = w_dram.rearrange("(kc p) n -> p kc n", p=128)
    for hf in range(2):
        P.add("pool", lambda e, hf=hf: e.dma_start(out=wo[:, hf * 4:(hf + 1) * 4, :], in_=wv[:, hf * 4:(hf + 1) * 4, :]),
              writes=[f"wo{hf}"], key=f"wo{hf}")
    k = 0
    for tb in range(4):
        for oc in range(8):
            b = 4 + (k % 2)
            k += 1
            for kc in range(8):
                mm(K, bank(K, b), wo[:, kc, oc * 128:(oc + 1) * 128], inT[:, kc, tb * 512:(tb + 1) * 512],
                   kc == 0, kc == 7, [f"wo{kc // 4}", in_res(kc)], [f"ps{b}"])
            P.add("dve", lambda e, b=b, oc=oc, tb=tb: e.scalar_tensor_tensor(
                K.x[:, oc, tb * 512:(tb + 1) * 512], bank(K, b), mod[:, g0 + oc:g0 + oc + 1],
                K.x[:, oc, tb * 512:(tb + 1) * 512], ALU.mult, ALU.add),
                reads=[f"ps{b}", xres(oc, tb), modres], writes=[xres(oc, tb)])
    P.barrier()
    A.reset(m0)


def swiglu_stream(K, hT, hres, groups, mod, modres, g0, comb_fn=None):
    P, A = K.P, K.A
    NF = max(g["nf"] for g in groups)
    Wg = [A.alloc([8, NF * 128], BF16) for _ in range(2)]
    Wu = [A.alloc([8, NF * 128], BF16) for _ in range(2)]
    Wd = [A.alloc([NF, 1024], BF16) for _ in range(2)]
    act = [A.alloc([NF, 512], BF16) for _ in range(2)]
    t1 = [A.alloc([512], BF16) for _ in range(2)]
    t2 = [A.alloc([512], BF16) for _ in range(2)]
    steps = [(gi, tb) for gi in range(len(groups)) for tb in range(4)]
    loaded = set()
    ctr = dict(gu=0, y=0)

    def ensure_loaded(gi):
        if gi in loaded or gi >= len(groups):
            return
        loaded.add(gi)
        par = gi % 2
        groups[gi]["load"](K, Wg[par], Wu[par], Wd[par], par)

    def emit_gu(si):
        gi, tb = steps[si]
        g = groups[gi]
        par = gi % 2
        assert gi in loaded
        ab = act[si % 2]
        ar = f"act{si % 2}"
        comb = comb_fn(g, tb) if comb_fn is not None else None
        for fi in range(g["nf"]):
            k = ctr["gu"]
            ctr["gu"] += 1
            bg = 0 + (k % 2)
            bu = 2 + (k % 2)
            for kc in range(8):
                mm(K, bank(K, bg), Wg[par][:, kc, fi * 128:(fi + 1) * 128], hT[:, kc, tb * 512:(tb + 1) * 512],
                   kc == 0, kc == 7, [f"Wg{par}", hres(kc)], [f"ps{bg}"])
            for kc in range(8):
                mm(K, bank(K, bu), Wu[par][:, kc, fi * 128:(fi + 1) * 128], hT[:, kc, tb * 512:(tb + 1) * 512],
                   kc == 0, kc == 7, [f"Wu{par}", hres(kc)], [f"ps{bu}"])
            tt = t1[k % 2]
            tr = f"t1_{k % 2}"
            P.add("act", lambda e, tt=tt, bg=bg: e.activation(tt, bank(K, bg), AF.Silu), reads=[f"ps{bg}"], writes=[tr])
            src, sr = tt, tr
            if comb is not None:
                cap, cres = comb
                t2b = t2[k % 2]
                t2r = f"t2_{k % 2}"
                P.add("pool", lambda e, t2b=t2b, tt=tt, cap=cap: e.tensor_tensor(t2b, tt, cap, ALU.mult),
                      reads=[tr, cres], writes=[t2r])
                src, sr = t2b, t2r
            P.add("dve", lambda e, ab=ab, fi=fi, src=src, bu=bu: e.tensor_tensor(ab[:, fi, :], src, bank(K, bu), ALU.mult),
                  reads=[sr, f"ps{bu}"], writes=[ar])

    def emit_down(si):
        gi, tb = steps[si]
        g = groups[gi]
        par = gi % 2
        ab = act[si % 2]
        ar = f"act{si % 2}"
        nf = g["nf"]
        for oc in range(8):
            k = ctr["y"]
            ctr["y"] += 1
            b = 4 + (k % 3)
            for fi in range(nf):
                mm(K, bank(K, b), Wd[par][:, fi, oc * 128:(oc + 1) * 128], ab[:, fi, :], fi == 0, fi == nf - 1,
                   [f"Wd{par}", ar], [f"ps{b}"])
            P.add("dve", lambda e, b=b, oc=oc, tb=tb: e.scalar_tensor_tensor(
                K.x[:, oc, tb * 512:(tb + 1) * 512], bank(K, b), mod[:, g0 + oc:g0 + oc + 1],
                K.x[:, oc, tb * 512:(tb + 1) * 512], ALU.mult, ALU.add),
                reads=[f"ps{b}", xres(oc, tb), modres], writes=[xres(oc, tb)])

    ensure_loaded(0)
    ensure_loaded(1)
    emit_gu(0)
    for si in range(len(steps)):
        if si + 1 < len(steps):
            emit_gu(si + 1)
        emit_down(si)
        if steps[si][1] == 3:
            ensure_loaded(steps[si][0] + 2)


def phase_A(K):
    P, A, din = K.P, K.A, K.din
    setup_consts(K)
    adaln(K, [0, 1])
    mod0, mod1 = K.mod
    mA = A.mark()
    hT = A.alloc([8, TE], BF16)
    OT = A.alloc([8, T], BF16)
    m1 = A.mark()
    alloc_norm_scratch(K)
    xs = [A.alloc([8, 512], F32), A.alloc([8, 512], F32)]
    xv = din["xT"].rearrange("(c p) t -> p c t", p=128)

    def hres(c, lo, hi):
        return [f"h{c}_{tb}" for tb in range(lo // 512, (hi - 1) // 512 + 1)]

    for tb in range(5):
        buf = xs[tb % 2]
        P.add("sp", lambda e, buf=buf, tb=tb: e.dma_start(out=buf, in_=xv[:, :, tb * 512:(tb + 1) * 512]),
              writes=[f"xs{tb % 2}"], key=f"xs{tb % 2}")
        modulate_block(K, lambda c, buf=buf: buf[:, c, :], lambda c, tb=tb: f"xs{tb % 2}", 512, mod0, "mod0", 8, 0,
                       lambda c, tb=tb: hT[:, c, tb * 512:(tb + 1) * 512], lambda c, tb=tb: f"h{c}_{tb}", 6)
    P.barrier()
    A.reset(m1)
    wqkv = [A.alloc([3, 8, 128], BF16) for _ in range(2)]
    qT = [A.alloc([T], BF16) for _ in range(2)]
    kT = [A.alloc([TE], BF16) for _ in range(2)]
    Vp = [A.alloc([20, 2, 128], BF16) for _ in range(2)]
    biasb = [A.alloc([NA_NBLK * 64], F32) for _ in range(2)]
    stmp = [A.alloc([384], F32) for _ in range(3)]
    pT = [A.alloc([384], BF16) for _ in range(3)]
    rden = [A.alloc([512], F32) for _ in range(2)]
    for par in range(2):
        P.add("pool", lambda e, par=par: e.memset(Vp[par], 0.0), writes=[f"Vp{par}"])
    wv = din["na_w_qkv"].rearrange("(kc p) n -> p kc n", p=128)
    nm = ["wq", "wk", "wv"]
    PB = 7
    for p in range(8):
        par = p % 2
        for s in range(3):
            P.add("pool", lambda e, par=par, s=s, p=p: e.dma_start(out=wqkv[par][:, s], in_=wv[:, :, s * 1024 + p * 128:s * 1024 + (p + 1) * 128]),
                  writes=[f"{nm[s]}{par}"], key=f"{nm[s]}{par}")
        for h2 in range(2):
            h = 2 * p + h2
            P.add("sp", lambda e, h2=h2, h=h: e.dma_start(out=biasb[h2], in_=din["na_bias"][h]), writes=[f"bias{h2}"], key=f"bias{h2}")
        for tb in range(4):
            lo = 256 + tb * 512
            for kc in range(8):
                mm(K, bank(K, PB), wqkv[par][:, 0, kc, :], hT[:, kc, lo:lo + 512], kc == 0, kc == 7,
                   [f"wq{par}"] + hres(kc, lo, lo + 512), [f"ps{PB}"])
            P.add("act", lambda e, par=par, tb=tb: e.copy(qT[par][:, tb * 512:(tb + 1) * 512], bank(K, PB)),
                  reads=[f"ps{PB}"], writes=[f"qT{par}"])
        for tb in range(5):
            lo = tb * 512
            for kc in range(8):
                mm(K, bank(K, PB), wqkv[par][:, 1, kc, :], hT[:, kc, lo:lo + 512], kc == 0, kc == 7,
                   [f"wk{par}"] + hres(kc, lo, lo + 512), [f"ps{PB}"])
            P.add("act", lambda e, par=par, tb=tb: e.copy(kT[par][:, tb * 512:(tb + 1) * 512], bank(K, PB)),
                  reads=[f"ps{PB}"], writes=[f"kT{par}"])
        for g in range(5):
            for j in range(4):
                t = g * 4 + j
                for kc in range(8):
                    mm(K, bank(K, PB)[:, j * 128:(j + 1) * 128], hT[:, kc, t * 128:(t + 1) * 128], wqkv[par][:, 2, kc, :],
                       kc == 0, kc == 7, [f"wv{par}"] + hres(kc, t * 128, (t + 1) * 128), [f"ps{PB}"])
            pv = bank(K, PB).rearrange("p (t c) -> p t c", c=128)
            P.add("dve", lambda e, par=par, g=g, pv=pv: e.tensor_copy(Vp[par][:, g * 4:(g + 1) * 4, 0, 0:64], pv[:, :, 0:64]),
                  reads=[f"ps{PB}"], writes=[f"Vp{par}"])
            P.add("dve", lambda e, par=par, g=g, pv=pv: e.tensor_copy(Vp[par][:, g * 4:(g + 1) * 4, 1, 64:128], pv[:, :, 64:128]),
                  reads=[f"ps{PB}"], writes=[f"Vp{par}"])
        steps = [(lr, h2) for lr in range(32) for h2 in range(2)]

        def emit_S(si, par=par):
            lr, h2 = steps[si]
            sb = si % 3
            for j, t in enumerate(na_tiles(lr)):
                mm(K, bank(K, sb)[:, j * 64:(j + 1) * 64], kT[par][h2 * 64:(h2 + 1) * 64, t * 128:(t + 1) * 128],
                   qT[par][h2 * 64:(h2 + 1) * 64, lr * 64:(lr + 1) * 64], True, True, [f"kT{par}", f"qT{par}"], [f"ps{sb}"])

        def emit_soft(si):
            lr, h2 = steps[si]
            sb = si % 3
            nt = len(na_tiles(lr))
            bo = na_boff(lr)
            tmp = stmp[si % 3]
            pt = pT[si % 3]
            P.add("dve", lambda e: e.scalar_tensor_tensor(tmp[:, :nt * 64], bank(K, sb)[:, :nt * 64], 0.125,
                                                          biasb[h2][:, bo * 64:(bo + nt) * 64], ALU.mult, ALU.add),
                  reads=[f"ps{sb}", f"bias{h2}"], writes=[f"stmp{si % 3}"])
            P.add("act", lambda e: e.activation(pt[:, :nt * 64], tmp[:, :nt * 64], AF.Exp),
                  reads=[f"stmp{si % 3}"], writes=[f"pT{si % 3}"])

        def emit_PV(si, par=par, p=p):
            lr, h2 = steps[si]
            grp, slot = lr // 8, lr % 8
            ob = 3 + (grp % 2)
            db = 5 + (grp % 2)
            pt = pT[si % 3]
            tiles = na_tiles(lr)
            for j, t in enumerate(tiles):
                first = (h2 == 0 and j == 0)
                last = (h2 == 1 and j == len(tiles) - 1)
                mm(K, bank(K, ob)[:, slot * 64:(slot + 1) * 64], Vp[par][:, t, h2, :], pt[:, j * 64:(j + 1) * 64],
                   first, last, [f"Vp{par}", f"pT{si % 3}"], [f"ps{ob}"])
                mm(K, bank(K, db)[:, slot * 64:(slot + 1) * 64], K.onespad[:, h2, :], pt[:, j * 64:(j + 1) * 64],
                   first, last, ["consts", f"pT{si % 3}"], [f"ps{db}"])
            if slot == 7 and h2 == 1:
                rd = rden[0]
                P.add("dve", lambda e: e.reciprocal(rd, bank(K, db)), reads=[f"ps{db}"], writes=["rden"])
                P.add("dve", lambda e: e.tensor_tensor(OT[:, p, grp * 512:(grp + 1) * 512], bank(K, ob), rd, ALU.mult),
                      reads=[f"ps{ob}", "rden"], writes=[f"OT{p}"])

        emit_S(0)
        emit_S(1)
        for si in range(len(steps)):
            emit_soft(si)
            if si + 2 < len(steps):
                emit_S(si + 2)
            emit_PV(si)
    P.barrier()
    A.reset(mA)
    OT2 = A.alloc([8, TE], BF16)
    OT = A.alloc([8, T], BF16)
    K.x = A.alloc([8, T], F32)
    load_x(K, din["xT"], 256)
    proj_residual(K, din["na_w_o"], OT, lambda kc: f"OT{kc}", mod0, "mod0", 16)
    A.reset(mA)
    hT2 = A.alloc([8, T], BF16)
    skip = A.alloc([8, TE - T], BF16)
    skip2 = A.alloc([8, T], BF16)
    K.x = A.alloc([8, T], F32)
    alloc_norm_scratch(K)
    for tb in range(4):
        modulate_block(K, lambda c, tb=tb: K.x[:, c, tb * 512:(tb + 1) * 512], lambda c, tb=tb: xres(c, tb), 512, mod0, "mod0", 32, 24,
                       lambda c, tb=tb: hT2[:, c, tb * 512:(tb + 1) * 512], lambda c: f"h2_{c}", 6)
    gu = din["ffn_w_gu"].rearrange("(kc p) n -> p kc n", p=128)
    dn = din["ffn_w_down"].rearrange("(f p) n -> p f n", p=128)
    groups = []
    for gi in range(11):
        def load(K, Wg, Wu, Wd, par, gi=gi):
            K.P.add("pool", lambda e: e.dma_start(out=Wg[:, :, 0:256], in_=gu[:, :, gi * 256:(gi + 1) * 256]), writes=[f"Wg{par}"], key=f"Wg{par}")
            K.P.add("pool", lambda e: e.dma_start(out=Wu[:, :, 0:256], in_=gu[:, :, FFN + gi * 256:FFN + (gi + 1) * 256]), writes=[f"Wu{par}"], key=f"Wu{par}")
            K.P.add("pool", lambda e: e.dma_start(out=Wd[:, 0:2, :], in_=dn[:, gi * 2:(gi + 1) * 2, :]), writes=[f"Wd{par}"], key=f"Wd{par}")
        groups.append(dict(load=load, nf=2))
    swiglu_stream(K, hT2, lambda kc: f"h2_{kc}", groups, mod0, "mod0", 40)
    P.barrier()
    A.reset(mA)
    K.mA = mA
    hT3 = A.alloc([8, T], BF16)
    K.hT3 = hT3
    K.cq_region = A.alloc([2, T], BF16)
    K.OTmark = A.mark()
    K.OTreg = A.alloc([8, T], BF16)
    K.xmark = A.mark()
    K.x = A.alloc([8, T], F32)
    alloc_norm_scratch(K)
    for tb in range(4):
        modulate_block(K, lambda c, tb=tb: K.x[:, c, tb * 512:(tb + 1) * 512], lambda c, tb=tb: xres(c, tb), 512, mod1, "mod1", 8, 0,
                       lambda c, tb=tb: hT3[:, c, tb * 512:(tb + 1) * 512], lambda c: f"h3_{c}", 6)
    xo = din["x1"].rearrange("(c p) t -> p c t", p=128)
    for c in range(8):
        P.add("sp", lambda e, c=c: e.dma_start(out=xo[:, c, :], in_=K.x[:, c, :]), reads=[xres(c, tb) for tb in range(4)],
              writes=[f"x1d{c}"], key=f"x{c}")
    mla_latent(K, hT3, lambda kc: f"h3_{kc}")


def mla_latent(K, hT3, hres):
    P, A, din = K.P, K.A, K.din
    wd = A.alloc([8, 160], BF16)
    wsw = A.alloc([8, 32], BF16)
    kvn = A.alloc([1], F32)
    ropeC = A.alloc([T], F32)
    ropeS = A.alloc([T], F32)
    dkv = A.alloc([512], F32)
    sqb = A.alloc([512], F32)
    rsb = A.alloc([512], F32)
    lat = A.alloc([T], F32)
    krot = A.alloc([T], F32)
    ktmp = A.alloc([512], F32)
    wv = din["mla_w_down"].rearrange("(kc p) n -> p kc n", p=128)
    P.add("pool", lambda e: e.dma_start(out=wd, in_=wv[:, :, 256:416]), writes=["wd"], key="wd")
    P.add("sp", lambda e: e.dma_start(out=kvn, in_=din["kv_norm"]), writes=["kvn"], key="kvn")
    P.add("sp", lambda e: e.dma_start(out=ropeC, in_=din["ropeC"]), writes=["ropeC"], key="ropeC")
    P.add("sp", lambda e: e.dma_start(out=ropeS, in_=din["ropeS"]), writes=["ropeS"], key="ropeS")
    wr = wd[:, :, 128:160].rearrange("p k (i two) -> p k i two", two=2)
    ws = wsw.rearrange("p k (i two) -> p k i two", two=2)
    P.add("dve", lambda e: e.tensor_scalar_mul(ws[:, :, :, 0], wr[:, :, :, 1], -1.0), reads=["wd"], writes=["wsw"])
    P.add("dve", lambda e: e.tensor_copy(ws[:, :, :, 1], wr[:, :, :, 0]), reads=["wd"], writes=["wsw"])
    for tb in range(4):
        sl = slice(tb * 512, (tb + 1) * 512)
        for kc in range(8):
            mm(K, bank(K, 0), wd[:, kc, 0:128], hT3[:, kc, sl], kc == 0, kc == 7, ["wd", hres(kc)], ["ps0"])
        for kc in range(8):
            mm(K, bank(K, 1)[0:32, :], wd[:, kc, 128:160], hT3[:, kc, sl], kc == 0, kc == 7, ["wd", hres(kc)], ["ps1"])
        for kc in range(8):
            mm(K, bank(K, 2)[0:32, :], wsw[:, kc, :], hT3[:, kc, sl], kc == 0, kc == 7, ["wsw", hres(kc)], ["ps2"])
        P.add("act", lambda e: e.copy(dkv, bank(K, 0)), reads=["ps0"], writes=["dkv"])
        P.add("pool", lambda e: e.tensor_tensor(sqb, dkv, dkv, ALU.mult), reads=["dkv"], writes=["sqb"])
        mm(K, bank(K, 3), K.ones128, sqb, True, True, ["sqb", "consts"], ["ps3"])
        P.add("act", lambda e: e.activation(rsb, bank(K, 3), AF.Sqrt, bias=EPS, scale=1.0), reads=["ps3"], writes=["rsb"])
        P.add("dve", lambda e: e.reciprocal(rsb, rsb), reads=["rsb"], writes=["rsb"])
        P.add("dve", lambda e, sl=sl: e.scalar_tensor_tensor(lat[:, sl], dkv, kvn[:, 0:1], rsb, ALU.mult, ALU.mult),
              reads=["dkv", "rsb", "kvn"], writes=["lat"])
        P.add("dve", lambda e, sl=sl: e.tensor_tensor(krot[0:32, sl], bank(K, 1)[0:32, :], ropeC[0:32, sl], ALU.mult),
              reads=["ps1", "ropeC"], writes=["krot"])
        P.add("dve", lambda e, sl=sl: e.tensor_tensor(ktmp[0:32, :], bank(K, 2)[0:32, :], ropeS[0:32, sl], ALU.mult),
              reads=["ps2", "ropeS"], writes=["ktmp"])
        P.add("pool", lambda e, sl=sl: e.tensor_tensor(krot[0:32, sl], krot[0:32, sl], ktmp[0:32, :], ALU.add),
              reads=["krot", "ktmp"], writes=["krot"])
    P.add("sp", lambda e: e.dma_start(out=din["lat"][0:128, :], in_=lat), reads=["lat"], writes=["latd0"], key="lat")
    P.add("sp", lambda e: e.dma_start(out=din["lat"][128:160, :], in_=krot[0:32, :]), reads=["krot"], writes=["latd1"], key="krot")


def phase_B_prologue_unfused(K):
    P, A, din = K.P, K.A, K.din
    setup_consts(K)
    adaln(K, [1])
    mA = A.mark()
    K.mA = mA
    K.hT3 = A.alloc([8, T], BF16)
    K.cq_region = A.alloc([2, T], BF16)
    K.OTmark = A.mark()
    K.OTreg = A.alloc([8, T], BF16)
    K.xmark = A.mark()
    K.x = A.alloc([8, T], F32)
    load_x(K, din["x1"], 0)
    alloc_norm_scratch(K)
    for tb in range(4):
        modulate_block(K, lambda c, tb=tb: K.x[:, c, tb * 512:(tb + 1) * 512], lambda c, tb=tb: xres(c, tb), 512, K.mod[1], "mod1", 8, 0,
                       lambda c, tb=tb: K.hT3[:, c, tb * 512:(tb + 1) * 512], lambda c: f"h3_{c}", 6)
    P.barrier()


def phase_B(K):
    P, A, din = K.P, K.A, K.din
    mod1 = K.mod[1]
    mB = K.mA
    oT = K.OTreg
    cqT = K.cq_region
    hT3 = K.hT3
    m1 = K.xmark
    A.reset(m1)
    wdq = A.alloc([8, 256], BF16)
    qn = A.alloc([2], F32)
    dq = [A.alloc([512], F32), A.alloc([512], F32)]
    sqq = [A.alloc([512], F32), A.alloc([512], F32)]
    rsq = A.alloc([512], F32)
    wv = din["mla_w_down"].rearrange("(kc p) n -> p kc n", p=128)
    P.add("pool", lambda e: e.dma_start(out=wdq, in_=wv[:, :, 0:256]), writes=["wdq"], key="wdq")
    P.add("sp", lambda e: e.dma_start(out=qn, in_=din["q_norm"]), writes=["qn"], key="qn")
    for tb in range(4):
        sl = slice(tb * 512, (tb + 1) * 512)
        for j in range(2):
            for kc in range(8):
                mm(K, bank(K, j), wdq[:, kc, j * 128:(j + 1) * 128], hT3[:, kc, sl], kc == 0, kc == 7, ["wdq", f"h3_{kc}"], [f"ps{j}"])
            P.add("act", lambda e, j=j: e.copy(dq[j], bank(K, j)), reads=[f"ps{j}"], writes=[f"dq{j}"])
            P.add("pool", lambda e, j=j: e.tensor_tensor(sqq[j], dq[j], dq[j], ALU.mult), reads=[f"dq{j}"], writes=[f"sqq{j}"])
            mm(K, bank(K, 2), K.ones256, sqq[j], j == 0, j == 1, [f"sqq{j}", "consts"], ["ps2"])
        P.add("act", lambda e: e.activation(rsq, bank(K, 2), AF.Sqrt, bias=EPS, scale=1.0), reads=["ps2"], writes=["rsq"])
        P.add("dve", lambda e: e.reciprocal(rsq, rsq), reads=["rsq"], writes=["rsq"])
        for j in range(2):
            P.add("dve", lambda e, j=j, sl=sl: e.scalar_tensor_tensor(cqT[:, j, sl], dq[j], qn[:, j:j + 1], rsq, ALU.mult, ALU.mult),
                  reads=[f"dq{j}", "rsq", "qn"], writes=["cqT"])
    P.barrier()
    A.reset(m1)
    save_off = A.off
    A.off = K.mA
    ckvT = A.alloc([S], BF16)
    kh0 = A.alloc([S], BF16)
    A.off = save_off
    khT = [kh0, A.alloc([S], BF16)]
    Vh = [A.alloc([64, 128], BF16) for _ in range(2)]
    qhT = [A.alloc([T], BF16) for _ in range(2)]
    wuq = A.alloc([2, 1536], BF16)
    wuqsw = A.alloc([2, 16, 96], BF16)
    wukv = A.alloc([2048], BF16)
    ropeC = A.alloc([T], F32)
    ropeS = A.alloc([T], F32)
    pT = [A.alloc([1024], BF16) for _ in range(3)]
    rt1 = [A.alloc([512], F32) for _ in range(2)]
    rt2 = [A.alloc([512], F32) for _ in range(2)]
    rd = [A.alloc([512], F32) for _ in range(2)]
    otmp = [A.alloc([512], BF16) for _ in range(2)]
    la = din["latall"]
    bsel = A.alloc([2], F32)
    P.add("sp", lambda e: e.dma_start(out=bsel, in_=din["bsel"]), writes=["bsel"], key="bsel")
    CH = 512
    stA = [A.alloc([CH], F32) for _ in range(2)]
    stB = [A.alloc([CH], F32) for _ in range(2)]
    for par in range(2):
        P.add("pool", lambda e, par=par: e.memset(Vh[par][:, :, 64:128], 1.0), writes=[f"Vh1_{par}"])
    nch = T // CH
    it = 0
    for i in range(4):
        for j in range(nch):
            sa, sb_ = stA[it % 2], stB[it % 2]
            ra, rb = f"stA{it % 2}", f"stB{it % 2}"
            it += 1
            r0, r1 = i * 160, (4 + i) * 160
            cs = slice(j * CH, (j + 1) * CH)
            ks = slice(i * T + j * CH, i * T + (j + 1) * CH)
            P.add("sp", lambda e, sa=sa, r0=r0, cs=cs: e.dma_start(out=sa[0:128, :], in_=la[r0:r0 + 128, cs]), reads=["latall"], writes=[ra], key=ra)
            P.add("sp", lambda e, sb_=sb_, r1=r1, cs=cs: e.dma_start(out=sb_[0:128, :], in_=la[r1:r1 + 128, cs]), reads=["latall"], writes=[rb], key=rb)
            P.add("dve", lambda e, sa=sa: e.tensor_scalar_mul(sa, sa, bsel[:, 0:1]), reads=[ra, "bsel"], writes=[ra])
            P.add("dve", lambda e, sa=sa, sb_=sb_, ks=ks: e.scalar_tensor_tensor(ckvT[:, ks], sb_, bsel[:, 1:2], sa, ALU.mult, ALU.add),
                  reads=[ra, rb, "bsel"], writes=[f"ckv{i}"])
    for i in range(4):
        for j in range(nch):
            sa, sb_ = stA[it % 2], stB[it % 2]
            ra, rb = f"stA{it % 2}", f"stB{it % 2}"
            it += 1
            r0, r1 = i * 160 + 128, (4 + i) * 160 + 128
            cs = slice(j * CH, (j + 1) * CH)
            ks = slice(i * T + j * CH, i * T + (j + 1) * CH)
            P.add("sp", lambda e, sa=sa, r0=r0, cs=cs: e.dma_start(out=sa[64:96, :], in_=la[r0:r0 + 32, cs]), reads=["latall"], writes=[ra], key=ra)
            P.add("sp", lambda e, sb_=sb_, r1=r1, cs=cs: e.dma_start(out=sb_[64:96, :], in_=la[r1:r1 + 32, cs]), reads=["latall"], writes=[rb], key=rb)
            P.add("pool", lambda e, sa=sa: e.tensor_scalar_mul(sa[64:96, :], sa[64:96, :], bsel[64:96, 0:1]), reads=[ra, "bsel"], writes=[ra])
            for par in range(2):
                P.add("dve", lambda e, sa=sa, sb_=sb_, ks=ks, par=par: e.scalar_tensor_tensor(
                    khT[par][64:96, ks], sb_[64:96, :], bsel[64:96, 1:2], sa[64:96, :], ALU.mult, ALU.add),
                    reads=[ra, rb, "bsel"], writes=[f"khr{par}"])
    uq = din["mla_w_uq"].rearrange("(kc p) n -> p kc n", p=128)
    P.add("pool", lambda e: e.dma_start(out=wuq, in_=uq), writes=["wuq"], key="wuq")
    P.add("pool", lambda e: e.dma_start(out=wukv, in_=din["mla_w_ukv"]), writes=["wukv"], key="wukv")
    P.add("sp", lambda e: e.dma_start(out=ropeC, in_=din["ropeC"]), writes=["ropeC"], key="ropeC")
    P.add("sp", lambda e: e.dma_start(out=ropeS, in_=din["ropeS"]), writes=["ropeS"], key="ropeS")
    P.add("pool", lambda e: e.memset(wuqsw, 0.0), writes=["wuqsw"])
    wq4 = wuq.rearrange("p k (h f) -> p k h f", h=16)
    src = wq4[:, :, :, 64:96].rearrange("p k h (i two) -> p k h i two", two=2)
    dst = wuqsw[:, :, :, 64:96].rearrange("p k h (i two) -> p k h i two", two=2)
    for kc in range(2):
        P.add("dve", lambda e, kc=kc: e.tensor_scalar_mul(dst[:, kc, :, :, 0], src[:, kc, :, :, 1], -1.0), reads=["wuq", "wuqsw"], writes=["wuqsw"])
        P.add("dve", lambda e, kc=kc: e.tensor_copy(dst[:, kc, :, :, 1], src[:, kc, :, :, 0]), reads=["wuq", "wuqsw"], writes=["wuqsw"])
    SCALE = float(96 ** -0.5)
    PBK = [6, 7]
    pctr = [0]

    def pbank():
        b = PBK[pctr[0] % 2]
        pctr[0] += 1
        return b

    def emit_proj(h):
        par = h % 2
        for kb in range(16):
            b = pbank()
            mm(K, bank(K, b)[0:64, :], wukv[:, h * 128:h * 128 + 64], ckvT[:, kb * 512:(kb + 1) * 512], True, True,
               ["wukv", f"ckv{kb // 4}"], [f"ps{b}"])
            P.add("dve", lambda e, b=b, kb=kb, par=par: e.tensor_copy(khT[par][0:64, kb * 512:(kb + 1) * 512], bank(K, b)[0:64, :]),
                  reads=[f"ps{b}"], writes=[f"kh{par}_{kb}"])
        for g in range(8):
            b = pbank()
            for j in range(8):
                kt = g * 8 + j
                mm(K, bank(K, b)[:, j * 64:(j + 1) * 64], ckvT[:, kt * 128:(kt + 1) * 128], wukv[:, h * 128 + 64:h * 128 + 128], True, True,
                   ["wukv", f"ckv{kt // 16}"], [f"ps{b}"])
            pv = bank(K, b).rearrange("p (t c) -> p t c", c=64)
            P.add("dve", lambda e, g=g, par=par, pv=pv: e.tensor_copy(Vh[par][:, g * 8:(g + 1) * 8, 0:64], pv),
                  reads=[f"ps{b}"], writes=[f"Vh{par}_{g}"])
        for tb in range(4):
            sl = slice(tb * 512, (tb + 1) * 512)
            b1 = pbank()
            b2 = pbank()
            for kc in range(2):
                mm(K, bank(K, b1)[0:96, :], wuq[:, kc, h * 96:(h + 1) * 96], cqT[:, kc, sl], kc == 0, kc == 1, ["wuq", "cqT"], [f"ps{b1}"])
            for kc in range(2):
                mm(K, bank(K, b2)[0:96, :], wuqsw[:, kc, h, :], cqT[:, kc, sl], kc == 0, kc == 1, ["wuqsw", "cqT"], [f"ps{b2}"])
            i = tb % 2
            P.add("dve", lambda e, b1=b1, sl=sl, par=par: e.tensor_copy(qhT[par][0:64, sl], bank(K, b1)[0:64, :]),
                  reads=[f"ps{b1}"], writes=[f"qh{par}"])
            P.add("dve", lambda e, b1=b1, sl=sl, i=i: e.tensor_tensor(rt1[i][64:96, :], bank(K, b1)[64:96, :], ropeC[64:96, sl], ALU.mult),
                  reads=[f"ps{b1}", "ropeC"], writes=[f"rt1_{i}"])
            P.add("dve", lambda e, b2=b2, sl=sl, i=i: e.tensor_tensor(rt2[i][64:96, :], bank(K, b2)[64:96, :], ropeS[64:96, sl], ALU.mult),
                  reads=[f"ps{b2}", "ropeS"], writes=[f"rt2_{i}"])
            P.add("pool", lambda e, sl=sl, i=i, par=par: e.tensor_tensor(qhT[par][64:96, sl], rt1[i][64:96, :], rt2[i][64:96, :], ALU.add),
                  reads=[f"rt1_{i}", f"rt2_{i}"], writes=[f"qh{par}"])

    sctr = [0]

    def emit_S(h, qb, k2):
        par = h % 2
        si = sctr[0]
        sctr[0] += 1
        b0 = (si % 2) * 2
        for j in range(2):
            kt = k2 * 2 + j
            mm(K, bank(K, b0 + j), khT[par][0:96, kt * 128:(kt + 1) * 128], qhT[par][0:96, qb * 512:(qb + 1) * 512], True, True,
               [f"kh{par}_{kt // 4}", f"khr{par}", f"qh{par}"], [f"ps{b0 + j}"])
        return si

    def emit_exp_pv(h, qb, k2, si):
        par = h % 2
        b0 = (si % 2) * 2
        pt = pT[si % 3]
        ob = 4 + ((h * 4 + qb) % 2)
        P.add("act", lambda e: e.activation(pt, K.ps[:, b0 * 512:(b0 + 2) * 512], AF.Exp, scale=SCALE),
              reads=[f"ps{b0}", f"ps{b0 + 1}"], writes=[f"pT{si % 3}"])
        for j in range(2):
            kt = k2 * 2 + j
            mm(K, bank(K, ob), Vh[par][:, kt, :], pt[:, j * 512:(j + 1) * 512], kt == 0, kt == 63,
               [f"Vh{par}_{kt // 8}", f"Vh1_{par}", f"pT{si % 3}"], [f"ps{ob}"])
        if k2 == 31:
            i = (h * 4 + qb) % 2
            c = h // 2
            sl = slice(qb * 512, (qb + 1) * 512)
            P.add("dve", lambda e: e.reciprocal(rd[i][64:128, :], bank(K, ob)[64:128, :]), reads=[f"ps{ob}"], writes=[f"rd{i}"])
            P.add("dve", lambda e: e.tensor_copy(rd[i][0:64, :], rd[i][64:128, :]), reads=[f"rd{i}"], writes=[f"rd{i}"])
            if par == 0:
                P.add("dve", lambda e: e.tensor_tensor(oT[0:64, c, sl], bank(K, ob)[0:64, :], rd[i][0:64, :], ALU.mult),
                      reads=[f"ps{ob}", f"rd{i}"], writes=[f"oT{c}"])
            else:
                P.add("dve", lambda e: e.tensor_tensor(otmp[i][0:64, :], bank(K, ob)[0:64, :], rd[i][0:64, :], ALU.mult),
                      reads=[f"ps{ob}", f"rd{i}"], writes=[f"otmp{i}"])
                P.add("dve", lambda e: e.tensor_copy(oT[64:128, c, sl], otmp[i][0:64, :]), reads=[f"otmp{i}"], writes=[f"oT{c}"])

    emit_proj(0)
    seq = [(h, qb, k2) for h in range(16) for qb in range(4) for k2 in range(32)]
    pend = emit_S(*seq[0])
    for idx, (h, qb, k2) in enumerate(seq):
        nxt = None
        if idx + 1 < len(seq):
            if seq[idx + 1][1:] == (0, 0) and False:
                pass
            nxt = emit_S(*seq[idx + 1])
        emit_exp_pv(h, qb, k2, pend)
        pend = nxt
        if qb == 1 and k2 == 31 and h + 1 < 16:
            emit_proj(h + 1)
    P.barrier()
    A.reset(m1)
    K.x = A.alloc([8, T], F32)
    load_x(K, din["x1"], 0, dres="x1d")
    proj_residual(K, din["mla_w_o"], oT, lambda kc: f"oT{kc}", mod1, "mod1", 16)
    A.reset(mB)
    hT4 = A.alloc([8, T], BF16)
    skip = A.alloc([2, T], BF16)
    skip2 = A.alloc([8, T], BF16)
    assert A.mark() == K.xmark
    K.x = A.alloc([8, T], F32)
    mX = A.mark()
    alloc_norm_scratch(K)
    for tb in range(4):
        modulate_block(K, lambda c, tb=tb: K.x[:, c, tb * 512:(tb + 1) * 512], lambda c, tb=tb: xres(c, tb), 512, mod1, "mod1", 32, 24,
                       lambda c, tb=tb: hT4[:, c, tb * 512:(tb + 1) * 512], lambda c: f"h4_{c}", 6)
    save_off = A.off
    A.off = K.OTmark
    wr = A.alloc([8, 8], BF16)
    lg = A.alloc([16, 8], F32)
    eq = A.alloc([16, 8], F32)
    l2 = A.alloc([16, 8], F32)
    ex = A.alloc([16, 8], F32)
    comb = A.alloc([16, 8], F32)
    mx1 = A.alloc([16], F32)
    mx2 = A.alloc([16], F32)
    ssum = A.alloc([16], F32)
    onesF = A.alloc([128], F32)
    cm = [A.alloc([128], F32) for _ in range(2)]
    cbc = [A.alloc([T], F32) for _ in range(2)]
    assert A.off <= K.xmark
    A.off = save_off
    P.add("dve", lambda e: e.memset(onesF, 1.0), writes=["onesF"])
    P.add("pool", lambda e: e.dma_start(out=wr, in_=din["moe_w_router"].rearrange("(kc p) n -> p kc n", p=128)), writes=["wr"], key="wr")
    for tt in range(16):
        for kc in range(8):
            mm(K, bank(K, 6)[:, tt * 8:(tt + 1) * 8], hT4[:, kc, tt * 128:(tt + 1) * 128], wr[:, kc, :], kc == 0, kc == 7,
               ["wr", f"h4_{kc}"], ["ps6"])
    lgf = lg.rearrange("p a b -> p (a b)")
    P.add("dve", lambda e: e.tensor_copy(lgf, bank(K, 6)[:, 0:128]), reads=["ps6"], writes=["lg"])
    X = mybir.AxisListType.X

    def bc(v):
        return v.unsqueeze(2).to_broadcast([128, 16, 8])

    P.add("dve", lambda e: e.tensor_reduce(mx1, lg, X, ALU.max), reads=["lg"], writes=["mx1"])
    P.add("dve", lambda e: e.tensor_tensor(eq, lg, bc(mx1), ALU.is_equal), reads=["lg", "mx1"], writes=["eq"])
    P.add("dve", lambda e: e.scalar_tensor_tensor(l2, eq, -1e30, lg, ALU.mult, ALU.add), reads=["eq", "lg"], writes=["l2"])
    P.add("dve", lambda e: e.tensor_reduce(mx2, l2, X, ALU.max), reads=["l2"], writes=["mx2"])
    P.add("dve", lambda e: e.tensor_tensor(eq, lg, bc(mx2), ALU.is_ge), reads=["lg", "mx2", "eq"], writes=["eq"])
    P.add("dve", lambda e: e.tensor_tensor(l2, lg, bc(mx1), ALU.subtract), reads=["lg", "mx1", "l2"], writes=["l2"])
    P.add("act", lambda e: e.activation(ex, l2, AF.Exp), reads=["l2"], writes=["ex"])
    P.add("dve", lambda e: e.tensor_tensor(ex, ex, eq, ALU.mult), reads=["ex", "eq"], writes=["ex"])
    P.add("dve", lambda e: e.tensor_reduce(ssum, ex, X, ALU.add), reads=["ex"], writes=["ssum"])
    P.add("dve", lambda e: e.reciprocal(ssum, ssum), reads=["ssum"], writes=["ssum"])
    P.add("dve", lambda e: e.tensor_tensor(comb, ex, bc(ssum), ALU.mult), reads=["ex", "ssum"], writes=["comb"])

    def make_cbc(ex_):
        buf = cbc[ex_ % 2]
        res = f"cbc{ex_ % 2}"
        for tt in range(16):
            cmb = cm[tt % 2]
            P.add("dve", lambda e, cmb=cmb, tt=tt: e.tensor_scalar_mul(cmb, onesF, comb[:, tt, ex_:ex_ + 1]),
                  reads=["comb", "onesF"], writes=[f"cm{tt % 2}"])
            mm(K, bank(K, 7)[:, (tt % 4) * 128:(tt % 4 + 1) * 128], cmb, K.ident, True, True, [f"cm{tt % 2}", "ident"], ["ps7"])
            if tt % 4 == 3:
                t0 = tt - 3
                P.add("act", lambda e, t0=t0: e.copy(buf[:, t0 * 128:(t0 + 4) * 128], bank(K, 7)), reads=["ps7"], writes=[res])

    gu = din["moe_w_gu"]
    dn = din["moe_w_down"]
    groups = []
    for ex_ in range(NEXP):
        guv = gu[ex_].rearrange("(kc p) n -> p kc n", p=128)
        dnv = dn[ex_].rearrange("(f p) n -> p f n", p=128)
        for gj in range(7):
            def load(K, Wg, Wu, Wd, par, guv=guv, dnv=dnv, gj=gj, ex_=ex_):
                if gj == 0:
                    make_cbc(ex_)
                K.P.add("pool", lambda e: e.dma_start(out=Wg[:, :, 0:256], in_=guv[:, :, gj * 256:(gj + 1) * 256]), writes=[f"Wg{par}"], key=f"Wg{par}")
                K.P.add("pool", lambda e: e.dma_start(out=Wu[:, :, 0:256], in_=guv[:, :, EDIM + gj * 256:EDIM + (gj + 1) * 256]), writes=[f"Wu{par}"], key=f"Wu{par}")
                K.P.add("pool", lambda e: e.dma_start(out=Wd[:, 0:2, :], in_=dnv[:, gj * 2:(gj + 1) * 2, :]), writes=[f"Wd{par}"], key=f"Wd{par}")
            groups.append(dict(load=load, nf=2, ex=ex_))

    def comb_fn(g, tb):
        e_ = g["ex"]
        return cbc[e_ % 2][:, tb * 512:(tb + 1) * 512], f"cbc{e_ % 2}"

    swiglu_stream(K, hT4, lambda kc: f"h4_{kc}", groups, mod1, "mod1", 40, comb_fn=comb_fn)
    P.barrier()
    A.reset(mX)
    alloc_norm_scratch(K)
    K.fscale = A.alloc([8], F32)
    P.add("sp", lambda e: e.dma_start(out=K.fscale, in_=din["final_norm"]), writes=["fscale"], key="fscale")
    ost = [A.alloc([8, 512], F32) for _ in range(2)]
    ov = din["out"].rearrange("(c p) t -> p c t", p=128)
    for tb in range(4):
        ob_ = ost[tb % 2]
        modulate_block(K, lambda c, tb=tb: K.x[:, c, tb * 512:(tb + 1) * 512], lambda c, tb=tb: xres(c, tb), 512, None, None, 0, 0,
                       lambda c, ob_=ob_: ob_[:, c, :], lambda c, tb=tb: f"ost{tb % 2}", 6)
        P.add("sp", lambda e, ob_=ob_, tb=tb: e.dma_start(out=ov[:, :, tb * 512:(tb + 1) * 512], in_=ob_), reads=[f"ost{tb % 2}"], key=f"ost{tb % 2}")


FUSED = False


def build_A():
    nc = bass.Bass("TRN2", target_bir_lowering=False)
    K = Ctx()
    K.nc = nc
    K.P = Prog(nc)
    K.A = Arena(nc, ARENA_WORDS)
    K.ps = nc.alloc_psum_tensor("ps", [128, 4096], F32)

    def inp(name, shape):
        return nc.dram_tensor(name, list(shape), F32, kind="ExternalInput").ap()

    def outp(name, shape):
        return nc.dram_tensor(name, list(shape), F32, kind="ExternalOutput").ap()

    K.din = dict(
        xT=inp("xT", (D, TE)), cT=inp("cT", (128, 8)), w_ada=inp("w_ada", (2, D, 6 * D)), b_ada=inp("b_ada", (2, 128, 48)),
        ident=inp("ident", (128, 128)), na_w_qkv=inp("na_w_qkv", (D, 3 * D)), na_bias=inp("na_bias", (16, 128, NA_NBLK * 64)),
        na_w_o=inp("na_w_o", (D, D)), ffn_w_gu=inp("ffn_w_gu", (D, 2 * FFN)), ffn_w_down=inp("ffn_w_down", (FFN, D)),
        mla_w_down=inp("mla_w_down", (D, 416)), kv_norm=inp("kv_norm", (128, 1)),
        ropeC=inp("ropeC", (128, T)), ropeS=inp("ropeS", (128, T)),
        x1=outp("x1", (D, T)), lat=outp("lat", (160, T)),
    )
    phase_A(K)
    K.P.emit()
    print("A stats", K.P.stats, "arena peak words", K.A.peak)
    return nc


def build_B():
    nc = bass.Bass("TRN2", target_bir_lowering=False)
    K = Ctx()
    K.nc = nc
    K.P = Prog(nc)
    K.A = Arena(nc, ARENA_WORDS)
    K.ps = nc.alloc_psum_tensor("ps", [128, 4096], F32)

    def inp(name, shape):
        return nc.dram_tensor(name, list(shape), F32, kind="ExternalInput").ap()

    def outp(name, shape):
        return nc.dram_tensor(name, list(shape), F32, kind="ExternalOutput").ap()

    K.din = dict(
        x1=inp("x1", (D, T)), latall=inp("latall", (NC * 160, T)), cT=inp("cT", (128, 8)), w_ada=inp("w_ada", (2, D, 6 * D)),
        b_ada=inp("b_ada", (2, 128, 48)), ident=inp("ident", (128, 128)), mla_w_down=inp("mla_w_down", (D, 416)),
        ropeC=inp("ropeC", (128, T)), ropeS=inp("ropeS", (128, T)), bsel=inp("bsel", (128, 2)),
        q_norm=inp("q_norm", (128, 2)), mla_w_uq=inp("mla_w_uq", (256, 1536)), mla_w_ukv=inp("mla_w_ukv", (128, 2048)),
        mla_w_o=inp("mla_w_o", (D, D)), moe_w_router=inp("moe_w_router", (D, 8)), moe_w_gu=inp("moe_w_gu", (NEXP, D, 2 * EDIM)),
        moe_w_down=inp("moe_w_down", (NEXP, EDIM, D)), final_norm=inp("final_norm", (128, 8)),
        out=outp("out", (D, T)),
    )
    phase_B_prologue_unfused(K)
    phase_B(K)
    K.P.emit()
    print("B stats", K.P.stats, "arena peak words", K.A.peak)
    return nc


def build_AB():
    nc = bass.Bass("TRN2", target_bir_lowering=False)
    K = Ctx()
    K.nc = nc
    K.P = Prog(nc)
    K.A = Arena(nc, ARENA_WORDS)
    K.ps = nc.alloc_psum_tensor("ps", [128, 4096], F32)

    def inp(name, shape):
        return nc.dram_tensor(name, list(shape), F32, kind="ExternalInput").ap()

    def outp(name, shape):
        return nc.dram_tensor(name, list(shape), F32, kind="ExternalOutput").ap()

    x1d = nc.dram_tensor("x1d", [D, T], F32)
    latd = nc.dram_tensor("latd", [160, T], F32)
    latall = nc.dram_tensor("latall", [NC * 160, T], F32)
    K.din = dict(
        xT=inp("xT", (D, TE)), cT=inp("cT", (128, 8)), w_ada=inp("w_ada", (2, D, 6 * D)), b_ada=inp("b_ada", (2, 128, 48)),
        ident=inp("ident", (128, 128)), na_w_qkv=inp("na_w_qkv", (D, 3 * D)), na_bias=inp("na_bias", (16, 128, NA_NBLK * 64)),
        na_w_o=inp("na_w_o", (D, D)), ffn_w_gu=inp("ffn_w_gu", (D, 2 * FFN)), ffn_w_down=inp("ffn_w_down", (FFN, D)),
        mla_w_down=inp("mla_w_down", (D, 416)), kv_norm=inp("kv_norm", (128, 1)),
        ropeC=inp("ropeC", (128, T)), ropeS=inp("ropeS", (128, T)), bsel=inp("bsel", (128, 2)),
        q_norm=inp("q_norm", (128, 2)), mla_w_uq=inp("mla_w_uq", (256, 1536)), mla_w_ukv=inp("mla_w_ukv", (128, 2048)),
        mla_w_o=inp("mla_w_o", (D, D)), moe_w_router=inp("moe_w_router", (D, 8)), moe_w_gu=inp("moe_w_gu", (NEXP, D, 2 * EDIM)),
        moe_w_down=inp("moe_w_down", (NEXP, EDIM, D)), final_norm=inp("final_norm", (128, 8)),
        x1=x1d.ap(), lat=latd.ap(), latall=latall.ap(),
        out=outp("out", (D, T)),
    )
    phase_A(K)
    K.P.barrier()
    K.P.add("pool", lambda e: e.collective_compute("AllGather", ALU.bypass, replica_groups=[list(range(NC))],
                                                   ins=[latd.ap().opt()], outs=[latall.ap().opt()]),
            reads=["latd0", "latd1"], writes=["latall"], key="cc", inc=1)
    phase_B(K)
    K.P.emit()
    print("AB stats", K.P.stats, "arena peak words", K.A.peak)
    return nc


def host_common(inputs):
    x = np.asarray(inputs["x"], np.float32)
    c = np.asarray(inputs["c"], np.float32)
    per_core = []
    for core in range(NC):
        b, q = core // 4, core % 4
        r0 = 32 * q - 4
        xe = np.zeros((TE, D), np.float32)
        lo, hi = max(r0, 0), min(r0 + 40, 128)
        xe[(lo - r0) * 64:(hi - r0) * 64] = x[b, lo * 64:hi * 64]
        Cf, Sf = host_rope_tables(q)
        per_core.append(dict(
            xT=np.ascontiguousarray(xe.T),
            cT=np.ascontiguousarray(c[b].reshape(8, 128).T),
            ropeC=Cf, ropeS=Sf,
        ))
    shared = dict(
        w_ada=np.asarray(inputs["w_ada"], np.float32),
        b_ada=np.ascontiguousarray(np.asarray(inputs["b_ada"], np.float32).reshape(2, 48, 128).transpose(0, 2, 1)),
        ident=np.eye(128, dtype=np.float32),
    )
    return per_core, shared


_CACHE = {}


def kernel(**inputs):
    per_core, shared = host_common(inputs)
    rpb = np.asarray(inputs["na_rpb"], np.float32)[0]
    bias_q = [host_na_bias(rpb, q) for q in range(4)]
    if FUSED and "AB" not in _CACHE:
        _CACHE["AB"] = build_AB()
    if not FUSED and "A" not in _CACHE:
        _CACHE["A"] = build_A()
        _CACHE["B"] = build_B()
    f32 = lambda k: np.asarray(inputs[k], np.float32)[0]
    common = dict(
        na_w_qkv=f32("na_w_qkv"), na_w_o=f32("na_w_o"), ffn_w_gu=f32("ffn_w_gu"), ffn_w_down=f32("ffn_w_down"),
        mla_w_down=f32("mla_w_down"), kv_norm=f32("mla_kv_norm").reshape(128, 1),
        q_norm=np.ascontiguousarray(f32("mla_q_norm").reshape(2, 128).T),
        mla_w_uq=f32("mla_w_uq"), mla_w_ukv=f32("mla_w_ukv"), mla_w_o=f32("mla_w_o"), moe_w_router=f32("moe_w_router"),
        moe_w_gu=f32("moe_w_gu"), moe_w_down=f32("moe_w_down"),
        final_norm=np.ascontiguousarray(np.asarray(inputs["final_norm"], np.float32).reshape(8, 128).T),
    )
    in_maps = []
    for core in range(NC):
        b = core // 4
        m = dict(per_core[core])
        m.update(shared)
        m.update(common)
        m["na_bias"] = bias_q[core % 4]
        bs = np.zeros((128, 2), np.float32)
        bs[:, b] = 1.0
        m["bsel"] = bs
        in_maps.append(m)
    if FUSED:
        res = run_bass_kernel_spmd(_CACHE["AB"], in_maps, core_ids=list(range(NC))).results
    else:
        keysA = ("xT", "cT", "w_ada", "b_ada", "ident", "na_w_qkv", "na_bias", "na_w_o", "ffn_w_gu", "ffn_w_down",
                 "mla_w_down", "kv_norm", "ropeC", "ropeS")
        resA = run_bass_kernel_spmd(_CACHE["A"], [{k: m[k] for k in keysA} for m in in_maps], core_ids=list(range(NC))).results
        if inputs.get("_debugA") is not None:
            return resA
        latall = np.ascontiguousarray(np.concatenate([resA[c]["lat"] for c in range(NC)], axis=0))
        keysB = ("cT", "w_ada", "b_ada", "ident", "mla_w_down", "ropeC", "ropeS", "bsel", "q_norm", "mla_w_uq", "mla_w_ukv",
                 "mla_w_o", "moe_w_router", "moe_w_gu", "moe_w_down", "final_norm")
        mapsB = []
        for c in range(NC):
            mb = {k: in_maps[c][k] for k in keysB}
            mb["x1"] = resA[c]["x1"]
            mb["latall"] = latall
            mapsB.append(mb)
        res = run_bass_kernel_spmd(_CACHE["B"], mapsB, core_ids=list(range(NC))).results
    out = np.empty((2, S, D), np.float32)
    for core in range(NC):
        b, q = core // 4, core % 4
        out[b, q * T:(q + 1) * T, :] = res[core]["out"].T
    return out
```

```python
import contextlib
import numpy as np
import concourse.bass as bass
import concourse.mybir as mybir
from concourse.bass_utils import run_bass_kernel_spmd

F32 = mybir.dt.float32
BF16 = mybir.dt.bfloat16
ALU = mybir.AluOpType
AF = mybir.ActivationFunctionType

ENGS = ("pe", "act", "dve", "pool", "sp")
SAME_ENGINE_SYNC = True

D = 1024
T = 2048
TE = 2560
S = 8192
NC = 8
EPS = 1e-6
FFN = 2816
NEXP = 8
EDIM = 1792
NEG = -30000.0
ARENA_WORDS = 50 * 1024


class Prog:
    def __init__(self, nc):
        self.nc = nc
        self.ops = []
        self.last_writer = {}
        self.readers = {}
        self.pending_bar = {}
        self.last_on_eng = {}
        self.last_dma_on_key = {}

    def add(self, eng, fn, reads=(), writes=(), key=None, inc=16):
        i = len(self.ops)
        deps = set()
        for r in reads:
            w = self.last_writer.get(r)
            if w is not None:
                deps.add(w)
        for w_ in writes:
            w = self.last_writer.get(w_)
            if w is not None:
                deps.add(w)
            deps.update(self.readers.get(w_, ()))
        if eng in self.pending_bar:
            deps.update(self.pending_bar.pop(eng))
        deps.discard(i)
        for r in reads:
            self.readers.setdefault(r, []).append(i)
        for w_ in writes:
            self.last_writer[w_] = i
            self.readers[w_] = []
        self.ops.append(dict(eng=eng, fn=fn, deps=deps, key=key, inc=inc))
        self.last_on_eng[eng] = i
        if key is not None:
            self.last_dma_on_key[key] = i
        return i

    def barrier(self):
        b = set(self.last_on_eng.values()) | set(self.last_dma_on_key.values())
        for e in ENGS:
            self.pending_bar[e] = set(b) | self.pending_bar.get(e, set())

    def emit(self, final_wait_eng="sp"):
        nc = self.nc
        ops = self.ops
        n = len(ops)
        fin = set(self.last_on_eng.values()) | set(self.last_dma_on_key.values())
        ops.append(dict(eng=final_wait_eng, fn=None, deps=fin, key=None, inc=0))
        waited_eng = {}
        waited_key = {}
        needed = [False] * (n + 1)
        for i, op in enumerate(ops):
            e = op["eng"]
            best_eng = {}
            best_key = {}
            for d in op["deps"]:
                od = ops[d]
                if od["key"] is not None:
                    k = od["key"]
                    if d > best_key.get(k, -1):
                        best_key[k] = d
                else:
                    se = od["eng"]
                    if se == e and (se == "pe" or not SAME_ENGINE_SYNC):
                        continue
                    if d > best_eng.get(se, -1):
                        best_eng[se] = d
            fdeps = []
            for se, d in best_eng.items():
                if waited_eng.get((e, se), -1) >= d:
                    continue
                waited_eng[(e, se)] = d
                fdeps.append(d)
            for k, d in best_key.items():
                if waited_key.get((e, k), -1) >= d:
                    continue
                waited_key[(e, k)] = d
                fdeps.append(d)
            op["fdeps"] = fdeps
            for d in fdeps:
                needed[d] = True
        seq = {e: 0 for e in ENGS}
        keycnt = {}
        for i, op in enumerate(ops):
            if op["key"] is not None:
                k = op["key"]
                keycnt[k] = keycnt.get(k, 0) + op["inc"]
                op["semval"] = keycnt[k]
            elif needed[i]:
                seq[op["eng"]] += 1
                op["semval"] = seq[op["eng"]]
        self.stats = dict(n_ops=n, n_sig=dict(seq), n_keys=len(keycnt))
        running = {}
        for i, op in enumerate(ops):
            waits = []
            for d in op["fdeps"]:
                od = ops[d]
                if od["key"] is not None:
                    waits.append(("key", od["key"], running[od["key"]]))
                else:
                    waits.append(("eng", od["eng"], od["semval"]))
            op["waits"] = waits
            op["sig"] = needed[i]
            if op["key"] is not None:
                running[op["key"]] = op["semval"]
        sems = {}
        with contextlib.ExitStack() as st:
            for e in ENGS:
                sems[("eng", e)] = st.enter_context(nc.semaphore("s_" + e))
            for k in keycnt:
                sems[("key", k)] = st.enter_context(nc.semaphore("k_" + k))
            block = st.enter_context(nc.Block())
            by_eng = {e: [op for op in ops if op["eng"] == e] for e in ENGS}

            def run(eh, e):
                for op in by_eng[e]:
                    for (kind, name, val) in op["waits"]:
                        eh.wait_ge(sems[(kind, name)], val)
                    if op["fn"] is None:
                        continue
                    ins = op["fn"](eh)
                    if op["key"] is not None:
                        ins.then_inc(sems[("key", op["key"])], op["inc"])
                    elif op["sig"]:
                        ins.then_inc(sems[("eng", e)], 1)

            @block.tensor
            def _(eh):
                run(eh, "pe")

            @block.scalar
            def _(eh):
                run(eh, "act")

            @block.vector
            def _(eh):
                run(eh, "dve")

            @block.gpsimd
            def _(eh):
                run(eh, "pool")

            @block.sync
            def _(eh):
                run(eh, "sp")


class Arena:
    def __init__(self, nc, nwords):
        self.t = nc.alloc_sbuf_tensor("arena", [128, nwords], F32)
        self.n = nwords
        self.off = 0
        self.peak = 0

    def alloc(self, shape, dtype):
        nel = int(np.prod(shape))
        nb = nel * (4 if dtype == F32 else 2)
        nw = (nb + 3) // 4
        nw = (nw + 7) // 8 * 8
        assert self.off + nw <= self.n, f"arena overflow {self.off}+{nw}>{self.n}"
        ap = self.t[:, self.off:self.off + nw]
        self.off += nw
        self.peak = max(self.peak, self.off)
        if dtype != F32:
            ap = ap.bitcast(dtype)
        ap = ap[:, 0:nel]
        if len(shape) == 2:
            ap = ap.rearrange("p (a b) -> p a b", a=shape[0], b=shape[1])
        elif len(shape) == 3:
            ap = ap.rearrange("p (a b c) -> p a b c", a=shape[0], b=shape[1], c=shape[2])
        elif len(shape) == 4:
            ap = ap.rearrange("p (a b c d) -> p a b c d", a=shape[0], b=shape[1], c=shape[2], d=shape[3])
        return ap

    def mark(self):
        return self.off

    def reset(self, m):
        self.off = m


class Ctx:
    pass


def na_tiles(lr):
    if lr < 4:
        return list(range(lr // 2, 6))
    if lr <= 28:
        return list(range(lr // 2, (lr + 7) // 2 + 1))
    return list(range(14, (lr + 7) // 2 + 1))


NA_SETS = [4, 5, 0, 1, 2, 3, 29, 30, 31]


def na_boff(lr):
    offs = {}
    o = 0
    for s in NA_SETS:
        offs[s] = o
        o += len(na_tiles(s))
    if lr < 4 or lr > 28:
        return offs[lr]
    return offs[4] if lr % 2 == 0 else offs[5]


NA_NBLK = sum(len(na_tiles(s)) for s in NA_SETS)


def host_na_bias(rpb, q):
    out = np.empty((16, 128, NA_NBLK * 64), np.float32)
    kk = np.arange(128)
    c = np.arange(64)
    cs = np.clip(c - 8, 0, 48)
    o = 0
    for s in NA_SETS:
        r = 32 * q + s
        rs = min(max(r - 4, 0), 120)
        for t in na_tiles(s):
            kr = 2 * t + kk // 64
            kcol = kk % 64
            krow = 32 * q - 4 + kr
            vrow = (krow >= rs) & (krow < rs + 8)
            vcol = (kcol[:, None] >= cs[None, :]) & (kcol[:, None] < cs[None, :] + 16)
            valid = vrow[:, None] & vcol
            dr = np.clip(krow - r + 7, 0, 14)
            dc = np.clip(kcol[:, None] - c[None, :] + 15, 0, 30)
            vals = rpb[:, dr[:, None], dc]
            out[:, :, o * 64:(o + 1) * 64] = np.where(valid[None], vals, np.float32(NEG))
            o += 1
    return out


def host_rope_tables(q):
    lr = np.arange(32)
    row = (32 * q + lr).astype(np.float32)
    col = np.arange(64).astype(np.float32)
    n = 8
    inv = (np.float32(10000.0) ** (-np.arange(n, dtype=np.float32) / np.float32(n))).astype(np.float32)
    rowang = row[:, None] * inv[None, :]
    colang = col[:, None] * inv[None, :]
    ang = np.zeros((32, 64, 16), np.float32)
    ang[:, :, 0:8] = rowang[:, None, :]
    ang[:, :, 8:16] = colang[None, :, :]
    ang = ang.reshape(2048, 16)
    cos = np.cos(ang).astype(np.float32)
    sin = np.sin(ang).astype(np.float32)
    C = np.repeat(cos.T, 2, axis=0)
    Sn = np.repeat(sin.T, 2, axis=0)
    Cf = np.zeros((128, 2048), np.float32)
    Sf = np.zeros((128, 2048), np.float32)
    for base in (0, 64):
        Cf[base:base + 32] = C
        Sf[base:base + 32] = Sn
    return Cf, Sf


def bank(K, b):
    return K.ps[:, b * 512:(b + 1) * 512]


def mm(K, out, lhsT, rhs, start, stop, reads, writes):
    K.P.add("pe", lambda e: e.matmul(out, lhsT, rhs, start=start, stop=stop), reads=reads, writes=writes)


def setup_consts(K):
    P, A = K.P, K.A
    K.onesD = A.alloc([128], F32)
    K.ones256 = A.alloc([128], F32)
    K.ones128 = A.alloc([128], F32)
    K.ident = A.alloc([128], F32)
    K.onespad = A.alloc([2, 128], BF16)
    P.add("dve", lambda e: e.memset(K.onesD, 1.0 / 1024), writes=["consts"])
    P.add("dve", lambda e: e.memset(K.ones256, 1.0 / 256), writes=["consts"])
    P.add("dve", lambda e: e.memset(K.ones128, 1.0 / 128), writes=["consts"])
    P.add("dve", lambda e: e.memset(K.onespad, 0.0), writes=["consts"])
    P.add("dve", lambda e: e.memset(K.onespad[:, 0, 0:64], 1.0), writes=["consts"])
    P.add("dve", lambda e: e.memset(K.onespad[:, 1, 64:128], 1.0), writes=["consts"])
    P.add("sp", lambda e: e.dma_start(out=K.ident, in_=K.din["ident"]), writes=["ident"], key="ident")
    K.cact = A.alloc([8], F32)
    K.mod = [A.alloc([48], F32), A.alloc([48], F32)]
    K.bada = [A.alloc([48], F32), A.alloc([48], F32)]
    K.norm_ctr = 0


def adaln(K, layers):
    P, A = K.P, K.A
    m0 = A.mark()
    wa = [A.alloc([8, 768], F32), A.alloc([8, 768], F32)]
    P.add("sp", lambda e: e.dma_start(out=K.cact, in_=K.din["cT"]), writes=["cact"], key="cact")
    P.add("act", lambda e: e.activation(K.cact, K.cact, AF.Silu), reads=["cact"], writes=["cact"])
    cnt = 0
    for li in layers:
        P.add("sp", lambda e, li=li: e.dma_start(out=K.bada[li], in_=K.din["b_ada"][li]), writes=[f"bada{li}"], key=f"bada{li}")
        wv = K.din["w_ada"][li].rearrange("(kc p) n -> p kc n", p=128)
        for jb in range(8):
            buf = wa[cnt % 2]
            res = f"wa{cnt % 2}"
            cnt += 1
            P.add("sp", lambda e, buf=buf, jb=jb, wv=wv: e.dma_start(out=buf, in_=wv[:, :, jb * 768:(jb + 1) * 768]),
                  writes=[res], key=res)
            for j in range(6):
                col = jb * 6 + j
                for kc in range(8):
                    mm(K, bank(K, 0)[:, col:col + 1], buf[:, kc, j * 128:(j + 1) * 128], K.cact[:, kc:kc + 1],
                       kc == 0, kc == 7, [res, "cact"], ["ps0"])
        mod = K.mod[li]
        P.add("dve", lambda e, mod=mod, li=li: e.tensor_tensor(mod, bank(K, 0)[:, 0:48], K.bada[li], ALU.add),
              reads=["ps0", f"bada{li}"], writes=[f"mod{li}"])
        P.add("dve", lambda e, mod=mod: e.tensor_scalar_add(mod[:, 8:16], mod[:, 8:16], 1.0), reads=[f"mod{li}"], writes=[f"mod{li}"])
        P.add("dve", lambda e, mod=mod: e.tensor_scalar_add(mod[:, 32:40], mod[:, 32:40], 1.0), reads=[f"mod{li}"], writes=[f"mod{li}"])
    P.barrier()
    A.reset(m0)


def alloc_norm_scratch(K):
    A = K.A
    K.sq = [A.alloc([512], F32), A.alloc([512], F32)]
    K.rs = [A.alloc([512], F32), A.alloc([512], F32)]
    K.ntmp = [A.alloc([512], F32), A.alloc([512], F32)]


def modulate_block(K, src_fn, src_res, n, mod, modres, sc0, sh0, dst_fn, dst_res_fn, nb0):
    P = K.P
    i = K.norm_ctr
    K.norm_ctr += 1
    psb = nb0 + (i % 2)
    for c in range(8):
        sq = K.sq[c % 2]
        P.add("pool", lambda e, sq=sq, c=c: e.tensor_tensor(sq[:, :n], src_fn(c), src_fn(c), ALU.mult),
              reads=[src_res(c)], writes=[f"sq{c % 2}"])
        mm(K, bank(K, psb)[:, :n], K.onesD, sq[:, :n], c == 0, c == 7, [f"sq{c % 2}", "consts"], [f"ps{psb}"])
    rs = K.rs[i % 2]
    rr = f"rs{i % 2}"
    P.add("act", lambda e: e.activation(rs[:, :n], bank(K, psb)[:, :n], AF.Sqrt, bias=EPS, scale=1.0),
          reads=[f"ps{psb}"], writes=[rr])
    P.add("dve", lambda e: e.reciprocal(rs[:, :n], rs[:, :n]), reads=[rr], writes=[rr])
    for c in range(8):
        if mod is not None:
            tmp = K.ntmp[c % 2]
            tr = f"ntmp{c % 2}"
            P.add("dve", lambda e, c=c, tmp=tmp: e.scalar_tensor_tensor(tmp[:, :n], src_fn(c), mod[:, sc0 + c:sc0 + c + 1], rs[:, :n],
                                                                        ALU.mult, ALU.mult),
                  reads=[src_res(c), rr, modres], writes=[tr])
            P.add("act", lambda e, c=c, tmp=tmp: e.activation(dst_fn(c), tmp[:, :n], AF.Identity, bias=mod[:, sh0 + c:sh0 + c + 1], scale=1.0),
                  reads=[tr, modres], writes=[dst_res_fn(c)])
        else:
            P.add("dve", lambda e, c=c: e.scalar_tensor_tensor(dst_fn(c), src_fn(c), K.fscale[:, c:c + 1], rs[:, :n],
                                                               ALU.mult, ALU.mult),
                  reads=[src_res(c), rr, "fscale"], writes=[dst_res_fn(c)])


def load_x(K, src_dram, tok0, dres=None):
    P = K.P
    v = src_dram.rearrange("(c p) t -> p c t", p=128)
    for c in range(8):
        P.add("sp", lambda e, c=c: e.dma_start(out=K.x[:, c, :], in_=v[:, c, tok0:tok0 + T]),
              reads=([f"{dres}{c}"] if dres else []), writes=[f"x{c}_{tb}" for tb in range(4)], key=f"x{c}")


def xres(c, tb):
    return f"x{c}_{tb}"


def proj_residual(K, w_dram, inT, in_res, mod, modres, g0):
    P, A = K.P, K.A
    m0 = A.mark()
    wo = A.alloc([8, 1024], BF16)
    wv = w_dram.rearrange("(kc p) n -> p kc n", p=128)
    for hf in range(2):
        P.add("pool", lambda e, hf=hf: e.dma_start(out=wo[:, hf * 4:(hf + 1) * 4, :], in_=wv[:, hf * 4:(hf + 1) * 4, :]),
              writes=[f"wo{hf}"], key=f"wo{hf}")
    k = 0
    for tb in range(4):
        for oc in range(8):
            b = 4 + (k % 2)
            k += 1
            for kc in range(8):
                mm(K, bank(K, b), wo[:, kc, oc * 128:(oc + 1) * 128], inT[:, kc, tb * 512:(tb + 1) * 512],
                   kc == 0, kc == 7, [f"wo{kc // 4}", in_res(kc)], [f"ps{b}"])
            P.add("dve", lambda e, b=b, oc=oc, tb=tb: e.scalar_tensor_tensor(
                K.x[:, oc, tb * 512:(tb + 1) * 512], bank(K, b), mod[:, g0 + oc:g0 + oc + 1],
                K.x[:, oc, tb * 512:(tb + 1) * 512], ALU.mult, ALU.add),
                reads=[f"ps{b}", xres(oc, tb), modres], writes=[xres(oc, tb)])
    P.barrier()
    A.reset(m0)


def swiglu_stream(K, hT, hres, groups, mod, modres, g0, comb_fn=None):
    P, A = K.P, K.A
    NF = max(g["nf"] for g in groups)
    Wg = [A.alloc([8, NF * 128], BF16) for _ in range(2)]
    Wu = [A.alloc([8, NF * 128], BF16) for _ in range(2)]
    Wd = [A.alloc([NF, 1024], BF16) for _ in range(2)]
    act = [A.alloc([NF, 512], BF16) for _ in range(2)]
    t1 = [A.alloc([512], BF16) for _ in range(2)]
    t2 = [A.alloc([512], BF16) for _ in range(2)]
    steps = [(gi, tb) for gi in range(len(groups)) for tb in range(4)]
    loaded = set()
    ctr = dict(gu=0, y=0)

    def ensure_loaded(gi):
        if gi in loaded or gi >= len(groups):
            return
        loaded.add(gi)
        par = gi % 2
        groups[gi]["load"](K, Wg[par], Wu[par], Wd[par], par)

    def emit_gu(si):
        gi, tb = steps[si]
        g = groups[gi]
        par = gi % 2
        assert gi in loaded
        ab = act[si % 2]
        ar = f"act{si % 2}"
        comb = comb_fn(g, tb) if comb_fn is not None else None
        for fi in range(g["nf"]):
            k = ctr["gu"]
            ctr["gu"] += 1
            bg = 0 + (k % 2)
            bu = 2 + (k % 2)
            for kc in range(8):
                mm(K, bank(K, bg), Wg[par][:, kc, fi * 128:(fi + 1) * 128], hT[:, kc, tb * 512:(tb + 1) * 512],
                   kc == 0, kc == 7, [f"Wg{par}", hres(kc)], [f"ps{bg}"])
            for kc in range(8):
                mm(K, bank(K, bu), Wu[par][:, kc, fi * 128:(fi + 1) * 128], hT[:, kc, tb * 512:(tb + 1) * 512],
                   kc == 0, kc == 7, [f"Wu{par}", hres(kc)], [f"ps{bu}"])
            tt = t1[k % 2]
            tr = f"t1_{k % 2}"
            P.add("act", lambda e, tt=tt, bg=bg: e.activation(tt, bank(K, bg), AF.Silu), reads=[f"ps{bg}"], writes=[tr])
            src, sr = tt, tr
            if comb is not None:
                cap, cres = comb
                t2b = t2[k % 2]
                t2r = f"t2_{k % 2}"
                P.add("pool", lambda e, t2b=t2b, tt=tt, cap=cap: e.tensor_tensor(t2b, tt, cap, ALU.mult),
                      reads=[tr, cres], writes=[t2r])
                src, sr = t2b, t2r
            P.add("dve", lambda e, ab=ab, fi=fi, src=src, bu=bu: e.tensor_tensor(ab[:, fi, :], src, bank(K, bu), ALU.mult),
                  reads=[sr, f"ps{bu}"], writes=[ar])

    def emit_down(si):
        gi, tb = steps[si]
        g = groups[gi]
        par = gi % 2
        ab = act[si % 2]
        ar = f"act{si % 2}"
        nf = g["nf"]
        for oc in range(8):
            k = ctr["y"]
            ctr["y"] += 1
            b = 4 + (k % 4)
            for fi in range(nf):
                mm(K, bank(K, b), Wd[par][:, fi, oc * 128:(oc + 1) * 128], ab[:, fi, :], fi == 0, fi == nf - 1,
                   [f"Wd{par}", ar], [f"ps{b}"])
            P.add("dve", lambda e, b=b, oc=oc, tb=tb: e.scalar_tensor_tensor(
                K.x[:, oc, tb * 512:(tb + 1) * 512], bank(K, b), mod[:, g0 + oc:g0 + oc + 1],
                K.x[:, oc, tb * 512:(tb + 1) * 512], ALU.mult, ALU.add),
                reads=[f"ps{b}", xres(oc, tb), modres], writes=[xres(oc, tb)])

    ensure_loaded(0)
    ensure_loaded(1)
    emit_gu(0)
    for si in range(len(steps)):
        if si + 1 < len(steps):
            emit_gu(si + 1)
        emit_down(si)
        if steps[si][1] == 3:
            ensure_loaded(steps[si][0] + 2)


def phase_A(K):
    P, A, din = K.P, K.A, K.din
    setup_consts(K)
    adaln(K, [0, 1])
    mod0, mod1 = K.mod
    mA = A.mark()
    hT = A.alloc([8, TE], BF16)
    OT = A.alloc([8, T], BF16)
    m1 = A.mark()
    alloc_norm_scratch(K)
    xs = [A.alloc([8, 512], F32), A.alloc([8, 512], F32)]
    xv = din["xT"].rearrange("(c p) t -> p c t", p=128)

    def hres(c, lo, hi):
        return [f"h{c}_{tb}" for tb in range(lo // 512, (hi - 1) // 512 + 1)]

    for tb in range(5):
        buf = xs[tb % 2]
        P.add("sp", lambda e, buf=buf, tb=tb: e.dma_start(out=buf, in_=xv[:, :, tb * 512:(tb + 1) * 512]),
              writes=[f"xs{tb % 2}"], key=f"xs{tb % 2}")
        modulate_block(K, lambda c, buf=buf: buf[:, c, :], lambda c, tb=tb: f"xs{tb % 2}", 512, mod0, "mod0", 8, 0,
                       lambda c, tb=tb: hT[:, c, tb * 512:(tb + 1) * 512], lambda c, tb=tb: f"h{c}_{tb}", 6)
    P.barrier()
    A.reset(m1)
    wqkv = [A.alloc([3, 8, 128], BF16) for _ in range(2)]
    qT = [A.alloc([T], BF16) for _ in range(2)]
    kT = [A.alloc([TE], BF16) for _ in range(2)]
    Vp = [A.alloc([20, 2, 128], BF16) for _ in range(2)]
    biasb = [A.alloc([NA_NBLK * 64], F32) for _ in range(2)]
    stmp = [A.alloc([384], F32) for _ in range(3)]
    pT = [A.alloc([384], BF16) for _ in range(3)]
    rden = [A.alloc([512], F32) for _ in range(2)]
    for par in range(2):
        P.add("pool", lambda e, par=par: e.memset(Vp[par], 0.0), writes=[f"Vp{par}"])
    wv = din["na_w_qkv"].rearrange("(kc p) n -> p kc n", p=128)
    nm = ["wq", "wk", "wv"]
    PB = 7
    for p in range(8):
        par = p % 2
        for s in range(3):
            P.add("pool", lambda e, par=par, s=s, p=p: e.dma_start(out=wqkv[par][:, s], in_=wv[:, :, s * 1024 + p * 128:s * 1024 + (p + 1) * 128]),
                  writes=[f"{nm[s]}{par}"], key=f"{nm[s]}{par}")
        for h2 in range(2):
            h = 2 * p + h2
            P.add("sp", lambda e, h2=h2, h=h: e.dma_start(out=biasb[h2], in_=din["na_bias"][h]), writes=[f"bias{h2}"], key=f"bias{h2}")
        for tb in range(4):
            lo = 256 + tb * 512
            for kc in range(8):
                mm(K, bank(K, PB), wqkv[par][:, 0, kc, :], hT[:, kc, lo:lo + 512], kc == 0, kc == 7,
                   [f"wq{par}"] + hres(kc, lo, lo + 512), [f"ps{PB}"])
            P.add("act", lambda e, par=par, tb=tb: e.copy(qT[par][:, tb * 512:(tb + 1) * 512], bank(K, PB)),
                  reads=[f"ps{PB}"], writes=[f"qT{par}"])
        for tb in range(5):
            lo = tb * 512
            for kc in range(8):
                mm(K, bank(K, PB), wqkv[par][:, 1, kc, :], hT[:, kc, lo:lo + 512], kc == 0, kc == 7,
                   [f"wk{par}"] + hres(kc, lo, lo + 512), [f"ps{PB}"])
            P.add("act", lambda e, par=par, tb=tb: e.copy(kT[par][:, tb * 512:(tb + 1) * 512], bank(K, PB)),
                  reads=[f"ps{PB}"], writes=[f"kT{par}"])
        for g in range(5):
            for j in range(4):
                t = g * 4 + j
                for kc in range(8):
                    mm(K, bank(K, PB)[:, j * 128:(j + 1) * 128], hT[:, kc, t * 128:(t + 1) * 128], wqkv[par][:, 2, kc, :],
                       kc == 0, kc == 7, [f"wv{par}"] + hres(kc, t * 128, (t + 1) * 128), [f"ps{PB}"])
            pv = bank(K, PB).rearrange("p (t c) -> p t c", c=128)
            P.add("dve", lambda e, par=par, g=g, pv=pv: e.tensor_copy(Vp[par][:, g * 4:(g + 1) * 4, 0, 0:64], pv[:, :, 0:64]),
                  reads=[f"ps{PB}"], writes=[f"Vp{par}"])
            P.add("dve", lambda e, par=par, g=g, pv=pv: e.tensor_copy(Vp[par][:, g * 4:(g + 1) * 4, 1, 64:128], pv[:, :, 64:128]),
                  reads=[f"ps{PB}"], writes=[f"Vp{par}"])
        steps = [(lr, h2) for lr in range(32) for h2 in range(2)]

        def emit_S(si, par=par):
            lr, h2 = steps[si]
            sb = si % 3
            for j, t in enumerate(na_tiles(lr)):
                mm(K, bank(K, sb)[:, j * 64:(j + 1) * 64], kT[par][h2 * 64:(h2 + 1) * 64, t * 128:(t + 1) * 128],
                   qT[par][h2 * 64:(h2 + 1) * 64, lr * 64:(lr + 1) * 64], True, True, [f"kT{par}", f"qT{par}"], [f"ps{sb}"])

        def emit_soft(si):
            lr, h2 = steps[si]
            sb = si % 3
            nt = len(na_tiles(lr))
            bo = na_boff(lr)
            tmp = stmp[si % 3]
            pt = pT[si % 3]
            P.add("dve", lambda e: e.scalar_tensor_tensor(tmp[:, :nt * 64], bank(K, sb)[:, :nt * 64], 0.125,
                                                          biasb[h2][:, bo * 64:(bo + nt) * 64], ALU.mult, ALU.add),
                  reads=[f"ps{sb}", f"bias{h2}"], writes=[f"stmp{si % 3}"])
            P.add("act", lambda e: e.activation(pt[:, :nt * 64], tmp[:, :nt * 64], AF.Exp),
                  reads=[f"stmp{si % 3}"], writes=[f"pT{si % 3}"])

        def emit_PV(si, par=par, p=p):
            lr, h2 = steps[si]
            grp, slot = lr // 8, lr % 8
            ob = 3 + (grp % 2)
            db = 5 + (grp % 2)
            pt = pT[si % 3]
            tiles = na_tiles(lr)
            for j, t in enumerate(tiles):
                first = (h2 == 0 and j == 0)
                last = (h2 == 1 and j == len(tiles) - 1)
                mm(K, bank(K, ob)[:, slot * 64:(slot + 1) * 64], Vp[par][:, t, h2, :], pt[:, j * 64:(j + 1) * 64],
                   first, last, [f"Vp{par}", f"pT{si % 3}"], [f"ps{ob}"])
                mm(K, bank(K, db)[:, slot * 64:(slot + 1) * 64], K.onespad[:, h2, :], pt[:, j * 64:(j + 1) * 64],
                   first, last, ["consts", f"pT{si % 3}"], [f"ps{db}"])
            if slot == 7 and h2 == 1:
                rd = rden[0]
                P.add("dve", lambda e: e.reciprocal(rd, bank(K, db)), reads=[f"ps{db}"], writes=["rden"])
                P.add("dve", lambda e: e.tensor_tensor(OT[:, p, grp * 512:(grp + 1) * 512], bank(K, ob), rd, ALU.mult),
                      reads=[f"ps{ob}", "rden"], writes=[f"OT{p}"])

        emit_S(0)
        emit_S(1)
        for si in range(len(steps)):
            emit_soft(si)
            if si + 2 < len(steps):
                emit_S(si + 2)
            emit_PV(si)
    P.barrier()
    A.reset(mA)
    OT2 = A.alloc([8, TE], BF16)
    OT = A.alloc([8, T], BF16)
    K.x = A.alloc([8, T], F32)
    load_x(K, din["xT"], 256)
    proj_residual(K, din["na_w_o"], OT, lambda kc: f"OT{kc}", mod0, "mod0", 16)
    A.reset(mA)
    hT2 = A.alloc([8, T], BF16)
    skip = A.alloc([8, TE - T], BF16)
    skip2 = A.alloc([8, T], BF16)
    K.x = A.alloc([8, T], F32)
    alloc_norm_scratch(K)
    for tb in range(4):
        modulate_block(K, lambda c, tb=tb: K.x[:, c, tb * 512:(tb + 1) * 512], lambda c, tb=tb: xres(c, tb), 512, mod0, "mod0", 32, 24,
                       lambda c, tb=tb: hT2[:, c, tb * 512:(tb + 1) * 512], lambda c: f"h2_{c}", 6)
    gu = din["ffn_w_gu"].rearrange("(kc p) n -> p kc n", p=128)
    dn = din["ffn_w_down"].rearrange("(f p) n -> p f n", p=128)
    groups = []
    for gi in range(11):
        def load(K, Wg, Wu, Wd, par, gi=gi):
            K.P.add("pool", lambda e: e.dma_start(out=Wg[:, :, 0:256], in_=gu[:, :, gi * 256:(gi + 1) * 256]), writes=[f"Wg{par}"], key=f"Wg{par}")
            K.P.add("pool", lambda e: e.dma_start(out=Wu[:, :, 0:256], in_=gu[:, :, FFN + gi * 256:FFN + (gi + 1) * 256]), writes=[f"Wu{par}"], key=f"Wu{par}")
            K.P.add("pool", lambda e: e.dma_start(out=Wd[:, 0:2, :], in_=dn[:, gi * 2:(gi + 1) * 2, :]), writes=[f"Wd{par}"], key=f"Wd{par}")
        groups.append(dict(load=load, nf=2))
    swiglu_stream(K, hT2, lambda kc: f"h2_{kc}", groups, mod0, "mod0", 40)
    P.barrier()
    A.reset(mA)
    K.mA = mA
    hT3 = A.alloc([8, T], BF16)
    K.hT3 = hT3
    K.cq_region = A.alloc([2, T], BF16)
    K.OTmark = A.mark()
    K.OTreg = A.alloc([8, T], BF16)
    K.xmark = A.mark()
    K.x = A.alloc([8, T], F32)
    alloc_norm_scratch(K)
    for tb in range(4):
        modulate_block(K, lambda c, tb=tb: K.x[:, c, tb * 512:(tb + 1) * 512], lambda c, tb=tb: xres(c, tb), 512, mod1, "mod1", 8, 0,
                       lambda c, tb=tb: hT3[:, c, tb * 512:(tb + 1) * 512], lambda c: f"h3_{c}", 6)
    xo = din["x1"].rearrange("(c p) t -> p c t", p=128)
    for c in range(8):
        P.add("sp", lambda e, c=c: e.dma_start(out=xo[:, c, :], in_=K.x[:, c, :]), reads=[xres(c, tb) for tb in range(4)],
              writes=[f"x1d{c}"], key=f"x{c}")
    mla_latent(K, hT3, lambda kc: f"h3_{kc}")


def mla_latent(K, hT3, hres):
    P, A, din = K.P, K.A, K.din
    wd = A.alloc([8, 160], BF16)
    wsw = A.alloc([8, 32], BF16)
    kvn = A.alloc([1], F32)
    ropeC = A.alloc([T], F32)
    ropeS = A.alloc([T], F32)
    dkv = A.alloc([512], F32)
    sqb = A.alloc([512], F32)
    rsb = A.alloc([512], F32)
    lat = A.alloc([T], F32)
    krot = A.alloc([T], F32)
    ktmp = A.alloc([512], F32)
    wv = din["mla_w_down"].rearrange("(kc p) n -> p kc n", p=128)
    P.add("pool", lambda e: e.dma_start(out=wd, in_=wv[:, :, 256:416]), writes=["wd"], key="wd")
    P.add("sp", lambda e: e.dma_start(out=kvn, in_=din["kv_norm"]), writes=["kvn"], key="kvn")
    P.add("sp", lambda e: e.dma_start(out=ropeC, in_=din["ropeC"]), writes=["ropeC"], key="ropeC")
    P.add("sp", lambda e: e.dma_start(out=ropeS, in_=din["ropeS"]), writes=["ropeS"], key="ropeS")
    wr = wd[:, :, 128:160].rearrange("p k (i two) -> p k i two", two=2)
    ws = wsw.rearrange("p k (i two) -> p k i two", two=2)
    P.add("dve", lambda e: e.tensor_scalar_mul(ws[:, :, :, 0], wr[:, :, :, 1], -1.0), reads=["wd"], writes=["wsw"])
    P.add("dve", lambda e: e.tensor_copy(ws[:, :, :, 1], wr[:, :, :, 0]), reads=["wd"], writes=["wsw"])
    for tb in range(4):
        sl = slice(tb * 512, (tb + 1) * 512)
        for kc in range(8):
            mm(K, bank(K, 0), wd[:, kc, 0:128], hT3[:, kc, sl], kc == 0, kc == 7, ["wd", hres(kc)], ["ps0"])
        for kc in range(8):
            mm(K, bank(K, 1)[0:32, :], wd[:, kc, 128:160], hT3[:, kc, sl], kc == 0, kc == 7, ["wd", hres(kc)], ["ps1"])
        for kc in range(8):
            mm(K, bank(K, 2)[0:32, :], wsw[:, kc, :], hT3[:, kc, sl], kc == 0, kc == 7, ["wsw", hres(kc)], ["ps2"])
        P.add("act", lambda e: e.copy(dkv, bank(K, 0)), reads=["ps0"], writes=["dkv"])
        P.add("pool", lambda e: e.tensor_tensor(sqb, dkv, dkv, ALU.mult), reads=["dkv"], writes=["sqb"])
        mm(K, bank(K, 3), K.ones128, sqb, True, True, ["sqb", "consts"], ["ps3"])
        P.add("act", lambda e: e.activation(rsb, bank(K, 3), AF.Sqrt, bias=EPS, scale=1.0), reads=["ps3"], writes=["rsb"])
        P.add("dve", lambda e: e.reciprocal(rsb, rsb), reads=["rsb"], writes=["rsb"])
        P.add("dve", lambda e, sl=sl: e.scalar_tensor_tensor(lat[:, sl], dkv, kvn[:, 0:1], rsb, ALU.mult, ALU.mult),
              reads=["dkv", "rsb", "kvn"], writes=["lat"])
        P.add("dve", lambda e, sl=sl: e.tensor_tensor(krot[0:32, sl], bank(K, 1)[0:32, :], ropeC[0:32, sl], ALU.mult),
              reads=["ps1", "ropeC"], writes=["krot"])
        P.add("dve", lambda e, sl=sl: e.tensor_tensor(ktmp[0:32, :], bank(K, 2)[0:32, :], ropeS[0:32, sl], ALU.mult),
              reads=["ps2", "ropeS"], writes=["ktmp"])
        P.add("pool", lambda e, sl=sl: e.tensor_tensor(krot[0:32, sl], krot[0:32, sl], ktmp[0:32, :], ALU.add),
              reads=["krot", "ktmp"], writes=["krot"])
    P.add("sp", lambda e: e.dma_start(out=din["lat"][0:128, :], in_=lat), reads=["lat"], writes=["latd0"], key="lat")
    P.add("sp", lambda e: e.dma_start(out=din["lat"][128:160, :], in_=krot[0:32, :]), reads=["krot"], writes=["latd1"], key="krot")


def phase_B_prologue_unfused(K):
    P, A, din = K.P, K.A, K.din
    setup_consts(K)
    adaln(K, [1])
    mA = A.mark()
    K.mA = mA
    K.hT3 = A.alloc([8, T], BF16)
    K.cq_region = A.alloc([2, T], BF16)
    K.OTmark = A.mark()
    K.OTreg = A.alloc([8, T], BF16)
    K.xmark = A.mark()
    K.x = A.alloc([8, T], F32)
    load_x(K, din["x1"], 0)
    alloc_norm_scratch(K)
    for tb in range(4):
        modulate_block(K, lambda c, tb=tb: K.x[:, c, tb * 512:(tb + 1) * 512], lambda c, tb=tb: xres(c, tb), 512, K.mod[1], "mod1", 8, 0,
                       lambda c, tb=tb: K.hT3[:, c, tb * 512:(tb + 1) * 512], lambda c: f"h3_{c}", 6)
    P.barrier()


def phase_B(K):
    P, A, din = K.P, K.A, K.din
    mod1 = K.mod[1]
    mB = K.mA
    oT = K.OTreg
    cqT = K.cq_region
    hT3 = K.hT3
    m1 = K.xmark
    A.reset(m1)
    wdq = A.alloc([8, 256], BF16)
    qn = A.alloc([2], F32)
    dq = [A.alloc([512], F32), A.alloc([512], F32)]
    sqq = [A.alloc([512], F32), A.alloc([512], F32)]
    rsq = A.alloc([512], F32)
    wv = din["mla_w_down"].rearrange("(kc p) n -> p kc n", p=128)
    P.add("pool", lambda e: e.dma_start(out=wdq, in_=wv[:, :, 0:256]), writes=["wdq"], key="wdq")
    P.add("sp", lambda e: e.dma_start(out=qn, in_=din["q_norm"]), writes=["qn"], key="qn")
    for tb in range(4):
        sl = slice(tb * 512, (tb + 1) * 512)
        for j in range(2):
            for kc in range(8):
                mm(K, bank(K, j), wdq[:, kc, j * 128:(j + 1) * 128], hT3[:, kc, sl], kc == 0, kc == 7, ["wdq", f"h3_{kc}"], [f"ps{j}"])
            P.add("act", lambda e, j=j: e.copy(dq[j], bank(K, j)), reads=[f"ps{j}"], writes=[f"dq{j}"])
            P.add("pool", lambda e, j=j: e.tensor_tensor(sqq[j], dq[j], dq[j], ALU.mult), reads=[f"dq{j}"], writes=[f"sqq{j}"])
            mm(K, bank(K, 2), K.ones256, sqq[j], j == 0, j == 1, [f"sqq{j}", "consts"], ["ps2"])
        P.add("act", lambda e: e.activation(rsq, bank(K, 2), AF.Sqrt, bias=EPS, scale=1.0), reads=["ps2"], writes=["rsq"])
        P.add("dve", lambda e: e.reciprocal(rsq, rsq), reads=["rsq"], writes=["rsq"])
        for j in range(2):
            P.add("dve", lambda e, j=j, sl=sl: e.scalar_tensor_tensor(cqT[:, j, sl], dq[j], qn[:, j:j + 1], rsq, ALU.mult, ALU.mult),
                  reads=[f"dq{j}", "rsq", "qn"], writes=["cqT"])
    P.barrier()
    A.reset(m1)
    save_off = A.off
    A.off = K.mA
    ckvT = A.alloc([S], BF16)
    kh0 = A.alloc([S], BF16)
    A.off = save_off
    khT = [kh0, A.alloc([S], BF16)]
    Vh = [A.alloc([64, 128], BF16) for _ in range(2)]
    qhT = [A.alloc([T], BF16) for _ in range(2)]
    wuq = A.alloc([2, 1536], BF16)
    wuqsw = A.alloc([2, 16, 96], BF16)
    wukv = A.alloc([2048], BF16)
    ropeC = A.alloc([T], F32)
    ropeS = A.alloc([T], F32)
    pT = [A.alloc([1024], BF16) for _ in range(3)]
    rt1 = [A.alloc([512], F32) for _ in range(2)]
    rt2 = [A.alloc([512], F32) for _ in range(2)]
    rd = [A.alloc([512], F32) for _ in range(2)]
    otmp = [A.alloc([512], BF16) for _ in range(2)]
    la = din["latall"]
    bsel = A.alloc([2], F32)
    P.add("sp", lambda e: e.dma_start(out=bsel, in_=din["bsel"]), writes=["bsel"], key="bsel")
    CH = 512
    stA = [A.alloc([CH], F32) for _ in range(2)]
    stB = [A.alloc([CH], F32) for _ in range(2)]
    for par in range(2):
        P.add("pool", lambda e, par=par: e.memset(Vh[par][:, :, 64:128], 1.0), writes=[f"Vh1_{par}"])
    nch = T // CH
    it = 0
    for i in range(4):
        for j in range(nch):
            sa, sb_ = stA[it % 2], stB[it % 2]
            ra, rb = f"stA{it % 2}", f"stB{it % 2}"
            it += 1
            r0, r1 = i * 160, (4 + i) * 160
            cs = slice(j * CH, (j + 1) * CH)
            ks = slice(i * T + j * CH, i * T + (j + 1) * CH)
            P.add("sp", lambda e, sa=sa, r0=r0, cs=cs: e.dma_start(out=sa[0:128, :], in_=la[r0:r0 + 128, cs]), reads=["latall"], writes=[ra], key=ra)
            P.add("sp", lambda e, sb_=sb_, r1=r1, cs=cs: e.dma_start(out=sb_[0:128, :], in_=la[r1:r1 + 128, cs]), reads=["latall"], writes=[rb], key=rb)
            P.add("dve", lambda e, sa=sa: e.tensor_scalar_mul(sa, sa, bsel[:, 0:1]), reads=[ra, "bsel"], writes=[ra])
            P.add("dve", lambda e, sa=sa, sb_=sb_, ks=ks: e.scalar_tensor_tensor(ckvT[:, ks], sb_, bsel[:, 1:2], sa, ALU.mult, ALU.add),
                  reads=[ra, rb, "bsel"], writes=[f"ckv{i}"])
    for i in range(4):
        for j in range(nch):
            sa, sb_ = stA[it % 2], stB[it % 2]
            ra, rb = f"stA{it % 2}", f"stB{it % 2}"
            it += 1
            r0, r1 = i * 160 + 128, (4 + i) * 160 + 128
            cs = slice(j * CH, (j + 1) * CH)
            ks = slice(i * T + j * CH, i * T + (j + 1) * CH)
            P.add("sp", lambda e, sa=sa, r0=r0, cs=cs: e.dma_start(out=sa[64:96, :], in_=la[r0:r0 + 32, cs]), reads=["latall"], writes=[ra], key=ra)
            P.add("sp", lambda e, sb_=sb_, r1=r1, cs=cs: e.dma_start(out=sb_[64:96, :], in_=la[r1:r1 + 32, cs]), reads=["latall"], writes=[rb], key=rb)
            P.add("pool", lambda e, sa=sa: e.tensor_scalar_mul(sa[64:96, :], sa[64:96, :], bsel[64:96, 0:1]), reads=[ra, "bsel"], writes=[ra])
            for par in range(2):
                P.add("dve", lambda e, sa=sa, sb_=sb_, ks=ks, par=par: e.scalar_tensor_tensor(
                    khT[par][64:96, ks], sb_[64:96, :], bsel[64:96, 1:2], sa[64:96, :], ALU.mult, ALU.add),
                    reads=[ra, rb, "bsel"], writes=[f"khr{par}"])
    uq = din["mla_w_uq"].rearrange("(kc p) n -> p kc n", p=128)
    P.add("pool", lambda e: e.dma_start(out=wuq, in_=uq), writes=["wuq"], key="wuq")
    P.add("pool", lambda e: e.dma_start(out=wukv, in_=din["mla_w_ukv"]), writes=["wukv"], key="wukv")
    P.add("sp", lambda e: e.dma_start(out=ropeC, in_=din["ropeC"]), writes=["ropeC"], key="ropeC")
    P.add("sp", lambda e: e.dma_start(out=ropeS, in_=din["ropeS"]), writes=["ropeS"], key="ropeS")
    P.add("pool", lambda e: e.memset(wuqsw, 0.0), writes=["wuqsw"])
    wq4 = wuq.rearrange("p k (h f) -> p k h f", h=16)
    src = wq4[:, :, :, 64:96].rearrange("p k h (i two) -> p k h i two", two=2)
    dst = wuqsw[:, :, :, 64:96].rearrange("p k h (i two) -> p k h i two", two=2)
    for kc in range(2):
        P.add("dve", lambda e, kc=kc: e.tensor_scalar_mul(dst[:, kc, :, :, 0], src[:, kc, :, :, 1], -1.0), reads=["wuq", "wuqsw"], writes=["wuqsw"])
        P.add("dve", lambda e, kc=kc: e.tensor_copy(dst[:, kc, :, :, 1], src[:, kc, :, :, 0]), reads=["wuq", "wuqsw"], writes=["wuqsw"])
    SCALE = float(96 ** -0.5)
    PBK = [6, 7]
    pctr = [0]

    def pbank():
        b = PBK[pctr[0] % 2]
        pctr[0] += 1
        return b

    def emit_proj(h):
        par = h % 2
        for kb in range(16):
            b = pbank()
            mm(K, bank(K, b)[0:64, :], wukv[:, h * 128:h * 128 + 64], ckvT[:, kb * 512:(kb + 1) * 512], True, True,
               ["wukv", f"ckv{kb // 4}"], [f"ps{b}"])
            P.add("dve", lambda e, b=b, kb=kb, par=par: e.tensor_copy(khT[par][0:64, kb * 512:(kb + 1) * 512], bank(K, b)[0:64, :]),
                  reads=[f"ps{b}"], writes=[f"kh{par}_{kb}"])
        for g in range(8):
            b = pbank()
            for j in range(8):
                kt = g * 8 + j
                mm(K, bank(K, b)[:, j * 64:(j + 1) * 64], ckvT[:, kt * 128:(kt + 1) * 128], wukv[:, h * 128 + 64:h * 128 + 128], True, True,
                   ["wukv", f"ckv{kt // 16}"], [f"ps{b}"])
            pv = bank(K, b).rearrange("p (t c) -> p t c", c=64)
            P.add("dve", lambda e, g=g, par=par, pv=pv: e.tensor_copy(Vh[par][:, g * 8:(g + 1) * 8, 0:64], pv),
                  reads=[f"ps{b}"], writes=[f"Vh{par}_{g}"])
        for tb in range(4):
            sl = slice(tb * 512, (tb + 1) * 512)
            b1 = pbank()
            b2 = pbank()
            for kc in range(2):
                mm(K, bank(K, b1)[0:96, :], wuq[:, kc, h * 96:(h + 1) * 96], cqT[:, kc, sl], kc == 0, kc == 1, ["wuq", "cqT"], [f"ps{b1}"])
            for kc in range(2):
                mm(K, bank(K, b2)[0:96, :], wuqsw[:, kc, h, :], cqT[:, kc, sl], kc == 0, kc == 1, ["wuqsw", "cqT"], [f"ps{b2}"])
            i = tb % 2
            P.add("dve", lambda e, b1=b1, sl=sl, par=par: e.tensor_copy(qhT[par][0:64, sl], bank(K, b1)[0:64, :]),
                  reads=[f"ps{b1}"], writes=[f"qh{par}"])
            P.add("dve", lambda e, b1=b1, sl=sl, i=i: e.tensor_tensor(rt1[i][64:96, :], bank(K, b1)[64:96, :], ropeC[64:96, sl], ALU.mult),
                  reads=[f"ps{b1}", "ropeC"], writes=[f"rt1_{i}"])
            P.add("dve", lambda e, b2=b2, sl=sl, i=i: e.tensor_tensor(rt2[i][64:96, :], bank(K, b2)[64:96, :], ropeS[64:96, sl], ALU.mult),
                  reads=[f"ps{b2}", "ropeS"], writes=[f"rt2_{i}"])
            P.add("pool", lambda e, sl=sl, i=i, par=par: e.tensor_tensor(qhT[par][64:96, sl], rt1[i][64:96, :], rt2[i][64:96, :], ALU.add),
                  reads=[f"rt1_{i}", f"rt2_{i}"], writes=[f"qh{par}"])

    sctr = [0]

    def emit_S(h, qb, k2):
        par = h % 2
        si = sctr[0]
        sctr[0] += 1
        b0 = (si % 2) * 2
        for j in range(2):
            kt = k2 * 2 + j
            mm(K, bank(K, b0 + j), khT[par][0:96, kt * 128:(kt + 1) * 128], qhT[par][0:96, qb * 512:(qb + 1) * 512], True, True,
               [f"kh{par}_{kt // 4}", f"khr{par}", f"qh{par}"], [f"ps{b0 + j}"])
        return si

    def emit_exp_pv(h, qb, k2, si):
        par = h % 2
        b0 = (si % 2) * 2
        pt = pT[si % 3]
        ob = 4 + ((h * 4 + qb) % 2)
        P.add("act", lambda e: e.activation(pt, K.ps[:, b0 * 512:(b0 + 2) * 512], AF.Exp, scale=SCALE),
              reads=[f"ps{b0}", f"ps{b0 + 1}"], writes=[f"pT{si % 3}"])
        for j in range(2):
            kt = k2 * 2 + j
            mm(K, bank(K, ob), Vh[par][:, kt, :], pt[:, j * 512:(j + 1) * 512], kt == 0, kt == 63,
               [f"Vh{par}_{kt // 8}", f"Vh1_{par}", f"pT{si % 3}"], [f"ps{ob}"])
        if k2 == 31:
            i = (h * 4 + qb) % 2
            c = h // 2
            sl = slice(qb * 512, (qb + 1) * 512)
            P.add("dve", lambda e: e.reciprocal(rd[i][64:128, :], bank(K, ob)[64:128, :]), reads=[f"ps{ob}"], writes=[f"rd{i}"])
            P.add("dve", lambda e: e.tensor_copy(rd[i][0:64, :], rd[i][64:128, :]), reads=[f"rd{i}"], writes=[f"rd{i}"])
            if par == 0:
                P.add("dve", lambda e: e.tensor_tensor(oT[0:64, c, sl], bank(K, ob)[0:64, :], rd[i][0:64, :], ALU.mult),
                      reads=[f"ps{ob}", f"rd{i}"], writes=[f"oT{c}"])
            else:
                P.add("dve", lambda e: e.tensor_tensor(otmp[i][0:64, :], bank(K, ob)[0:64, :], rd[i][0:64, :], ALU.mult),
                      reads=[f"ps{ob}", f"rd{i}"], writes=[f"otmp{i}"])
                P.add("dve", lambda e: e.tensor_copy(oT[64:128, c, sl], otmp[i][0:64, :]), reads=[f"otmp{i}"], writes=[f"oT{c}"])

    emit_proj(0)
    seq = [(h, qb, k2) for h in range(16) for qb in range(4) for k2 in range(32)]
    pend = emit_S(*seq[0])
    for idx, (h, qb, k2) in enumerate(seq):
        nxt = None
        if idx + 1 < len(seq):
            if seq[idx + 1][1:] == (0, 0) and False:
                pass
            nxt = emit_S(*seq[idx + 1])
        emit_exp_pv(h, qb, k2, pend)
        pend = nxt
        if qb == 1 and k2 == 31 and h + 1 < 16:
            emit_proj(h + 1)
    P.barrier()
    A.reset(m1)
    K.x = A.alloc([8, T], F32)
    load_x(K, din["x1"], 0, dres="x1d")
    proj_residual(K, din["mla_w_o"], oT, lambda kc: f"oT{kc}", mod1, "mod1", 16)
    A.reset(mB)
    hT4 = A.alloc([8, T], BF16)
    skip = A.alloc([2, T], BF16)
    skip2 = A.alloc([8, T], BF16)
    assert A.mark() == K.xmark
    K.x = A.alloc([8, T], F32)
    mX = A.mark()
    alloc_norm_scratch(K)
    for tb in range(4):
        modulate_block(K, lambda c, tb=tb: K.x[:, c, tb * 512:(tb + 1) * 512], lambda c, tb=tb: xres(c, tb), 512, mod1, "mod1", 32, 24,
                       lambda c, tb=tb: hT4[:, c, tb * 512:(tb + 1) * 512], lambda c: f"h4_{c}", 6)
    save_off = A.off
    A.off = K.OTmark
    wr = A.alloc([8, 8], BF16)
    lg = A.alloc([16, 8], F32)
    eq = A.alloc([16, 8], F32)
    l2 = A.alloc([16, 8], F32)
    ex = A.alloc([16, 8], F32)
    comb = A.alloc([16, 8], F32)
    mx1 = A.alloc([16], F32)
    mx2 = A.alloc([16], F32)
    ssum = A.alloc([16], F32)
    onesF = A.alloc([128], F32)
    cm = [A.alloc([128], F32) for _ in range(2)]
    cbc = [A.alloc([T], F32) for _ in range(2)]
    assert A.off <= K.xmark
    A.off = save_off
    P.add("dve", lambda e: e.memset(onesF, 1.0), writes=["onesF"])
    P.add("pool", lambda e: e.dma_start(out=wr, in_=din["moe_w_router"].rearrange("(kc p) n -> p kc n", p=128)), writes=["wr"], key="wr")
    for tt in range(16):
        for kc in range(8):
            mm(K, bank(K, 6)[:, tt * 8:(tt + 1) * 8], hT4[:, kc, tt * 128:(tt + 1) * 128], wr[:, kc, :], kc == 0, kc == 7,
               ["wr", f"h4_{kc}"], ["ps6"])
    lgf = lg.rearrange("p a b -> p (a b)")
    P.add("dve", lambda e: e.tensor_copy(lgf, bank(K, 6)[:, 0:128]), reads=["ps6"], writes=["lg"])
    X = mybir.AxisListType.X

    def bc(v):
        return v.unsqueeze(2).to_broadcast([128, 16, 8])

    P.add("dve", lambda e: e.tensor_reduce(mx1, lg, X, ALU.max), reads=["lg"], writes=["mx1"])
    P.add("dve", lambda e: e.tensor_tensor(eq, lg, bc(mx1), ALU.is_equal), reads=["lg", "mx1"], writes=["eq"])
    P.add("dve", lambda e: e.scalar_tensor_tensor(l2, eq, -1e30, lg, ALU.mult, ALU.add), reads=["eq", "lg"], writes=["l2"])
    P.add("dve", lambda e: e.tensor_reduce(mx2, l2, X, ALU.max), reads=["l2"], writes=["mx2"])
    P.add("dve", lambda e: e.tensor_tensor(eq, lg, bc(mx2), ALU.is_ge), reads=["lg", "mx2", "eq"], writes=["eq"])
    P.add("dve", lambda e: e.tensor_tensor(l2, lg, bc(mx1), ALU.subtract), reads=["lg", "mx1", "l2"], writes=["l2"])
    P.add("act", lambda e: e.activation(ex, l2, AF.Exp), reads=["l2"], writes=["ex"])
    P.add("dve", lambda e: e.tensor_tensor(ex, ex, eq, ALU.mult), reads=["ex", "eq"], writes=["ex"])
    P.add("dve", lambda e: e.tensor_reduce(ssum, ex, X, ALU.add), reads=["ex"], writes=["ssum"])
    P.add("dve", lambda e: e.reciprocal(ssum, ssum), reads=["ssum"], writes=["ssum"])
    P.add("dve", lambda e: e.tensor_tensor(comb, ex, bc(ssum), ALU.mult), reads=["ex", "ssum"], writes=["comb"])

    def make_cbc(ex_):
        buf = cbc[ex_ % 2]
        res = f"cbc{ex_ % 2}"
        for tt in range(16):
            cmb = cm[tt % 2]
            P.add("dve", lambda e, cmb=cmb, tt=tt: e.tensor_scalar_mul(cmb, onesF, comb[:, tt, ex_:ex_ + 1]),
                  reads=["comb", "onesF"], writes=[f"cm{tt % 2}"])
            mm(K, bank(K, 7)[:, (tt % 4) * 128:(tt % 4 + 1) * 128], cmb, K.ident, True, True, [f"cm{tt % 2}", "ident"], ["ps7"])
            if tt % 4 == 3:
                t0 = tt - 3
                P.add("act", lambda e, t0=t0: e.copy(buf[:, t0 * 128:(t0 + 4) * 128], bank(K, 7)), reads=["ps7"], writes=[res])

    gu = din["moe_w_gu"]
    dn = din["moe_w_down"]
    groups = []
    for ex_ in range(NEXP):
        guv = gu[ex_].rearrange("(kc p) n -> p kc n", p=128)
        dnv = dn[ex_].rearrange("(f p) n -> p f n", p=128)
        for gj in range(7):
            def load(K, Wg, Wu, Wd, par, guv=guv, dnv=dnv, gj=gj, ex_=ex_):
                if gj == 0:
                    make_cbc(ex_)
                K.P.add("pool", lambda e: e.dma_start(out=Wg[:, :, 0:256], in_=guv[:, :, gj * 256:(gj + 1) * 256]), writes=[f"Wg{par}"], key=f"Wg{par}")
                K.P.add("pool", lambda e: e.dma_start(out=Wu[:, :, 0:256], in_=guv[:, :, EDIM + gj * 256:EDIM + (gj + 1) * 256]), writes=[f"Wu{par}"], key=f"Wu{par}")
                K.P.add("pool", lambda e: e.dma_start(out=Wd[:, 0:2, :], in_=dnv[:, gj * 2:(gj + 1) * 2, :]), writes=[f"Wd{par}"], key=f"Wd{par}")
            groups.append(dict(load=load, nf=2, ex=ex_))

    def comb_fn(g, tb):
        e_ = g["ex"]
        return cbc[e_ % 2][:, tb * 512:(tb + 1) * 512], f"cbc{e_ % 2}"

    swiglu_stream(K, hT4, lambda kc: f"h4_{kc}", groups, mod1, "mod1", 40, comb_fn=comb_fn)
    P.barrier()
    A.reset(mX)
    alloc_norm_scratch(K)
    K.fscale = A.alloc([8], F32)
    P.add("sp", lambda e: e.dma_start(out=K.fscale, in_=din["final_norm"]), writes=["fscale"], key="fscale")
    ost = [A.alloc([8, 512], F32) for _ in range(2)]
    ov = din["out"].rearrange("(c p) t -> p c t", p=128)
    for tb in range(4):
        ob_ = ost[tb % 2]
        modulate_block(K, lambda c, tb=tb: K.x[:, c, tb * 512:(tb + 1) * 512], lambda c, tb=tb: xres(c, tb), 512, None, None, 0, 0,
                       lambda c, ob_=ob_: ob_[:, c, :], lambda c, tb=tb: f"ost{tb % 2}", 6)
        P.add("sp", lambda e, ob_=ob_, tb=tb: e.dma_start(out=ov[:, :, tb * 512:(tb + 1) * 512], in_=ob_), reads=[f"ost{tb % 2}"], key=f"ost{tb % 2}")


FUSED = True


def build_A():
    nc = bass.Bass("TRN2", target_bir_lowering=False)
    K = Ctx()
    K.nc = nc
    K.P = Prog(nc)
    K.A = Arena(nc, ARENA_WORDS)
    K.ps = nc.alloc_psum_tensor("ps", [128, 4096], F32)

    def inp(name, shape):
        return nc.dram_tensor(name, list(shape), F32, kind="ExternalInput").ap()

    def outp(name, shape):
        return nc.dram_tensor(name, list(shape), F32, kind="ExternalOutput").ap()

    K.din = dict(
        xT=inp("xT", (D, TE)), cT=inp("cT", (128, 8)), w_ada=inp("w_ada", (2, D, 6 * D)), b_ada=inp("b_ada", (2, 128, 48)),
        ident=inp("ident", (128, 128)), na_w_qkv=inp("na_w_qkv", (D, 3 * D)), na_bias=inp("na_bias", (16, 128, NA_NBLK * 64)),
        na_w_o=inp("na_w_o", (D, D)), ffn_w_gu=inp("ffn_w_gu", (D, 2 * FFN)), ffn_w_down=inp("ffn_w_down", (FFN, D)),
        mla_w_down=inp("mla_w_down", (D, 416)), kv_norm=inp("kv_norm", (128, 1)),
        ropeC=inp("ropeC", (128, T)), ropeS=inp("ropeS", (128, T)),
        x1=outp("x1", (D, T)), lat=outp("lat", (160, T)),
    )
    phase_A(K)
    K.P.emit()
    print("A stats", K.P.stats, "arena peak words", K.A.peak)
    return nc


def build_B():
    nc = bass.Bass("TRN2", target_bir_lowering=False)
    K = Ctx()
    K.nc = nc
    K.P = Prog(nc)
    K.A = Arena(nc, ARENA_WORDS)
    K.ps = nc.alloc_psum_tensor("ps", [128, 4096], F32)

    def inp(name, shape):
        return nc.dram_tensor(name, list(shape), F32, kind="ExternalInput").ap()

    def outp(name, shape):
        return nc.dram_tensor(name, list(shape), F32, kind="ExternalOutput").ap()

    K.din = dict(
        x1=inp("x1", (D, T)), latall=inp("latall", (NC * 160, T)), cT=inp("cT", (128, 8)), w_ada=inp("w_ada", (2, D, 6 * D)),
        b_ada=inp("b_ada", (2, 128, 48)), ident=inp("ident", (128, 128)), mla_w_down=inp("mla_w_down", (D, 416)),
        ropeC=inp("ropeC", (128, T)), ropeS=inp("ropeS", (128, T)), bsel=inp("bsel", (128, 2)),
        q_norm=inp("q_norm", (128, 2)), mla_w_uq=inp("mla_w_uq", (256, 1536)), mla_w_ukv=inp("mla_w_ukv", (128, 2048)),
        mla_w_o=inp("mla_w_o", (D, D)), moe_w_router=inp("moe_w_router", (D, 8)), moe_w_gu=inp("moe_w_gu", (NEXP, D, 2 * EDIM)),
        moe_w_down=inp("moe_w_down", (NEXP, EDIM, D)), final_norm=inp("final_norm", (128, 8)),
        out=outp("out", (D, T)),
    )
    phase_B_prologue_unfused(K)
    phase_B(K)
    K.P.emit()
    print("B stats", K.P.stats, "arena peak words", K.A.peak)
    return nc


def build_AB():
    nc = bass.Bass("TRN2", target_bir_lowering=False)
    K = Ctx()
    K.nc = nc
    K.P = Prog(nc)
    K.A = Arena(nc, ARENA_WORDS)
    K.ps = nc.alloc_psum_tensor("ps", [128, 4096], F32)

    def inp(name, shape):
        return nc.dram_tensor(name, list(shape), F32, kind="ExternalInput").ap()

    def outp(name, shape):
        return nc.dram_tensor(name, list(shape), F32, kind="ExternalOutput").ap()

    x1d = nc.dram_tensor("x1d", [D, T], F32)
    latd = nc.dram_tensor("latd", [160, T], F32)
    latall = nc.dram_tensor("latall", [NC * 160, T], F32)
    K.din = dict(
        xT=inp("xT", (D, TE)), cT=inp("cT", (128, 8)), w_ada=inp("w_ada", (2, D, 6 * D)), b_ada=inp("b_ada", (2, 128, 48)),
        ident=inp("ident", (128, 128)), na_w_qkv=inp("na_w_qkv", (D, 3 * D)), na_bias=inp("na_bias", (16, 128, NA_NBLK * 64)),
        na_w_o=inp("na_w_o", (D, D)), ffn_w_gu=inp("ffn_w_gu", (D, 2 * FFN)), ffn_w_down=inp("ffn_w_down", (FFN, D)),
        mla_w_down=inp("mla_w_down", (D, 416)), kv_norm=inp("kv_norm", (128, 1)),
        ropeC=inp("ropeC", (128, T)), ropeS=inp("ropeS", (128, T)), bsel=inp("bsel", (128, 2)),
        q_norm=inp("q_norm", (128, 2)), mla_w_uq=inp("mla_w_uq", (256, 1536)), mla_w_ukv=inp("mla_w_ukv", (128, 2048)),
        mla_w_o=inp("mla_w_o", (D, D)), moe_w_router=inp("moe_w_router", (D, 8)), moe_w_gu=inp("moe_w_gu", (NEXP, D, 2 * EDIM)),
        moe_w_down=inp("moe_w_down", (NEXP, EDIM, D)), final_norm=inp("final_norm", (128, 8)),
        x1=x1d.ap(), lat=latd.ap(), latall=latall.ap(),
        out=outp("out", (D, T)),
    )
    phase_A(K)
    K.P.barrier()
    K.P.add("pool", lambda e: e.collective_compute("AllGather", ALU.bypass, replica_groups=[list(range(NC))],
                                                   ins=[latd.ap().opt()], outs=[latall.ap().opt()]),
            reads=["latd0", "latd1"], writes=["latall"], key="cc", inc=1)
    phase_B(K)
    K.P.emit()
    print("AB stats", K.P.stats, "arena peak words", K.A.peak)
    return nc


def host_common(inputs):
    x = np.asarray(inputs["x"], np.float32)
    c = np.asarray(inputs["c"], np.float32)
    per_core = []
    for core in range(NC):
        b, q = core // 4, core % 4
        r0 = 32 * q - 4
        xe = np.zeros((TE, D), np.float32)
        lo, hi = max(r0, 0), min(r0 + 40, 128)
        xe[(lo - r0) * 64:(hi - r0) * 64] = x[b, lo * 64:hi * 64]
        Cf, Sf = host_rope_tables(q)
        per_core.append(dict(
            xT=np.ascontiguousarray(xe.T),
            cT=np.ascontiguousarray(c[b].reshape(8, 128).T),
            ropeC=Cf, ropeS=Sf,
        ))
    shared = dict(
        w_ada=np.asarray(inputs["w_ada"], np.float32),
        b_ada=np.ascontiguousarray(np.asarray(inputs["b_ada"], np.float32).reshape(2, 48, 128).transpose(0, 2, 1)),
        ident=np.eye(128, dtype=np.float32),
    )
    return per_core, shared


_CACHE = {}


def kernel(**inputs):
    per_core, shared = host_common(inputs)
    rpb = np.asarray(inputs["na_rpb"], np.float32)[0]
    bias_q = [host_na_bias(rpb, q) for q in range(4)]
    if FUSED and "AB" not in _CACHE:
        _CACHE["AB"] = build_AB()
    if not FUSED and "A" not in _CACHE:
        _CACHE["A"] = build_A()
        _CACHE["B"] = build_B()
    f32 = lambda k: np.asarray(inputs[k], np.float32)[0]
    common = dict(
        na_w_qkv=f32("na_w_qkv"), na_w_o=f32("na_w_o"), ffn_w_gu=f32("ffn_w_gu"), ffn_w_down=f32("ffn_w_down"),
        mla_w_down=f32("mla_w_down"), kv_norm=f32("mla_kv_norm").reshape(128, 1),
        q_norm=np.ascontiguousarray(f32("mla_q_norm").reshape(2, 128).T),
        mla_w_uq=f32("mla_w_uq"), mla_w_ukv=f32("mla_w_ukv"), mla_w_o=f32("mla_w_o"), moe_w_router=f32("moe_w_router"),
        moe_w_gu=f32("moe_w_gu"), moe_w_down=f32("moe_w_down"),
        final_norm=np.ascontiguousarray(np.asarray(inputs["final_norm"], np.float32).reshape(8, 128).T),
    )
    in_maps = []
    for core in range(NC):
        b = core // 4
        m = dict(per_core[core])
        m.update(shared)
        m.update(common)
        m["na_bias"] = bias_q[core % 4]
        bs = np.zeros((128, 2), np.float32)
        bs[:, b] = 1.0
        m["bsel"] = bs
        in_maps.append(m)
    if FUSED:
        res = run_bass_kernel_spmd(_CACHE["AB"], in_maps, core_ids=list(range(NC))).results
    else:
        keysA = ("xT", "cT", "w_ada", "b_ada", "ident", "na_w_qkv", "na_bias", "na_w_o", "ffn_w_gu", "ffn_w_down",
                 "mla_w_down", "kv_norm", "ropeC", "ropeS")
        resA = run_bass_kernel_spmd(_CACHE["A"], [{k: m[k] for k in keysA} for m in in_maps], core_ids=list(range(NC))).results
        if inputs.get("_debugA") is not None:
            return resA
        latall = np.ascontiguousarray(np.concatenate([resA[c]["lat"] for c in range(NC)], axis=0))
        keysB = ("cT", "w_ada", "b_ada", "ident", "mla_w_down", "ropeC", "ropeS", "bsel", "q_norm", "mla_w_uq", "mla_w_ukv",
                 "mla_w_o", "moe_w_router", "moe_w_gu", "moe_w_down", "final_norm")
        mapsB = []
        for c in range(NC):
            mb = {k: in_maps[c][k] for k in keysB}
            mb["x1"] = resA[c]["x1"]
            mb["latall"] = latall
            mapsB.append(mb)
        res = run_bass_kernel_spmd(_CACHE["B"], mapsB, core_ids=list(range(NC))).results
    out = np.empty((2, S, D), np.float32)
    for core in range(NC):
        b, q = core // 4, core % 4
        out[b, q * T:(q + 1) * T, :] = res[core]["out"].T
    return out
```

```python
import contextlib
import numpy as np
import concourse.bass as bass
import concourse.mybir as mybir
from concourse.bass_utils import run_bass_kernel_spmd

F32 = mybir.dt.float32
BF16 = mybir.dt.bfloat16
ALU = mybir.AluOpType
AF = mybir.ActivationFunctionType

ENGS = ("pe", "act", "dve", "pool", "sp")
SAME_ENGINE_SYNC = True

D = 1024
T = 2048
TE = 2560
S = 8192
NC = 8
EPS = 1e-6
FFN = 2816
NEXP = 8
EDIM = 1792
NEG = -30000.0
ARENA_WORDS = 50 * 1024


class Prog:
    def __init__(self, nc):
        self.nc = nc
        self.ops = []
        self.last_writer = {}
        self.readers = {}
        self.pending_bar = {}
        self.last_on_eng = {}
        self.last_dma_on_key = {}

    def add(self, eng, fn, reads=(), writes=(), key=None, inc=16):
        i = len(self.ops)
        deps = set()
        for r in reads:
            w = self.last_writer.get(r)
            if w is not None:
                deps.add(w)
        for w_ in writes:
            w = self.last_writer.get(w_)
            if w is not None:
                deps.add(w)
            deps.update(self.readers.get(w_, ()))
        if eng in self.pending_bar:
            deps.update(self.pending_bar.pop(eng))
        deps.discard(i)
        for r in reads:
            self.readers.setdefault(r, []).append(i)
        for w_ in writes:
            self.last_writer[w_] = i
            self.readers[w_] = []
        self.ops.append(dict(eng=eng, fn=fn, deps=deps, key=key, inc=inc))
        self.last_on_eng[eng] = i
        if key is not None:
            self.last_dma_on_key[key] = i
        return i

    def barrier(self):
        b = set(self.last_on_eng.values()) | set(self.last_dma_on_key.values())
        for e in ENGS:
            self.pending_bar[e] = set(b) | self.pending_bar.get(e, set())

    def emit(self, final_wait_eng="sp"):
        nc = self.nc
        ops = self.ops
        n = len(ops)
        fin = set(self.last_on_eng.values()) | set(self.last_dma_on_key.values())
        ops.append(dict(eng=final_wait_eng, fn=None, deps=fin, key=None, inc=0))
        waited_eng = {}
        waited_key = {}
        needed = [False] * (n + 1)
        for i, op in enumerate(ops):
            e = op["eng"]
            best_eng = {}
            best_key = {}
            for d in op["deps"]:
                od = ops[d]
                if od["key"] is not None:
                    k = od["key"]
                    if d > best_key.get(k, -1):
                        best_key[k] = d
                else:
                    se = od["eng"]
                    if se == e and (se == "pe" or not SAME_ENGINE_SYNC):
                        continue
                    if d > best_eng.get(se, -1):
                        best_eng[se] = d
            fdeps = []
            for se, d in best_eng.items():
                if waited_eng.get((e, se), -1) >= d:
                    continue
                waited_eng[(e, se)] = d
                fdeps.append(d)
            for k, d in best_key.items():
                if waited_key.get((e, k), -1) >= d:
                    continue
                waited_key[(e, k)] = d
                fdeps.append(d)
            op["fdeps"] = fdeps
            for d in fdeps:
                needed[d] = True
        seq = {e: 0 for e in ENGS}
        keycnt = {}
        for i, op in enumerate(ops):
            if op["key"] is not None:
                k = op["key"]
                keycnt[k] = keycnt.get(k, 0) + op["inc"]
                op["semval"] = keycnt[k]
            elif needed[i]:
                seq[op["eng"]] += 1
                op["semval"] = seq[op["eng"]]
        self.stats = dict(n_ops=n, n_sig=dict(seq), n_keys=len(keycnt))
        running = {}
        for i, op in enumerate(ops):
            waits = []
            for d in op["fdeps"]:
                od = ops[d]
                if od["key"] is not None:
                    waits.append(("key", od["key"], running[od["key"]]))
                else:
                    waits.append(("eng", od["eng"], od["semval"]))
            op["waits"] = waits
            op["sig"] = needed[i]
            if op["key"] is not None:
                running[op["key"]] = op["semval"]
        sems = {}
        with contextlib.ExitStack() as st:
            for e in ENGS:
                sems[("eng", e)] = st.enter_context(nc.semaphore("s_" + e))
            for k in keycnt:
                sems[("key", k)] = st.enter_context(nc.semaphore("k_" + k))
            block = st.enter_context(nc.Block())
            by_eng = {e: [op for op in ops if op["eng"] == e] for e in ENGS}

            def run(eh, e):
                for op in by_eng[e]:
                    for (kind, name, val) in op["waits"]:
                        eh.wait_ge(sems[(kind, name)], val)
                    if op["fn"] is None:
                        continue
                    ins = op["fn"](eh)
                    if op["key"] is not None:
                        ins.then_inc(sems[("key", op["key"])], op["inc"])
                    elif op["sig"]:
                        ins.then_inc(sems[("eng", e)], 1)

            @block.tensor
            def _(eh):
                run(eh, "pe")

            @block.scalar
            def _(eh):
                run(eh, "act")

            @block.vector
            def _(eh):
                run(eh, "dve")

            @block.gpsimd
            def _(eh):
                run(eh, "pool")

            @block.sync
            def _(eh):
                run(eh, "sp")


class Arena:
    def __init__(self, nc, nwords):
        self.t = nc.alloc_sbuf_tensor("arena", [128, nwords], F32)
        self.n = nwords
        self.off = 0
        self.peak = 0

    def alloc(self, shape, dtype):
        nel = int(np.prod(shape))
        nb = nel * (4 if dtype == F32 else 2)
        nw = (nb + 3) // 4
        nw = (nw + 7) // 8 * 8
        assert self.off + nw <= self.n, f"arena overflow {self.off}+{nw}>{self.n}"
        ap = self.t[:, self.off:self.off + nw]
        self.off += nw
        self.peak = max(self.peak, self.off)
        if dtype != F32:
            ap = ap.bitcast(dtype)
        ap = ap[:, 0:nel]
        if len(shape) == 2:
            ap = ap.rearrange("p (a b) -> p a b", a=shape[0], b=shape[1])
        elif len(shape) == 3:
            ap = ap.rearrange("p (a b c) -> p a b c", a=shape[0], b=shape[1], c=shape[2])
        elif len(shape) == 4:
            ap = ap.rearrange("p (a b c d) -> p a b c d", a=shape[0], b=shape[1], c=shape[2], d=shape[3])
        return ap

    def mark(self):
        return self.off

    def reset(self, m):
        self.off = m


class Ctx:
    pass


def na_tiles(lr):
    if lr < 4:
        return list(range(lr // 2, 6))
    if lr <= 28:
        return list(range(lr // 2, (lr + 7) // 2 + 1))
    return list(range(14, (lr + 7) // 2 + 1))


NA_SETS = [4, 5, 0, 1, 2, 3, 29, 30, 31]


def na_boff(lr):
    offs = {}
    o = 0
    for s in NA_SETS:
        offs[s] = o
        o += len(na_tiles(s))
    if lr < 4 or lr > 28:
        return offs[lr]
    return offs[4] if lr % 2 == 0 else offs[5]


NA_NBLK = sum(len(na_tiles(s)) for s in NA_SETS)


def host_na_bias(rpb, q):
    out = np.empty((16, 128, NA_NBLK * 64), np.float32)
    kk = np.arange(128)
    c = np.arange(64)
    cs = np.clip(c - 8, 0, 48)
    o = 0
    for s in NA_SETS:
        r = 32 * q + s
        rs = min(max(r - 4, 0), 120)
        for t in na_tiles(s):
            kr = 2 * t + kk // 64
            kcol = kk % 64
            krow = 32 * q - 4 + kr
            vrow = (krow >= rs) & (krow < rs + 8)
            vcol = (kcol[:, None] >= cs[None, :]) & (kcol[:, None] < cs[None, :] + 16)
            valid = vrow[:, None] & vcol
            dr = np.clip(krow - r + 7, 0, 14)
            dc = np.clip(kcol[:, None] - c[None, :] + 15, 0, 30)
            vals = rpb[:, dr[:, None], dc]
            out[:, :, o * 64:(o + 1) * 64] = np.where(valid[None], vals, np.float32(NEG))
            o += 1
    return out


def host_rope_tables(q):
    lr = np.arange(32)
    row = (32 * q + lr).astype(np.float32)
    col = np.arange(64).astype(np.float32)
    n = 8
    inv = (np.float32(10000.0) ** (-np.arange(n, dtype=np.float32) / np.float32(n))).astype(np.float32)
    rowang = row[:, None] * inv[None, :]
    colang = col[:, None] * inv[None, :]
    ang = np.zeros((32, 64, 16), np.float32)
    ang[:, :, 0:8] = rowang[:, None, :]
    ang[:, :, 8:16] = colang[None, :, :]
    ang = ang.reshape(2048, 16)
    cos = np.cos(ang).astype(np.float32)
    sin = np.sin(ang).astype(np.float32)
    C = np.repeat(cos.T, 2, axis=0)
    Sn = np.repeat(sin.T, 2, axis=0)
    Cf = np.zeros((128, 2048), np.float32)
    Sf = np.zeros((128, 2048), np.float32)
    for base in (0, 64):
        Cf[base:base + 32] = C
        Sf[base:base + 32] = Sn
    return Cf, Sf


def bank(K, b):
    return K.ps[:, b * 512:(b + 1) * 512]


def mm(K, out, lhsT, rhs, start, stop, reads, writes):
    K.P.add("pe", lambda e: e.matmul(out, lhsT, rhs, start=start, stop=stop), reads=reads, writes=writes)


def setup_consts(K):
    P, A = K.P, K.A
    K.onesD = A.alloc([128], F32)
    K.ones256 = A.alloc([128], F32)
    K.ones128 = A.alloc([128], F32)
    K.ident = A.alloc([128], F32)
    K.onespad = A.alloc([2, 128], BF16)
    P.add("dve", lambda e: e.memset(K.onesD, 1.0 / 1024), writes=["consts"])
    P.add("dve", lambda e: e.memset(K.ones256, 1.0 / 256), writes=["consts"])
    P.add("dve", lambda e: e.memset(K.ones128, 1.0 / 128), writes=["consts"])
    P.add("dve", lambda e: e.memset(K.onespad, 0.0), writes=["consts"])
    P.add("dve", lambda e: e.memset(K.onespad[:, 0, 0:64], 1.0), writes=["consts"])
    P.add("dve", lambda e: e.memset(K.onespad[:, 1, 64:128], 1.0), writes=["consts"])
    P.add("sp", lambda e: e.dma_start(out=K.ident, in_=K.din["ident"]), writes=["ident"], key="ident")
    K.cact = A.alloc([8], F32)
    K.mod = [A.alloc([48], F32), A.alloc([48], F32)]
    K.bada = [A.alloc([48], F32), A.alloc([48], F32)]
    K.norm_ctr = 0


def adaln(K, layers):
    P, A = K.P, K.A
    m0 = A.mark()
    wa = [A.alloc([8, 768], F32), A.alloc([8, 768], F32)]
    P.add("sp", lambda e: e.dma_start(out=K.cact, in_=K.din["cT"]), writes=["cact"], key="cact")
    P.add("act", lambda e: e.activation(K.cact, K.cact, AF.Silu), reads=["cact"], writes=["cact"])
    cnt = 0
    for li in layers:
        P.add("sp", lambda e, li=li: e.dma_start(out=K.bada[li], in_=K.din["b_ada"][li]), writes=[f"bada{li}"], key=f"bada{li}")
        wv = K.din["w_ada"][li].rearrange("(kc p) n -> p kc n", p=128)
        for jb in range(8):
            buf = wa[cnt % 2]
            res = f"wa{cnt % 2}"
            cnt += 1
            P.add("sp", lambda e, buf=buf, jb=jb, wv=wv: e.dma_start(out=buf, in_=wv[:, :, jb * 768:(jb + 1) * 768]),
                  writes=[res], key=res)
            for j in range(6):
                col = jb * 6 + j
                for kc in range(8):
                    mm(K, bank(K, 0)[:, col:col + 1], buf[:, kc, j * 128:(j + 1) * 128], K.cact[:, kc:kc + 1],
                       kc == 0, kc == 7, [res, "cact"], ["ps0"])
        mod = K.mod[li]
        P.add("dve", lambda e, mod=mod, li=li: e.tensor_tensor(mod, bank(K, 0)[:, 0:48], K.bada[li], ALU.add),
              reads=["ps0", f"bada{li}"], writes=[f"mod{li}"])
        P.add("dve", lambda e, mod=mod: e.tensor_scalar_add(mod[:, 8:16], mod[:, 8:16], 1.0), reads=[f"mod{li}"], writes=[f"mod{li}"])
        P.add("dve", lambda e, mod=mod: e.tensor_scalar_add(mod[:, 32:40], mod[:, 32:40], 1.0), reads=[f"mod{li}"], writes=[f"mod{li}"])
    P.barrier()
    A.reset(m0)


def alloc_norm_scratch(K):
    A = K.A
    K.sq = [A.alloc([512], F32), A.alloc([512], F32)]
    K.rs = [A.alloc([512], F32), A.alloc([512], F32)]
    K.ntmp = [A.alloc([512], F32), A.alloc([512], F32)]


def modulate_block(K, src_fn, src_res, n, mod, modres, sc0, sh0, dst_fn, dst_res_fn, nb0):
    P = K.P
    i = K.norm_ctr
    K.norm_ctr += 1
    psb = nb0 + (i % 2)
    for c in range(8):
        sq = K.sq[c % 2]
        P.add("pool", lambda e, sq=sq, c=c: e.tensor_tensor(sq[:, :n], src_fn(c), src_fn(c), ALU.mult),
              reads=[src_res(c)], writes=[f"sq{c % 2}"])
        mm(K, bank(K, psb)[:, :n], K.onesD, sq[:, :n], c == 0, c == 7, [f"sq{c % 2}", "consts"], [f"ps{psb}"])
    rs = K.rs[i % 2]
    rr = f"rs{i % 2}"
    P.add("act", lambda e: e.activation(rs[:, :n], bank(K, psb)[:, :n], AF.Sqrt, bias=EPS, scale=1.0),
          reads=[f"ps{psb}"], writes=[rr])
    P.add("dve", lambda e: e.reciprocal(rs[:, :n], rs[:, :n]), reads=[rr], writes=[rr])
    for c in range(8):
        if mod is not None:
            tmp = K.ntmp[c % 2]
            tr = f"ntmp{c % 2}"
            P.add("dve", lambda e, c=c, tmp=tmp: e.scalar_tensor_tensor(tmp[:, :n], src_fn(c), mod[:, sc0 + c:sc0 + c + 1], rs[:, :n],
                                                                        ALU.mult, ALU.mult),
                  reads=[src_res(c), rr, modres], writes=[tr])
            P.add("act", lambda e, c=c, tmp=tmp: e.activation(dst_fn(c), tmp[:, :n], AF.Identity, bias=mod[:, sh0 + c:sh0 + c + 1], scale=1.0),
                  reads=[tr, modres], writes=[dst_res_fn(c)])
        else:
            P.add("dve", lambda e, c=c: e.scalar_tensor_tensor(dst_fn(c), src_fn(c), K.fscale[:, c:c + 1], rs[:, :n],
                                                               ALU.mult, ALU.mult),
                  reads=[src_res(c), rr, "fscale"], writes=[dst_res_fn(c)])


def load_x(K, src_dram, tok0, dres=None):
    P = K.P
    v = src_dram.rearrange("(c p) t -> p c t", p=128)
    for c in range(8):
        P.add("sp", lambda e, c=c: e.dma_start(out=K.x[:, c, :], in_=v[:, c, tok0:tok0 + T]),
              reads=([f"{dres}{c}"] if dres else []), writes=[f"x{c}_{tb}" for tb in range(4)], key=f"x{c}")


def xres(c, tb):
    return f"x{c}_{tb}"


def proj_residual(K, w_dram, inT, in_res, mod, modres, g0):
    P, A = K.P, K.A
    m0 = A.mark()
    wo = A.alloc([8, 1024], BF16)
    wv = w_dram.rearrange("(kc p) n -> p kc n", p=128)
    for hf in range(2):
        P.add("pool", lambda e, hf=hf: e.dma_start(out=wo[:, hf * 4:(hf + 1) * 4, :], in_=wv[:, hf * 4:(hf + 1) * 4, :]),
              writes=[f"wo{hf}"], key=f"wo{hf}")
    k = 0
    for tb in range(4):
        for oc in range(8):
            b = 4 + (k % 2)
            k += 1
            for kc in range(8):
                mm(K, bank(K, b), wo[:, kc, oc * 128:(oc + 1) * 128], inT[:, kc, tb * 512:(tb + 1) * 512],
                   kc == 0, kc == 7, [f"wo{kc // 4}", in_res(kc)], [f"ps{b}"])
            P.add("dve", lambda e, b=b, oc=oc, tb=tb: e.scalar_tensor_tensor(
                K.x[:, oc, tb * 512:(tb + 1) * 512], bank(K, b), mod[:, g0 + oc:g0 + oc + 1],
                K.x[:, oc, tb * 512:(tb + 1) * 512], ALU.mult, ALU.add),
                reads=[f"ps{b}", xres(oc, tb), modres], writes=[xres(oc, tb)])
    P.barrier()
    A.reset(m0)


def swiglu_stream(K, hT, hres, groups, mod, modres, g0, comb_fn=None):
    P, A = K.P, K.A
    NF = max(g["nf"] for g in groups)
    Wg = [A.alloc([8, NF * 128], BF16) for _ in range(2)]
    Wu = [A.alloc([8, NF * 128], BF16) for _ in range(2)]
    Wd = [A.alloc([NF, 1024], BF16) for _ in range(2)]
    act = [A.alloc([NF, 512], BF16) for _ in range(2)]
    t1 = [A.alloc([512], BF16) for _ in range(2)]
    t2 = [A.alloc([512], BF16) for _ in range(2)]
    steps = [(gi, tb) for gi in range(len(groups)) for tb in range(4)]
    loaded = set()
    ctr = dict(gu=0, y=0)

    def ensure_loaded(gi):
        if gi in loaded or gi >= len(groups):
            return
        loaded.add(gi)
        par = gi % 2
        groups[gi]["load"](K, Wg[par], Wu[par], Wd[par], par)

    def emit_gu(si):
        gi, tb = steps[si]
        g = groups[gi]
        par = gi % 2
        assert gi in loaded
        ab = act[si % 2]
        ar = f"act{si % 2}"
        comb = comb_fn(g, tb) if comb_fn is not None else None
        for fi in range(g["nf"]):
            k = ctr["gu"]
            ctr["gu"] += 1
            bg = 0 + (k % 2)
            bu = 2 + (k % 2)
            for kc in range(8):
                mm(K, bank(K, bg), Wg[par][:, kc, fi * 128:(fi + 1) * 128], hT[:, kc, tb * 512:(tb + 1) * 512],
                   kc == 0, kc == 7, [f"Wg{par}", hres(kc)], [f"ps{bg}"])
            for kc in range(8):
                mm(K, bank(K, bu), Wu[par][:, kc, fi * 128:(fi + 1) * 128], hT[:, kc, tb * 512:(tb + 1) * 512],
                   kc == 0, kc == 7, [f"Wu{par}", hres(kc)], [f"ps{bu}"])
            tt = t1[k % 2]
            tr = f"t1_{k % 2}"
            P.add("act", lambda e, tt=tt, bg=bg: e.activation(tt, bank(K, bg), AF.Silu), reads=[f"ps{bg}"], writes=[tr])
            src, sr = tt, tr
            if comb is not None:
                cap, cres = comb
                t2b = t2[k % 2]
                t2r = f"t2_{k % 2}"
                P.add("pool", lambda e, t2b=t2b, tt=tt, cap=cap: e.tensor_tensor(t2b, tt, cap, ALU.mult),
                      reads=[tr, cres], writes=[t2r])
                src, sr = t2b, t2r
            P.add("dve", lambda e, ab=ab, fi=fi, src=src, bu=bu: e.tensor_tensor(ab[:, fi, :], src, bank(K, bu), ALU.mult),
                  reads=[sr, f"ps{bu}"], writes=[ar])

    def emit_down(si):
        gi, tb = steps[si]
        g = groups[gi]
        par = gi % 2
        ab = act[si % 2]
        ar = f"act{si % 2}"
        nf = g["nf"]
        for oc in range(8):
            k = ctr["y"]
            ctr["y"] += 1
            b = 4 + (k % 4)
            for fi in range(nf):
                mm(K, bank(K, b), Wd[par][:, fi, oc * 128:(oc + 1) * 128], ab[:, fi, :], fi == 0, fi == nf - 1,
                   [f"Wd{par}", ar], [f"ps{b}"])
            P.add("dve", lambda e, b=b, oc=oc, tb=tb: e.scalar_tensor_tensor(
                K.x[:, oc, tb * 512:(tb + 1) * 512], bank(K, b), mod[:, g0 + oc:g0 + oc + 1],
                K.x[:, oc, tb * 512:(tb + 1) * 512], ALU.mult, ALU.add),
                reads=[f"ps{b}", xres(oc, tb), modres], writes=[xres(oc, tb)])

    ensure_loaded(0)
    ensure_loaded(1)
    emit_gu(0)
    for si in range(len(steps)):
        if si + 1 < len(steps):
            emit_gu(si + 1)
        emit_down(si)
        if steps[si][1] == 3:
            ensure_loaded(steps[si][0] + 2)


def phase_A(K):
    P, A, din = K.P, K.A, K.din
    setup_consts(K)
    adaln(K, [0, 1])
    mod0, mod1 = K.mod
    mA = A.mark()
    hT = A.alloc([8, TE], BF16)
    OT = A.alloc([8, T], BF16)
    m1 = A.mark()
    alloc_norm_scratch(K)
    xs = [A.alloc([8, 512], F32), A.alloc([8, 512], F32)]
    xv = din["xT"].rearrange("(c p) t -> p c t", p=128)

    def hres(c, lo, hi):
        return [f"h{c}_{tb}" for tb in range(lo // 512, (hi - 1) // 512 + 1)]

    for tb in range(5):
        buf = xs[tb % 2]
        P.add("sp", lambda e, buf=buf, tb=tb: e.dma_start(out=buf, in_=xv[:, :, tb * 512:(tb + 1) * 512]),
              writes=[f"xs{tb % 2}"], key=f"xs{tb % 2}")
        modulate_block(K, lambda c, buf=buf: buf[:, c, :], lambda c, tb=tb: f"xs{tb % 2}", 512, mod0, "mod0", 8, 0,
                       lambda c, tb=tb: hT[:, c, tb * 512:(tb + 1) * 512], lambda c, tb=tb: f"h{c}_{tb}", 6)
    P.barrier()
    A.reset(m1)
    wqkv = [A.alloc([3, 8, 128], BF16) for _ in range(2)]
    qT = [A.alloc([T], BF16) for _ in range(2)]
    kT = [A.alloc([TE], BF16) for _ in range(2)]
    Vp = [A.alloc([20, 2, 128], BF16) for _ in range(2)]
    biasb = [A.alloc([NA_NBLK * 64], F32) for _ in range(2)]
    stmp = [A.alloc([384], F32) for _ in range(3)]
    pT = [A.alloc([384], BF16) for _ in range(3)]
    rden = [A.alloc([512], F32) for _ in range(2)]
    for par in range(2):
        P.add("pool", lambda e, par=par: e.memset(Vp[par], 0.0), writes=[f"Vp{par}"])
    wv = din["na_w_qkv"].rearrange("(kc p) n -> p kc n", p=128)
    nm = ["wq", "wk", "wv"]
    PB = 7
    for p in range(8):
        par = p % 2
        for s in range(3):
            P.add("pool", lambda e, par=par, s=s, p=p: e.dma_start(out=wqkv[par][:, s], in_=wv[:, :, s * 1024 + p * 128:s * 1024 + (p + 1) * 128]),
                  writes=[f"{nm[s]}{par}"], key=f"{nm[s]}{par}")
        for h2 in range(2):
            h = 2 * p + h2
            P.add("sp", lambda e, h2=h2, h=h: e.dma_start(out=biasb[h2], in_=din["na_bias"][h]), writes=[f"bias{h2}"], key=f"bias{h2}")
        for tb in range(4):
            lo = 256 + tb * 512
            for kc in range(8):
                mm(K, bank(K, PB), wqkv[par][:, 0, kc, :], hT[:, kc, lo:lo + 512], kc == 0, kc == 7,
                   [f"wq{par}"] + hres(kc, lo, lo + 512), [f"ps{PB}"])
            P.add("act", lambda e, par=par, tb=tb: e.copy(qT[par][:, tb * 512:(tb + 1) * 512], bank(K, PB)),
                  reads=[f"ps{PB}"], writes=[f"qT{par}"])
        for tb in range(5):
            lo = tb * 512
            for kc in range(8):
                mm(K, bank(K, PB), wqkv[par][:, 1, kc, :], hT[:, kc, lo:lo + 512], kc == 0, kc == 7,
                   [f"wk{par}"] + hres(kc, lo, lo + 512), [f"ps{PB}"])
            P.add("act", lambda e, par=par, tb=tb: e.copy(kT[par][:, tb * 512:(tb + 1) * 512], bank(K, PB)),
                  reads=[f"ps{PB}"], writes=[f"kT{par}"])
        for g in range(5):
            for j in range(4):
                t = g * 4 + j
                for kc in range(8):
                    mm(K, bank(K, PB)[:, j * 128:(j + 1) * 128], hT[:, kc, t * 128:(t + 1) * 128], wqkv[par][:, 2, kc, :],
                       kc == 0, kc == 7, [f"wv{par}"] + hres(kc, t * 128, (t + 1) * 128), [f"ps{PB}"])
            pv = bank(K, PB).rearrange("p (t c) -> p t c", c=128)
            P.add("dve", lambda e, par=par, g=g, pv=pv: e.tensor_copy(Vp[par][:, g * 4:(g + 1) * 4, 0, 0:64], pv[:, :, 0:64]),
                  reads=[f"ps{PB}"], writes=[f"Vp{par}"])
            P.add("dve", lambda e, par=par, g=g, pv=pv: e.tensor_copy(Vp[par][:, g * 4:(g + 1) * 4, 1, 64:128], pv[:, :, 64:128]),
                  reads=[f"ps{PB}"], writes=[f"Vp{par}"])
        steps = [(lr, h2) for lr in range(32) for h2 in range(2)]

        def emit_S(si, par=par):
            lr, h2 = steps[si]
            sb = si % 3
            for j, t in enumerate(na_tiles(lr)):
                mm(K, bank(K, sb)[:, j * 64:(j + 1) * 64], kT[par][h2 * 64:(h2 + 1) * 64, t * 128:(t + 1) * 128],
                   qT[par][h2 * 64:(h2 + 1) * 64, lr * 64:(lr + 1) * 64], True, True, [f"kT{par}", f"qT{par}"], [f"ps{sb}"])

        def emit_soft(si):
            lr, h2 = steps[si]
            sb = si % 3
            nt = len(na_tiles(lr))
            bo = na_boff(lr)
            tmp = stmp[si % 3]
            pt = pT[si % 3]
            P.add("dve", lambda e: e.scalar_tensor_tensor(tmp[:, :nt * 64], bank(K, sb)[:, :nt * 64], 0.125,
                                                          biasb[h2][:, bo * 64:(bo + nt) * 64], ALU.mult, ALU.add),
                  reads=[f"ps{sb}", f"bias{h2}"], writes=[f"stmp{si % 3}"])
            P.add("act", lambda e: e.activation(pt[:, :nt * 64], tmp[:, :nt * 64], AF.Exp),
                  reads=[f"stmp{si % 3}"], writes=[f"pT{si % 3}"])

        def emit_PV(si, par=par, p=p):
            lr, h2 = steps[si]
            grp, slot = lr // 8, lr % 8
            ob = 3 + (grp % 2)
            db = 5 + (grp % 2)
            pt = pT[si % 3]
            tiles = na_tiles(lr)
            for j, t in enumerate(tiles):
                first = (h2 == 0 and j == 0)
                last = (h2 == 1 and j == len(tiles) - 1)
                mm(K, bank(K, ob)[:, slot * 64:(slot + 1) * 64], Vp[par][:, t, h2, :], pt[:, j * 64:(j + 1) * 64],
                   first, last, [f"Vp{par}", f"pT{si % 3}"], [f"ps{ob}"])
                mm(K, bank(K, db)[:, slot * 64:(slot + 1) * 64], K.onespad[:, h2, :], pt[:, j * 64:(j + 1) * 64],
                   first, last, ["consts", f"pT{si % 3}"], [f"ps{db}"])
            if slot == 7 and h2 == 1:
                rd = rden[0]
                P.add("dve", lambda e: e.reciprocal(rd, bank(K, db)), reads=[f"ps{db}"], writes=["rden"])
                P.add("dve", lambda e: e.tensor_tensor(OT[:, p, grp * 512:(grp + 1) * 512], bank(K, ob), rd, ALU.mult),
                      reads=[f"ps{ob}", "rden"], writes=[f"OT{p}"])

        emit_S(0)
        emit_S(1)
        for si in range(len(steps)):
            emit_soft(si)
            if si + 2 < len(steps):
                emit_S(si + 2)
            emit_PV(si)
    P.barrier()
    A.reset(mA)
    OT2 = A.alloc([8, TE], BF16)
    OT = A.alloc([8, T], BF16)
    K.x = A.alloc([8, T], F32)
    load_x(K, din["xT"], 256)
    proj_residual(K, din["na_w_o"], OT, lambda kc: f"OT{kc}", mod0, "mod0", 16)
    A.reset(mA)
    hT2 = A.alloc([8, T], BF16)
    skip = A.alloc([8, TE - T], BF16)
    skip2 = A.alloc([8, T], BF16)
    K.x = A.alloc([8, T], F32)
    alloc_norm_scratch(K)
    for tb in range(4):
        modulate_block(K, lambda c, tb=tb: K.x[:, c, tb * 512:(tb + 1) * 512], lambda c, tb=tb: xres(c, tb), 512, mod0, "mod0", 32, 24,
                       lambda c, tb=tb: hT2[:, c, tb * 512:(tb + 1) * 512], lambda c: f"h2_{c}", 6)
    gu = din["ffn_w_gu"].rearrange("(kc p) n -> p kc n", p=128)
    dn = din["ffn_w_down"].rearrange("(f p) n -> p f n", p=128)
    groups = []
    for gi in range(11):
        def load(K, Wg, Wu, Wd, par, gi=gi):
            K.P.add("pool", lambda e: e.dma_start(out=Wg[:, :, 0:256], in_=gu[:, :, gi * 256:(gi + 1) * 256]), writes=[f"Wg{par}"], key=f"Wg{par}")
            K.P.add("pool", lambda e: e.dma_start(out=Wu[:, :, 0:256], in_=gu[:, :, FFN + gi * 256:FFN + (gi + 1) * 256]), writes=[f"Wu{par}"], key=f"Wu{par}")
            K.P.add("pool", lambda e: e.dma_start(out=Wd[:, 0:2, :], in_=dn[:, gi * 2:(gi + 1) * 2, :]), writes=[f"Wd{par}"], key=f"Wd{par}")
        groups.append(dict(load=load, nf=2))
    swiglu_stream(K, hT2, lambda kc: f"h2_{kc}", groups, mod0, "mod0", 40)
    P.barrier()
    A.reset(mA)
    K.mA = mA
    hT3 = A.alloc([8, T], BF16)
    K.hT3 = hT3
    K.cq_region = A.alloc([2, T], BF16)
    K.OTmark = A.mark()
    K.OTreg = A.alloc([8, T], BF16)
    K.xmark = A.mark()
    K.x = A.alloc([8, T], F32)
    alloc_norm_scratch(K)
    for tb in range(4):
        modulate_block(K, lambda c, tb=tb: K.x[:, c, tb * 512:(tb + 1) * 512], lambda c, tb=tb: xres(c, tb), 512, mod1, "mod1", 8, 0,
                       lambda c, tb=tb: hT3[:, c, tb * 512:(tb + 1) * 512], lambda c: f"h3_{c}", 6)
    xo = din["x1"].rearrange("(c p) t -> p c t", p=128)
    for c in range(8):
        P.add("sp", lambda e, c=c: e.dma_start(out=xo[:, c, :], in_=K.x[:, c, :]), reads=[xres(c, tb) for tb in range(4)],
              writes=[f"x1d{c}"], key=f"x{c}")
    mla_latent(K, hT3, lambda kc: f"h3_{kc}")


def mla_latent(K, hT3, hres):
    P, A, din = K.P, K.A, K.din
    wd = A.alloc([8, 160], BF16)
    wsw = A.alloc([8, 32], BF16)
    kvn = A.alloc([1], F32)
    ropeC = A.alloc([T], F32)
    ropeS = A.alloc([T], F32)
    dkv = A.alloc([512], F32)
    sqb = A.alloc([512], F32)
    rsb = A.alloc([512], F32)
    lat = A.alloc([T], F32)
    krot = A.alloc([T], F32)
    ktmp = A.alloc([512], F32)
    wv = din["mla_w_down"].rearrange("(kc p) n -> p kc n", p=128)
    P.add("pool", lambda e: e.dma_start(out=wd, in_=wv[:, :, 256:416]), writes=["wd"], key="wd")
    P.add("sp", lambda e: e.dma_start(out=kvn, in_=din["kv_norm"]), writes=["kvn"], key="kvn")
    P.add("sp", lambda e: e.dma_start(out=ropeC, in_=din["ropeC"]), writes=["ropeC"], key="ropeC")
    P.add("sp", lambda e: e.dma_start(out=ropeS, in_=din["ropeS"]), writes=["ropeS"], key="ropeS")
    wr = wd[:, :, 128:160].rearrange("p k (i two) -> p k i two", two=2)
    ws = wsw.rearrange("p k (i two) -> p k i two", two=2)
    P.add("dve", lambda e: e.tensor_scalar_mul(ws[:, :, :, 0], wr[:, :, :, 1], -1.0), reads=["wd"], writes=["wsw"])
    P.add("dve", lambda e: e.tensor_copy(ws[:, :, :, 1], wr[:, :, :, 0]), reads=["wd"], writes=["wsw"])
    for tb in range(4):
        sl = slice(tb * 512, (tb + 1) * 512)
        for kc in range(8):
            mm(K, bank(K, 0), wd[:, kc, 0:128], hT3[:, kc, sl], kc == 0, kc == 7, ["wd", hres(kc)], ["ps0"])
        for kc in range(8):
            mm(K, bank(K, 1)[0:32, :], wd[:, kc, 128:160], hT3[:, kc, sl], kc == 0, kc == 7, ["wd", hres(kc)], ["ps1"])
        for kc in range(8):
            mm(K, bank(K, 2)[0:32, :], wsw[:, kc, :], hT3[:, kc, sl], kc == 0, kc == 7, ["wsw", hres(kc)], ["ps2"])
        P.add("act", lambda e: e.copy(dkv, bank(K, 0)), reads=["ps0"], writes=["dkv"])
        P.add("pool", lambda e: e.tensor_tensor(sqb, dkv, dkv, ALU.mult), reads=["dkv"], writes=["sqb"])
        mm(K, bank(K, 3), K.ones128, sqb, True, True, ["sqb", "consts"], ["ps3"])
        P.add("act", lambda e: e.activation(rsb, bank(K, 3), AF.Sqrt, bias=EPS, scale=1.0), reads=["ps3"], writes=["rsb"])
        P.add("dve", lambda e: e.reciprocal(rsb, rsb), reads=["rsb"], writes=["rsb"])
        P.add("dve", lambda e, sl=sl: e.scalar_tensor_tensor(lat[:, sl], dkv, kvn[:, 0:1], rsb, ALU.mult, ALU.mult),
              reads=["dkv", "rsb", "kvn"], writes=["lat"])
        P.add("dve", lambda e, sl=sl: e.tensor_tensor(krot[0:32, sl], bank(K, 1)[0:32, :], ropeC[0:32, sl], ALU.mult),
              reads=["ps1", "ropeC"], writes=["krot"])
        P.add("dve", lambda e, sl=sl: e.tensor_tensor(ktmp[0:32, :], bank(K, 2)[0:32, :], ropeS[0:32, sl], ALU.mult),
              reads=["ps2", "ropeS"], writes=["ktmp"])
        P.add("pool", lambda e, sl=sl: e.tensor_tensor(krot[0:32, sl], krot[0:32, sl], ktmp[0:32, :], ALU.add),
              reads=["krot", "ktmp"], writes=["krot"])
    P.add("sp", lambda e: e.dma_start(out=din["lat"][0:128, :], in_=lat), reads=["lat"], writes=["latd0"], key="lat")
    P.add("sp", lambda e: e.dma_start(out=din["lat"][128:160, :], in_=krot[0:32, :]), reads=["krot"], writes=["latd1"], key="krot")


def phase_B_prologue_unfused(K):
    P, A, din = K.P, K.A, K.din
    setup_consts(K)
    adaln(K, [1])
    mA = A.mark()
    K.mA = mA
    K.hT3 = A.alloc([8, T], BF16)
    K.cq_region = A.alloc([2, T], BF16)
    K.OTmark = A.mark()
    K.OTreg = A.alloc([8, T], BF16)
    K.xmark = A.mark()
    K.x = A.alloc([8, T], F32)
    load_x(K, din["x1"], 0)
    alloc_norm_scratch(K)
    for tb in range(4):
        modulate_block(K, lambda c, tb=tb: K.x[:, c, tb * 512:(tb + 1) * 512], lambda c, tb=tb: xres(c, tb), 512, K.mod[1], "mod1", 8, 0,
                       lambda c, tb=tb: K.hT3[:, c, tb * 512:(tb + 1) * 512], lambda c: f"h3_{c}", 6)
    P.barrier()


def phase_B(K):
    P, A, din = K.P, K.A, K.din
    mod1 = K.mod[1]
    mB = K.mA
    oT = K.OTreg
    cqT = K.cq_region
    hT3 = K.hT3
    m1 = K.xmark
    A.reset(m1)
    wdq = A.alloc([8, 256], BF16)
    qn = A.alloc([2], F32)
    dq = [A.alloc([512], F32), A.alloc([512], F32)]
    sqq = [A.alloc([512], F32), A.alloc([512], F32)]
    rsq = A.alloc([512], F32)
    wv = din["mla_w_down"].rearrange("(kc p) n -> p kc n", p=128)
    P.add("pool", lambda e: e.dma_start(out=wdq, in_=wv[:, :, 0:256]), writes=["wdq"], key="wdq")
    P.add("sp", lambda e: e.dma_start(out=qn, in_=din["q_norm"]), writes=["qn"], key="qn")
    for tb in range(4):
        sl = slice(tb * 512, (tb + 1) * 512)
        for j in range(2):
            for kc in range(8):
                mm(K, bank(K, j), wdq[:, kc, j * 128:(j + 1) * 128], hT3[:, kc, sl], kc == 0, kc == 7, ["wdq", f"h3_{kc}"], [f"ps{j}"])
            P.add("act", lambda e, j=j: e.copy(dq[j], bank(K, j)), reads=[f"ps{j}"], writes=[f"dq{j}"])
            P.add("pool", lambda e, j=j: e.tensor_tensor(sqq[j], dq[j], dq[j], ALU.mult), reads=[f"dq{j}"], writes=[f"sqq{j}"])
            mm(K, bank(K, 2), K.ones256, sqq[j], j == 0, j == 1, [f"sqq{j}", "consts"], ["ps2"])
        P.add("act", lambda e: e.activation(rsq, bank(K, 2), AF.Sqrt, bias=EPS, scale=1.0), reads=["ps2"], writes=["rsq"])
        P.add("dve", lambda e: e.reciprocal(rsq, rsq), reads=["rsq"], writes=["rsq"])
        for j in range(2):
            P.add("dve", lambda e, j=j, sl=sl: e.scalar_tensor_tensor(cqT[:, j, sl], dq[j], qn[:, j:j + 1], rsq, ALU.mult, ALU.mult),
                  reads=[f"dq{j}", "rsq", "qn"], writes=["cqT"])
    P.barrier()
    A.reset(m1)
    save_off = A.off
    A.off = K.mA
    ckvT = A.alloc([S], BF16)
    kh0 = A.alloc([S], BF16)
    A.off = save_off
    khT = [kh0, A.alloc([S], BF16)]
    Vh = [A.alloc([64, 128], BF16) for _ in range(2)]
    qhT = [A.alloc([T], BF16) for _ in range(2)]
    wuq = A.alloc([2, 1536], BF16)
    wuqsw = A.alloc([2, 16, 96], BF16)
    wukv = A.alloc([2048], BF16)
    ropeC = A.alloc([T], F32)
    ropeS = A.alloc([T], F32)
    pT = [A.alloc([1024], BF16) for _ in range(3)]
    rt1 = [A.alloc([512], F32) for _ in range(2)]
    rt2 = [A.alloc([512], F32) for _ in range(2)]
    rd = [A.alloc([512], F32) for _ in range(2)]
    otmp = [A.alloc([512], BF16) for _ in range(2)]
    la = din["latall"]
    bsel = A.alloc([2], F32)
    P.add("sp", lambda e: e.dma_start(out=bsel, in_=din["bsel"]), writes=["bsel"], key="bsel")
    CH = 512
    stA = [A.alloc([CH], F32) for _ in range(2)]
    stB = [A.alloc([CH], F32) for _ in range(2)]
    for par in range(2):
        P.add("pool", lambda e, par=par: e.memset(Vh[par][:, :, 64:128], 1.0), writes=[f"Vh1_{par}"])
    nch = T // CH
    it = 0
    for i in range(4):
        for j in range(nch):
            sa, sb_ = stA[it % 2], stB[it % 2]
            ra, rb = f"stA{it % 2}", f"stB{it % 2}"
            it += 1
            r0, r1 = i * 160, (4 + i) * 160
            cs = slice(j * CH, (j + 1) * CH)
            ks = slice(i * T + j * CH, i * T + (j + 1) * CH)
            P.add("sp", lambda e, sa=sa, r0=r0, cs=cs: e.dma_start(out=sa[0:128, :], in_=la[r0:r0 + 128, cs]), reads=["latall"], writes=[ra], key=ra)
            P.add("sp", lambda e, sb_=sb_, r1=r1, cs=cs: e.dma_start(out=sb_[0:128, :], in_=la[r1:r1 + 128, cs]), reads=["latall"], writes=[rb], key=rb)
            P.add("dve", lambda e, sa=sa: e.tensor_scalar_mul(sa, sa, bsel[:, 0:1]), reads=[ra, "bsel"], writes=[ra])
            P.add("dve", lambda e, sa=sa, sb_=sb_, ks=ks: e.scalar_tensor_tensor(ckvT[:, ks], sb_, bsel[:, 1:2], sa, ALU.mult, ALU.add),
                  reads=[ra, rb, "bsel"], writes=[f"ckv{i}"])
    for i in range(4):
        for j in range(nch):
            sa, sb_ = stA[it % 2], stB[it % 2]
            ra, rb = f"stA{it % 2}", f"stB{it % 2}"
            it += 1
            r0, r1 = i * 160 + 128, (4 + i) * 160 + 128
            cs = slice(j * CH, (j + 1) * CH)
            ks = slice(i * T + j * CH, i * T + (j + 1) * CH)
            P.add("sp", lambda e, sa=sa, r0=r0, cs=cs: e.dma_start(out=sa[64:96, :], in_=la[r0:r0 + 32, cs]), reads=["latall"], writes=[ra], key=ra)
            P.add("sp", lambda e, sb_=sb_, r1=r1, cs=cs: e.dma_start(out=sb_[64:96, :], in_=la[r1:r1 + 32, cs]), reads=["latall"], writes=[rb], key=rb)
            P.add("pool", lambda e, sa=sa: e.tensor_scalar_mul(sa[64:96, :], sa[64:96, :], bsel[64:96, 0:1]), reads=[ra, "bsel"], writes=[ra])
            for par in range(2):
                P.add("dve", lambda e, sa=sa, sb_=sb_, ks=ks, par=par: e.scalar_tensor_tensor(
                    khT[par][64:96, ks], sb_[64:96, :], bsel[64:96, 1:2], sa[64:96, :], ALU.mult, ALU.add),
                    reads=[ra, rb, "bsel"], writes=[f"khr{par}"])
    uq = din["mla_w_uq"].rearrange("(kc p) n -> p kc n", p=128)
    P.add("pool", lambda e: e.dma_start(out=wuq, in_=uq), writes=["wuq"], key="wuq")
    P.add("pool", lambda e: e.dma_start(out=wukv, in_=din["mla_w_ukv"]), writes=["wukv"], key="wukv")
    P.add("sp", lambda e: e.dma_start(out=ropeC, in_=din["ropeC"]), writes=["ropeC"], key="ropeC")
    P.add("sp", lambda e: e.dma_start(out=ropeS, in_=din["ropeS"]), writes=["ropeS"], key="ropeS")
    P.add("pool", lambda e: e.memset(wuqsw, 0.0), writes=["wuqsw"])
    wq4 = wuq.rearrange("p k (h f) -> p k h f", h=16)
    src = wq4[:, :, :, 64:96].rearrange("p k h (i two) -> p k h i two", two=2)
    dst = wuqsw[:, :, :, 64:96].rearrange("p k h (i two) -> p k h i two", two=2)
    for kc in range(2):
        P.add("dve", lambda e, kc=kc: e.tensor_scalar_mul(dst[:, kc, :, :, 0], src[:, kc, :, :, 1], -1.0), reads=["wuq", "wuqsw"], writes=["wuqsw"])
        P.add("dve", lambda e, kc=kc: e.tensor_copy(dst[:, kc, :, :, 1], src[:, kc, :, :, 0]), reads=["wuq", "wuqsw"], writes=["wuqsw"])
    SCALE = float(96 ** -0.5)
    PBK = [6, 7]
    pctr = [0]

    def pbank():
        b = PBK[pctr[0] % 2]
        pctr[0] += 1
        return b

    def emit_proj(h):
        par = h % 2
        for kb in range(16):
            b = pbank()
            mm(K, bank(K, b)[0:64, :], wukv[:, h * 128:h * 128 + 64], ckvT[:, kb * 512:(kb + 1) * 512], True, True,
               ["wukv", f"ckv{kb // 4}"], [f"ps{b}"])
            P.add("dve", lambda e, b=b, kb=kb, par=par: e.tensor_copy(khT[par][0:64, kb * 512:(kb + 1) * 512], bank(K, b)[0:64, :]),
                  reads=[f"ps{b}"], writes=[f"kh{par}_{kb}"])
        for g in range(8):
            b = pbank()
            for j in range(8):
                kt = g * 8 + j
                mm(K, bank(K, b)[:, j * 64:(j + 1) * 64], ckvT[:, kt * 128:(kt + 1) * 128], wukv[:, h * 128 + 64:h * 128 + 128], True, True,
                   ["wukv", f"ckv{kt // 16}"], [f"ps{b}"])
            pv = bank(K, b).rearrange("p (t c) -> p t c", c=64)
            P.add("dve", lambda e, g=g, par=par, pv=pv: e.tensor_copy(Vh[par][:, g * 8:(g + 1) * 8, 0:64], pv),
                  reads=[f"ps{b}"], writes=[f"Vh{par}_{g}"])
        for tb in range(4):
            sl = slice(tb * 512, (tb + 1) * 512)
            b1 = pbank()
            b2 = pbank()
            for kc in range(2):
                mm(K, bank(K, b1)[0:96, :], wuq[:, kc, h * 96:(h + 1) * 96], cqT[:, kc, sl], kc == 0, kc == 1, ["wuq", "cqT"], [f"ps{b1}"])
            for kc in range(2):
                mm(K, bank(K, b2)[0:96, :], wuqsw[:, kc, h, :], cqT[:, kc, sl], kc == 0, kc == 1, ["wuqsw", "cqT"], [f"ps{b2}"])
            i = tb % 2
            P.add("dve", lambda e, b1=b1, sl=sl, par=par: e.tensor_copy(qhT[par][0:64, sl], bank(K, b1)[0:64, :]),
                  reads=[f"ps{b1}"], writes=[f"qh{par}"])
            P.add("dve", lambda e, b1=b1, sl=sl, i=i: e.tensor_tensor(rt1[i][64:96, :], bank(K, b1)[64:96, :], ropeC[64:96, sl], ALU.mult),
                  reads=[f"ps{b1}", "ropeC"], writes=[f"rt1_{i}"])
            P.add("dve", lambda e, b2=b2, sl=sl, i=i: e.tensor_tensor(rt2[i][64:96, :], bank(K, b2)[64:96, :], ropeS[64:96, sl], ALU.mult),
                  reads=[f"ps{b2}", "ropeS"], writes=[f"rt2_{i}"])
            P.add("pool", lambda e, sl=sl, i=i, par=par: e.tensor_tensor(qhT[par][64:96, sl], rt1[i][64:96, :], rt2[i][64:96, :], ALU.add),
                  reads=[f"rt1_{i}", f"rt2_{i}"], writes=[f"qh{par}"])

    sctr = [0]

    def emit_S(h, qb, k2):
        par = h % 2
        si = sctr[0]
        sctr[0] += 1
        b0 = (si % 2) * 2
        for j in range(2):
            kt = k2 * 2 + j
            mm(K, bank(K, b0 + j), khT[par][0:96, kt * 128:(kt + 1) * 128], qhT[par][0:96, qb * 512:(qb + 1) * 512], True, True,
               [f"kh{par}_{kt // 4}", f"khr{par}", f"qh{par}"], [f"ps{b0 + j}"])
        return si

    def emit_exp_pv(h, qb, k2, si):
        par = h % 2
        b0 = (si % 2) * 2
        pt = pT[si % 3]
        ob = 4 + ((h * 4 + qb) % 2)
        P.add("act", lambda e: e.activation(pt, K.ps[:, b0 * 512:(b0 + 2) * 512], AF.Exp, scale=SCALE),
              reads=[f"ps{b0}", f"ps{b0 + 1}"], writes=[f"pT{si % 3}"])
        for j in range(2):
            kt = k2 * 2 + j
            mm(K, bank(K, ob), Vh[par][:, kt, :], pt[:, j * 512:(j + 1) * 512], kt == 0, kt == 63,
               [f"Vh{par}_{kt // 8}", f"Vh1_{par}", f"pT{si % 3}"], [f"ps{ob}"])
        if k2 == 31:
            i = (h * 4 + qb) % 2
            c = h // 2
            sl = slice(qb * 512, (qb + 1) * 512)
            P.add("dve", lambda e: e.reciprocal(rd[i][64:128, :], bank(K, ob)[64:128, :]), reads=[f"ps{ob}"], writes=[f"rd{i}"])
            P.add("dve", lambda e: e.tensor_copy(rd[i][0:64, :], rd[i][64:128, :]), reads=[f"rd{i}"], writes=[f"rd{i}"])
            if par == 0:
                P.add("dve", lambda e: e.tensor_tensor(oT[0:64, c, sl], bank(K, ob)[0:64, :], rd[i][0:64, :], ALU.mult),
                      reads=[f"ps{ob}", f"rd{i}"], writes=[f"oT{c}"])
            else:
                P.add("dve", lambda e: e.tensor_tensor(otmp[i][0:64, :], bank(K, ob)[0:64, :], rd[i][0:64, :], ALU.mult),
                      reads=[f"ps{ob}", f"rd{i}"], writes=[f"otmp{i}"])
                P.add("dve", lambda e: e.tensor_copy(oT[64:128, c, sl], otmp[i][0:64, :]), reads=[f"otmp{i}"], writes=[f"oT{c}"])

    emit_proj(0)
    seq = [(h, qb, k2) for h in range(16) for qb in range(4) for k2 in range(32)]
    pend = emit_S(*seq[0])
    for idx, (h, qb, k2) in enumerate(seq):
        nxt = None
        if idx + 1 < len(seq):
            if seq[idx + 1][1:] == (0, 0) and False:
                pass
            nxt = emit_S(*seq[idx + 1])
        emit_exp_pv(h, qb, k2, pend)
        pend = nxt
        if qb == 1 and k2 == 31 and h + 1 < 16:
            emit_proj(h + 1)
    P.barrier()
    A.reset(m1)
    K.x = A.alloc([8, T], F32)
    load_x(K, din["x1"], 0, dres="x1d")
    proj_residual(K, din["mla_w_o"], oT, lambda kc: f"oT{kc}", mod1, "mod1", 16)
    A.reset(mB)
    hT4 = A.alloc([8, T], BF16)
    skip = A.alloc([2, T], BF16)
    skip2 = A.alloc([8, T], BF16)
    assert A.mark() == K.xmark
    K.x = A.alloc([8, T], F32)
    mX = A.mark()
    alloc_norm_scratch(K)
    for tb in range(4):
        modulate_block(K, lambda c, tb=tb: K.x[:, c, tb * 512:(tb + 1) * 512], lambda c, tb=tb: xres(c, tb), 512, mod1, "mod1", 32, 24,
                       lambda c, tb=tb: hT4[:, c, tb * 512:(tb + 1) * 512], lambda c: f"h4_{c}", 6)
    save_off = A.off
    A.off = K.OTmark
    wr = A.alloc([8, 8], BF16)
    lg = A.alloc([16, 8], F32)
    eq = A.alloc([16, 8], F32)
    l2 = A.alloc([16, 8], F32)
    ex = A.alloc([16, 8], F32)
    comb = A.alloc([16, 8], F32)
    mx1 = A.alloc([16], F32)
    mx2 = A.alloc([16], F32)
    ssum = A.alloc([16], F32)
    onesF = A.alloc([128], F32)
    cm = [A.alloc([128], F32) for _ in range(2)]
    cbc = [A.alloc([T], F32) for _ in range(2)]
    assert A.off <= K.xmark
    A.off = save_off
    P.add("dve", lambda e: e.memset(onesF, 1.0), writes=["onesF"])
    P.add("pool", lambda e: e.dma_start(out=wr, in_=din["moe_w_router"].rearrange("(kc p) n -> p kc n", p=128)), writes=["wr"], key="wr")
    for tt in range(16):
        for kc in range(8):
            mm(K, bank(K, 6)[:, tt * 8:(tt + 1) * 8], hT4[:, kc, tt * 128:(tt + 1) * 128], wr[:, kc, :], kc == 0, kc == 7,
               ["wr", f"h4_{kc}"], ["ps6"])
    lgf = lg.rearrange("p a b -> p (a b)")
    P.add("dve", lambda e: e.tensor_copy(lgf, bank(K, 6)[:, 0:128]), reads=["ps6"], writes=["lg"])
    X = mybir.AxisListType.X

    def bc(v):
        return v.unsqueeze(2).to_broadcast([128, 16, 8])

    P.add("dve", lambda e: e.tensor_reduce(mx1, lg, X, ALU.max), reads=["lg"], writes=["mx1"])
    P.add("dve", lambda e: e.tensor_tensor(eq, lg, bc(mx1), ALU.is_equal), reads=["lg", "mx1"], writes=["eq"])
    P.add("dve", lambda e: e.scalar_tensor_tensor(l2, eq, -1e30, lg, ALU.mult, ALU.add), reads=["eq", "lg"], writes=["l2"])
    P.add("dve", lambda e: e.tensor_reduce(mx2, l2, X, ALU.max), reads=["l2"], writes=["mx2"])
    P.add("dve", lambda e: e.tensor_tensor(eq, lg, bc(mx2), ALU.is_ge), reads=["lg", "mx2", "eq"], writes=["eq"])
    P.add("dve", lambda e: e.tensor_tensor(l2, lg, bc(mx1), ALU.subtract), reads=["lg", "mx1", "l2"], writes=["l2"])
    P.add("act", lambda e: e.activation(ex, l2, AF.Exp), reads=["l2"], writes=["ex"])
    P.add("dve", lambda e: e.tensor_tensor(ex, ex, eq, ALU.mult), reads=["ex", "eq"], writes=["ex"])
    P.add("dve", lambda e: e.tensor_reduce(ssum, ex, X, ALU.add), reads=["ex"], writes=["ssum"])
    P.add("dve", lambda e: e.reciprocal(ssum, ssum), reads=["ssum"], writes=["ssum"])
    P.add("dve", lambda e: e.tensor_tensor(comb, ex, bc(ssum), ALU.mult), reads=["ex", "ssum"], writes=["comb"])

    def make_cbc(ex_):
        buf = cbc[ex_ % 2]
        res = f"cbc{ex_ % 2}"
        for tt in range(16):
            cmb = cm[tt % 2]
            P.add("dve", lambda e, cmb=cmb, tt=tt: e.tensor_scalar_mul(cmb, onesF, comb[:, tt, ex_:ex_ + 1]),
                  reads=["comb", "onesF"], writes=[f"cm{tt % 2}"])
            mm(K, bank(K, 7)[:, (tt % 4) * 128:(tt % 4 + 1) * 128], cmb, K.ident, True, True, [f"cm{tt % 2}", "ident"], ["ps7"])
            if tt % 4 == 3:
                t0 = tt - 3
                P.add("act", lambda e, t0=t0: e.copy(buf[:, t0 * 128:(t0 + 4) * 128], bank(K, 7)), reads=["ps7"], writes=[res])

    gu = din["moe_w_gu"]
    dn = din["moe_w_down"]
    groups = []
    for ex_ in range(NEXP):
        guv = gu[ex_].rearrange("(kc p) n -> p kc n", p=128)
        dnv = dn[ex_].rearrange("(f p) n -> p f n", p=128)
        for gj in range(7):
            def load(K, Wg, Wu, Wd, par, guv=guv, dnv=dnv, gj=gj, ex_=ex_):
                if gj == 0:
                    make_cbc(ex_)
                K.P.add("pool", lambda e: e.dma_start(out=Wg[:, :, 0:256], in_=guv[:, :, gj * 256:(gj + 1) * 256]), writes=[f"Wg{par}"], key=f"Wg{par}")
                K.P.add("pool", lambda e: e.dma_start(out=Wu[:, :, 0:256], in_=guv[:, :, EDIM + gj * 256:EDIM + (gj + 1) * 256]), writes=[f"Wu{par}"], key=f"Wu{par}")
                K.P.add("pool", lambda e: e.dma_start(out=Wd[:, 0:2, :], in_=dnv[:, gj * 2:(gj + 1) * 2, :]), writes=[f"Wd{par}"], key=f"Wd{par}")
            groups.append(dict(load=load, nf=2, ex=ex_))

    def comb_fn(g, tb):
        e_ = g["ex"]
        return cbc[e_ % 2][:, tb * 512:(tb + 1) * 512], f"cbc{e_ % 2}"

    swiglu_stream(K, hT4, lambda kc: f"h4_{kc}", groups, mod1, "mod1", 40, comb_fn=comb_fn)
    P.barrier()
    A.reset(mX)
    alloc_norm_scratch(K)
    K.fscale = A.alloc([8], F32)
    P.add("sp", lambda e: e.dma_start(out=K.fscale, in_=din["final_norm"]), writes=["fscale"], key="fscale")
    ost = [A.alloc([8, 512], F32) for _ in range(2)]
    ov = din["out"].rearrange("(c p) t -> p c t", p=128)
    for tb in range(4):
        ob_ = ost[tb % 2]
        modulate_block(K, lambda c, tb=tb: K.x[:, c, tb * 512:(tb + 1) * 512], lambda c, tb=tb: xres(c, tb), 512, None, None, 0, 0,
                       lambda c, ob_=ob_: ob_[:, c, :], lambda c, tb=tb: f"ost{tb % 2}", 6)
        P.add("sp", lambda e, ob_=ob_, tb=tb: e.dma_start(out=ov[:, :, tb * 512:(tb + 1) * 512], in_=ob_), reads=[f"ost{tb % 2}"], key=f"ost{tb % 2}")


FUSED = False


def build_A():
    nc = bass.Bass("TRN2", target_bir_lowering=False)
    K = Ctx()
    K.nc = nc
    K.P = Prog(nc)
    K.A = Arena(nc, ARENA_WORDS)
    K.ps = nc.alloc_psum_tensor("ps", [128, 4096], F32)

    def inp(name, shape):
        return nc.dram_tensor(name, list(shape), F32, kind="ExternalInput").ap()

    def outp(name, shape):
        return nc.dram_tensor(name, list(shape), F32, kind="ExternalOutput").ap()

    K.din = dict(
        xT=inp("xT", (D, TE)), cT=inp("cT", (128, 8)), w_ada=inp("w_ada", (2, D, 6 * D)), b_ada=inp("b_ada", (2, 128, 48)),
        ident=inp("ident", (128, 128)), na_w_qkv=inp("na_w_qkv", (D, 3 * D)), na_bias=inp("na_bias", (16, 128, NA_NBLK * 64)),
        na_w_o=inp("na_w_o", (D, D)), ffn_w_gu=inp("ffn_w_gu", (D, 2 * FFN)), ffn_w_down=inp("ffn_w_down", (FFN, D)),
        mla_w_down=inp("mla_w_down", (D, 416)), kv_norm=inp("kv_norm", (128, 1)),
        ropeC=inp("ropeC", (128, T)), ropeS=inp("ropeS", (128, T)),
        x1=outp("x1", (D, T)), lat=outp("lat", (160, T)),
    )
    phase_A(K)
    K.P.emit()
    print("A stats", K.P.stats, "arena peak words", K.A.peak)
    return nc


def build_B():
    nc = bass.Bass("TRN2", target_bir_lowering=False)
    K = Ctx()
    K.nc = nc
    K.P = Prog(nc)
    K.A = Arena(nc, ARENA_WORDS)
    K.ps = nc.alloc_psum_tensor("ps", [128, 4096], F32)

    def inp(name, shape):
        return nc.dram_tensor(name, list(shape), F32, kind="ExternalInput").ap()

    def outp(name, shape):
        return nc.dram_tensor(name, list(shape), F32, kind="ExternalOutput").ap()

    K.din = dict(
        x1=inp("x1", (D, T)), latall=inp("latall", (NC * 160, T)), cT=inp("cT", (128, 8)), w_ada=inp("w_ada", (2, D, 6 * D)),
        b_ada=inp("b_ada", (2, 128, 48)), ident=inp("ident", (128, 128)), mla_w_down=inp("mla_w_down", (D, 416)),
        ropeC=inp("ropeC", (128, T)), ropeS=inp("ropeS", (128, T)), bsel=inp("bsel", (128, 2)),
        q_norm=inp("q_norm", (128, 2)), mla_w_uq=inp("mla_w_uq", (256, 1536)), mla_w_ukv=inp("mla_w_ukv", (128, 2048)),
        mla_w_o=inp("mla_w_o", (D, D)), moe_w_router=inp("moe_w_router", (D, 8)), moe_w_gu=inp("moe_w_gu", (NEXP, D, 2 * EDIM)),
        moe_w_down=inp("moe_w_down", (NEXP, EDIM, D)), final_norm=inp("final_norm", (128, 8)),
        out=outp("out", (D, T)),
    )
    phase_B_prologue_unfused(K)
    phase_B(K)
    K.P.emit()
    print("B stats", K.P.stats, "arena peak words", K.A.peak)
    return nc


def build_AB():
    nc = bass.Bass("TRN2", target_bir_lowering=False)
    K = Ctx()
    K.nc = nc
    K.P = Prog(nc)
    K.A = Arena(nc, ARENA_WORDS)
    K.ps = nc.alloc_psum_tensor("ps", [128, 4096], F32)

    def inp(name, shape):
        return nc.dram_tensor(name, list(shape), F32, kind="ExternalInput").ap()

    def outp(name, shape):
        return nc.dram_tensor(name, list(shape), F32, kind="ExternalOutput").ap()

    x1d = nc.dram_tensor("x1d", [D, T], F32)
    latd = nc.dram_tensor("latd", [160, T], F32)
    latall = nc.dram_tensor("latall", [NC * 160, T], F32)
    K.din = dict(
        xT=inp("xT", (D, TE)), cT=inp("cT", (128, 8)), w_ada=inp("w_ada", (2, D, 6 * D)), b_ada=inp("b_ada", (2, 128, 48)),
        ident=inp("ident", (128, 128)), na_w_qkv=inp("na_w_qkv", (D, 3 * D)), na_bias=inp("na_bias", (16, 128, NA_NBLK * 64)),
        na_w_o=inp("na_w_o", (D, D)), ffn_w_gu=inp("ffn_w_gu", (D, 2 * FFN)), ffn_w_down=inp("ffn_w_down", (FFN, D)),
        mla_w_down=inp("mla_w_down", (D, 416)), kv_norm=inp("kv_norm", (128, 1)),
        ropeC=inp("ropeC", (128, T)), ropeS=inp("ropeS", (128, T)), bsel=inp("bsel", (128, 2)),
        q_norm=inp("q_norm", (128, 2)), mla_w_uq=inp("mla_w_uq", (256, 1536)), mla_w_ukv=inp("mla_w_ukv", (128, 2048)),
        mla_w_o=inp("mla_w_o", (D, D)), moe_w_router=inp("moe_w_router", (D, 8)), moe_w_gu=inp("moe_w_gu", (NEXP, D, 2 * EDIM)),
        moe_w_down=inp("moe_w_down", (NEXP, EDIM, D)), final_norm=inp("final_norm", (128, 8)),
        x1=x1d.ap(), lat=latd.ap(), latall=latall.ap(),
        out=outp("out", (D, T)),
    )
    phase_A(K)
    K.P.barrier()
    K.P.add("pool", lambda e: e.collective_compute("AllGather", ALU.bypass, replica_groups=[list(range(NC))],
                                                   ins=[latd.ap().opt()], outs=[latall.ap().opt()]),
            reads=["latd0", "latd1"], writes=["latall"], key="cc", inc=1)
    phase_B(K)
    K.P.emit()
    print("AB stats", K.P.stats, "arena peak words", K.A.peak)
    return nc


def host_common(inputs):
    x = np.asarray(inputs["x"], np.float32)
    c = np.asarray(inputs["c"], np.float32)
    per_core = []
    for core in range(NC):
        b, q = core // 4, core % 4
        r0 = 32 * q - 4
        xe = np.zeros((TE, D), np.float32)
        lo, hi = max(r0, 0), min(r0 + 40, 128)
        xe[(lo - r0) * 64:(hi - r0) * 64] = x[b, lo * 64:hi * 64]
        Cf, Sf = host_rope_tables(q)
        per_core.append(dict(
            xT=np.ascontiguousarray(xe.T),
            cT=np.ascontiguousarray(c[b].reshape(8, 128).T),
            ropeC=Cf, ropeS=Sf,
        ))
    shared = dict(
        w_ada=np.asarray(inputs["w_ada"], np.float32),
        b_ada=np.ascontiguousarray(np.asarray(inputs["b_ada"], np.float32).reshape(2, 48, 128).transpose(0, 2, 1)),
        ident=np.eye(128, dtype=np.float32),
    )
    return per_core, shared


_CACHE = {}


def kernel(**inputs):
    per_core, shared = host_common(inputs)
    rpb = np.asarray(inputs["na_rpb"], np.float32)[0]
    bias_q = [host_na_bias(rpb, q) for q in range(4)]
    if FUSED and "AB" not in _CACHE:
        _CACHE["AB"] = build_AB()
    if not FUSED and "A" not in _CACHE:
        _CACHE["A"] = build_A()
        _CACHE["B"] = build_B()
    f32 = lambda k: np.asarray(inputs[k], np.float32)[0]
    common = dict(
        na_w_qkv=f32("na_w_qkv"), na_w_o=f32("na_w_o"), ffn_w_gu=f32("ffn_w_gu"), ffn_w_down=f32("ffn_w_down"),
        mla_w_down=f32("mla_w_down"), kv_norm=f32("mla_kv_norm").reshape(128, 1),
        q_norm=np.ascontiguousarray(f32("mla_q_norm").reshape(2, 128).T),
        mla_w_uq=f32("mla_w_uq"), mla_w_ukv=f32("mla_w_ukv"), mla_w_o=f32("mla_w_o"), moe_w_router=f32("moe_w_router"),
        moe_w_gu=f32("moe_w_gu"), moe_w_down=f32("moe_w_down"),
        final_norm=np.ascontiguousarray(np.asarray(inputs["final_norm"], np.float32).reshape(8, 128).T),
    )
    in_maps = []
    for core in range(NC):
        b = core // 4
        m = dict(per_core[core])
        m.update(shared)
        m.update(common)
        m["na_bias"] = bias_q[core % 4]
        bs = np.zeros((128, 2), np.float32)
        bs[:, b] = 1.0
        m["bsel"] = bs
        in_maps.append(m)
    if FUSED:
        res = run_bass_kernel_spmd(_CACHE["AB"], in_maps, core_ids=list(range(NC))).results
    else:
        keysA = ("xT", "cT", "w_ada", "b_ada", "ident", "na_w_qkv", "na_bias", "na_w_o", "ffn_w_gu", "ffn_w_down",
                 "mla_w_down", "kv_norm", "ropeC", "ropeS")
        resA = run_bass_kernel_spmd(_CACHE["A"], [{k: m[k] for k in keysA} for m in in_maps], core_ids=list(range(NC))).results
        if inputs.get("_debugA") is not None:
            return resA
        latall = np.ascontiguousarray(np.concatenate([resA[c]["lat"] for c in range(NC)], axis=0))
        keysB = ("cT", "w_ada", "b_ada", "ident", "mla_w_down", "ropeC", "ropeS", "bsel", "q_norm", "mla_w_uq", "mla_w_ukv",
                 "mla_w_o", "moe_w_router", "moe_w_gu", "moe_w_down", "final_norm")
        mapsB = []
        for c in range(NC):
            mb = {k: in_maps[c][k] for k in keysB}
            mb["x1"] = resA[c]["x1"]
            mb["latall"] = latall
            mapsB.append(mb)
        res = run_bass_kernel_spmd(_CACHE["B"], mapsB, core_ids=list(range(NC))).results
    out = np.empty((2, S, D), np.float32)
    for core in range(NC):
        b, q = core // 4, core % 4
        out[b, q * T:(q + 1) * T, :] = res[core]["out"].T
    return out
```

```python
import contextlib
import numpy as np
import concourse.bass as bass
import concourse.mybir as mybir
from concourse.bass_utils import run_bass_kernel_spmd

F32 = mybir.dt.float32
BF16 = mybir.dt.bfloat16
ALU = mybir.AluOpType
AF = mybir.ActivationFunctionType

ENGS = ("pe", "act", "dve", "pool", "sp")
SAME_ENGINE_SYNC = True

D = 1024
T = 2048
TE = 2560
S = 8192
NC = 8
EPS = 1e-6
FFN = 2816
NEXP = 8
EDIM = 1792
NEG = -30000.0
ARENA_WORDS = 50 * 1024


class Prog:
    def __init__(self, nc):
        self.nc = nc
        self.ops = []
        self.last_writer = {}
        self.readers = {}
        self.pending_bar = {}
        self.last_on_eng = {}
        self.last_dma_on_key = {}

    def add(self, eng, fn, reads=(), writes=(), key=None, inc=16):
        i = len(self.ops)
        deps = set()
        for r in reads:
            w = self.last_writer.get(r)
            if w is not None:
                deps.add(w)
        for w_ in writes:
            w = self.last_writer.get(w_)
            if w is not None:
                deps.add(w)
            deps.update(self.readers.get(w_, ()))
        if eng in self.pending_bar:
            deps.update(self.pending_bar.pop(eng))
        deps.discard(i)
        for r in reads:
            self.readers.setdefault(r, []).append(i)
        for w_ in writes:
            self.last_writer[w_] = i
            self.readers[w_] = []
        self.ops.append(dict(eng=eng, fn=fn, deps=deps, key=key, inc=inc))
        self.last_on_eng[eng] = i
        if key is not None:
            self.last_dma_on_key[key] = i
        return i

    def barrier(self):
        b = set(self.last_on_eng.values()) | set(self.last_dma_on_key.values())
        for e in ENGS:
            self.pending_bar[e] = set(b) | self.pending_bar.get(e, set())

    def emit(self, final_wait_eng="sp"):
        nc = self.nc
        ops = self.ops
        n = len(ops)
        fin = set(self.last_on_eng.values()) | set(self.last_dma_on_key.values())
        ops.append(dict(eng=final_wait_eng, fn=None, deps=fin, key=None, inc=0))
        waited_eng = {}
        waited_key = {}
        needed = [False] * (n + 1)
        for i, op in enumerate(ops):
            e = op["eng"]
            best_eng = {}
            best_key = {}
            for d in op["deps"]:
                od = ops[d]
                if od["key"] is not None:
                    k = od["key"]
                    if d > best_key.get(k, -1):
                        best_key[k] = d
                else:
                    se = od["eng"]
                    if se == e and (se == "pe" or not SAME_ENGINE_SYNC):
                        continue
                    if d > best_eng.get(se, -1):
                        best_eng[se] = d
            fdeps = []
            for se, d in best_eng.items():
                if waited_eng.get((e, se), -1) >= d:
                    continue
                waited_eng[(e, se)] = d
                fdeps.append(d)
            for k, d in best_key.items():
                if waited_key.get((e, k), -1) >= d:
                    continue
                waited_key[(e, k)] = d
                fdeps.append(d)
            op["fdeps"] = fdeps
            for d in fdeps:
                needed[d] = True
        seq = {e: 0 for e in ENGS}
        keycnt = {}
        for i, op in enumerate(ops):
            if op["key"] is not None:
                k = op["key"]
                keycnt[k] = keycnt.get(k, 0) + op["inc"]
                op["semval"] = keycnt[k]
            elif needed[i]:
                seq[op["eng"]] += 1
                op["semval"] = seq[op["eng"]]
        self.stats = dict(n_ops=n, n_sig=dict(seq), n_keys=len(keycnt))
        running = {}
        for i, op in enumerate(ops):
            waits = []
            for d in op["fdeps"]:
                od = ops[d]
                if od["key"] is not None:
                    waits.append(("key", od["key"], running[od["key"]]))
                else:
                    waits.append(("eng", od["eng"], od["semval"]))
            op["waits"] = waits
            op["sig"] = needed[i]
            if op["key"] is not None:
                running[op["key"]] = op["semval"]
        sems = {}
        with contextlib.ExitStack() as st:
            for e in ENGS:
                sems[("eng", e)] = st.enter_context(nc.semaphore("s_" + e))
            for k in keycnt:
                sems[("key", k)] = st.enter_context(nc.semaphore("k_" + k))
            block = st.enter_context(nc.Block())
            by_eng = {e: [op for op in ops if op["eng"] == e] for e in ENGS}

            def run(eh, e):
                for op in by_eng[e]:
                    for (kind, name, val) in op["waits"]:
                        eh.wait_ge(sems[(kind, name)], val)
                    if op["fn"] is None:
                        continue
                    ins = op["fn"](eh)
                    if op["key"] is not None:
                        ins.then_inc(sems[("key", op["key"])], op["inc"])
                    elif op["sig"]:
                        ins.then_inc(sems[("eng", e)], 1)

            @block.tensor
            def _(eh):
                run(eh, "pe")

            @block.scalar
            def _(eh):
                run(eh, "act")

            @block.vector
            def _(eh):
                run(eh, "dve")

            @block.gpsimd
            def _(eh):
                run(eh, "pool")

            @block.sync
            def _(eh):
                run(eh, "sp")


class Arena:
    def __init__(self, nc, nwords):
        self.t = nc.alloc_sbuf_tensor("arena", [128, nwords], F32)
        self.n = nwords
        self.off = 0
        self.peak = 0

    def alloc(self, shape, dtype):
        nel = int(np.prod(shape))
        nb = nel * (4 if dtype == F32 else 2)
        nw = (nb + 3) // 4
        nw = (nw + 7) // 8 * 8
        assert self.off + nw <= self.n, f"arena overflow {self.off}+{nw}>{self.n}"
        ap = self.t[:, self.off:self.off + nw]
        self.off += nw
        self.peak = max(self.peak, self.off)
        if dtype != F32:
            ap = ap.bitcast(dtype)
        ap = ap[:, 0:nel]
        if len(shape) == 2:
            ap = ap.rearrange("p (a b) -> p a b", a=shape[0], b=shape[1])
        elif len(shape) == 3:
            ap = ap.rearrange("p (a b c) -> p a b c", a=shape[0], b=shape[1], c=shape[2])
        elif len(shape) == 4:
            ap = ap.rearrange("p (a b c d) -> p a b c d", a=shape[0], b=shape[1], c=shape[2], d=shape[3])
        return ap

    def mark(self):
        return self.off

    def reset(self, m):
        self.off = m


class Ctx:
    pass


def na_tiles(lr):
    if lr < 4:
        return list(range(lr // 2, 6))
    if lr <= 28:
        return list(range(lr // 2, (lr + 7) // 2 + 1))
    return list(range(14, (lr + 7) // 2 + 1))


NA_SETS = [4, 5, 0, 1, 2, 3, 29, 30, 31]


def na_boff(lr):
    offs = {}
    o = 0
    for s in NA_SETS:
        offs[s] = o
        o += len(na_tiles(s))
    if lr < 4 or lr > 28:
        return offs[lr]
    return offs[4] if lr % 2 == 0 else offs[5]


NA_NBLK = sum(len(na_tiles(s)) for s in NA_SETS)


def host_na_bias(rpb, q):
    out = np.empty((16, 128, NA_NBLK * 64), np.float32)
    kk = np.arange(128)
    c = np.arange(64)
    cs = np.clip(c - 8, 0, 48)
    o = 0
    for s in NA_SETS:
        r = 32 * q + s
        rs = min(max(r - 4, 0), 120)
        for t in na_tiles(s):
            kr = 2 * t + kk // 64
            kcol = kk % 64
            krow = 32 * q - 4 + kr
            vrow = (krow >= rs) & (krow < rs + 8)
            vcol = (kcol[:, None] >= cs[None, :]) & (kcol[:, None] < cs[None, :] + 16)
            valid = vrow[:, None] & vcol
            dr = np.clip(krow - r + 7, 0, 14)
            dc = np.clip(kcol[:, None] - c[None, :] + 15, 0, 30)
            vals = rpb[:, dr[:, None], dc]
            out[:, :, o * 64:(o + 1) * 64] = np.where(valid[None], vals, np.float32(NEG))
            o += 1
    return out


def host_rope_tables(q):
    lr = np.arange(32)
    row = (32 * q + lr).astype(np.float32)
    col = np.arange(64).astype(np.float32)
    n = 8
    inv = (np.float32(10000.0) ** (-np.arange(n, dtype=np.float32) / np.float32(n))).astype(np.float32)
    rowang = row[:, None] * inv[None, :]
    colang = col[:, None] * inv[None, :]
    ang = np.zeros((32, 64, 16), np.float32)
    ang[:, :, 0:8] = rowang[:, None, :]
    ang[:, :, 8:16] = colang[None, :, :]
    ang = ang.reshape(2048, 16)
    cos = np.cos(ang).astype(np.float32)
    sin = np.sin(ang).astype(np.float32)
    C = np.repeat(cos.T, 2, axis=0)
    Sn = np.repeat(sin.T, 2, axis=0)
    Cf = np.zeros((128, 2048), np.float32)
    Sf = np.zeros((128, 2048), np.float32)
    for base in (0, 64):
        Cf[base:base + 32] = C
        Sf[base:base + 32] = Sn
    return Cf, Sf


def bank(K, b):
    return K.ps[:, b * 512:(b + 1) * 512]


def mm(K, out, lhsT, rhs, start, stop, reads, writes):
    K.P.add("pe", lambda e: e.matmul(out, lhsT, rhs, start=start, stop=stop), reads=reads, writes=writes)


def setup_consts(K):
    P, A = K.P, K.A
    K.onesD = A.alloc([128], F32)
    K.ones256 = A.alloc([128], F32)
    K.ones128 = A.alloc([128], F32)
    K.ident = A.alloc([128], F32)
    K.onespad = A.alloc([2, 128], BF16)
    P.add("dve", lambda e: e.memset(K.onesD, 1.0 / 1024), writes=["consts"])
    P.add("dve", lambda e: e.memset(K.ones256, 1.0 / 256), writes=["consts"])
    P.add("dve", lambda e: e.memset(K.ones128, 1.0 / 128), writes=["consts"])
    P.add("dve", lambda e: e.memset(K.onespad, 0.0), writes=["consts"])
    P.add("dve", lambda e: e.memset(K.onespad[:, 0, 0:64], 1.0), writes=["consts"])
    P.add("dve", lambda e: e.memset(K.onespad[:, 1, 64:128], 1.0), writes=["consts"])
    P.add("sp", lambda e: e.dma_start(out=K.ident, in_=K.din["ident"]), writes=["ident"], key="ident")
    K.cact = A.alloc([8], F32)
    K.mod = [A.alloc([48], F32), A.alloc([48], F32)]
    K.bada = [A.alloc([48], F32), A.alloc([48], F32)]
    K.norm_ctr = 0


def adaln(K, layers):
    P, A = K.P, K.A
    m0 = A.mark()
    wa = [A.alloc([8, 768], F32), A.alloc([8, 768], F32)]
    P.add("sp", lambda e: e.dma_start(out=K.cact, in_=K.din["cT"]), writes=["cact"], key="cact")
    P.add("act", lambda e: e.activation(K.cact, K.cact, AF.Silu), reads=["cact"], writes=["cact"])
    cnt = 0
    for li in layers:
        P.add("sp", lambda e, li=li: e.dma_start(out=K.bada[li], in_=K.din["b_ada"][li]), writes=[f"bada{li}"], key=f"bada{li}")
        wv = K.din["w_ada"][li].rearrange("(kc p) n -> p kc n", p=128)
        for jb in range(8):
            buf = wa[cnt % 2]
            res = f"wa{cnt % 2}"
            cnt += 1
            P.add("sp", lambda e, buf=buf, jb=jb, wv=wv: e.dma_start(out=buf, in_=wv[:, :, jb * 768:(jb + 1) * 768]),
                  writes=[res], key=res)
            for j in range(6):
                col = jb * 6 + j
                for kc in range(8):
                    mm(K, bank(K, 0)[:, col:col + 1], buf[:, kc, j * 128:(j + 1) * 128], K.cact[:, kc:kc + 1],
                       kc == 0, kc == 7, [res, "cact"], ["ps0"])
        mod = K.mod[li]
        P.add("dve", lambda e, mod=mod, li=li: e.tensor_tensor(mod, bank(K, 0)[:, 0:48], K.bada[li], ALU.add),
              reads=["ps0", f"bada{li}"], writes=[f"mod{li}"])
        P.add("dve", lambda e, mod=mod: e.tensor_scalar_add(mod[:, 8:16], mod[:, 8:16], 1.0), reads=[f"mod{li}"], writes=[f"mod{li}"])
        P.add("dve", lambda e, mod=mod: e.tensor_scalar_add(mod[:, 32:40], mod[:, 32:40], 1.0), reads=[f"mod{li}"], writes=[f"mod{li}"])
    P.barrier()
    A.reset(m0)


def alloc_norm_scratch(K):
    A = K.A
    K.sq = [A.alloc([512], F32), A.alloc([512], F32)]
    K.rs = [A.alloc([512], F32), A.alloc([512], F32)]
    K.ntmp = [A.alloc([512], F32), A.alloc([512], F32)]


def modulate_block(K, src_fn, src_res, n, mod, modres, sc0, sh0, dst_fn, dst_res_fn, nb0):
    P = K.P
    i = K.norm_ctr
    K.norm_ctr += 1
    psb = nb0 + (i % 2)
    for c in range(8):
        sq = K.sq[c % 2]
        P.add("pool", lambda e, sq=sq, c=c: e.tensor_tensor(sq[:, :n], src_fn(c), src_fn(c), ALU.mult),
              reads=[src_res(c)], writes=[f"sq{c % 2}"])
        mm(K, bank(K, psb)[:, :n], K.onesD, sq[:, :n], c == 0, c == 7, [f"sq{c % 2}", "consts"], [f"ps{psb}"])
    rs = K.rs[i % 2]
    rr = f"rs{i % 2}"
    P.add("act", lambda e: e.activation(rs[:, :n], bank(K, psb)[:, :n], AF.Sqrt, bias=EPS, scale=1.0),
          reads=[f"ps{psb}"], writes=[rr])
    P.add("dve", lambda e: e.reciprocal(rs[:, :n], rs[:, :n]), reads=[rr], writes=[rr])
    for c in range(8):
        if mod is not None:
            tmp = K.ntmp[c % 2]
            tr = f"ntmp{c % 2}"
            P.add("dve", lambda e, c=c, tmp=tmp: e.scalar_tensor_tensor(tmp[:, :n], src_fn(c), mod[:, sc0 + c:sc0 + c + 1], rs[:, :n],
                                                                        ALU.mult, ALU.mult),
                  reads=[src_res(c), rr, modres], writes=[tr])
            P.add("act", lambda e, c=c, tmp=tmp: e.activation(dst_fn(c), tmp[:, :n], AF.Identity, bias=mod[:, sh0 + c:sh0 + c + 1], scale=1.0),
                  reads=[tr, modres], writes=[dst_res_fn(c)])
        else:
            P.add("dve", lambda e, c=c: e.scalar_tensor_tensor(dst_fn(c), src_fn(c), K.fscale[:, c:c + 1], rs[:, :n],
                                                               ALU.mult, ALU.mult),
                  reads=[src_res(c), rr, "fscale"], writes=[dst_res_fn(c)])


def load_x(K, src_dram, tok0, dres=None):
    P = K.P
    v = src_dram.rearrange("(c p) t -> p c t", p=128)
    for c in range(8):
        P.add("sp", lambda e, c=c: e.dma_start(out=K.x[:, c, :], in_=v[:, c, tok0:tok0 + T]),
              reads=([f"{dres}{c}"] if dres else []), writes=[f"x{c}_{tb}" for tb in range(4)], key=f"x{c}")


def xres(c, tb):
    return f"x{c}_{tb}"


def proj_residual(K, w_dram, inT, in_res, mod, modres, g0):
    P, A = K.P, K.A
    m0 = A.mark()
    wo = A.alloc([8, 1024], BF16)
    wv = w_dram.rearrange("(kc p) n -> p kc n", p=128)
    for hf in range(2):
        P.add("pool", lambda e, hf=hf: e.dma_start(out=wo[:, hf * 4:(hf + 1) * 4, :], in_=wv[:, hf * 4:(hf + 1) * 4, :]),
              writes=[f"wo{hf}"], key=f"wo{hf}")
    k = 0
    for tb in range(4):
        for oc in range(8):
            b = 4 + (k % 2)
            k += 1
            for kc in range(8):
                mm(K, bank(K, b), wo[:, kc, oc * 128:(oc + 1) * 128], inT[:, kc, tb * 512:(tb + 1) * 512],
                   kc == 0, kc == 7, [f"wo{kc // 4}", in_res(kc)], [f"ps{b}"])
            P.add("dve", lambda e, b=b, oc=oc, tb=tb: e.scalar_tensor_tensor(
                K.x[:, oc, tb * 512:(tb + 1) * 512], bank(K, b), mod[:, g0 + oc:g0 + oc + 1],
                K.x[:, oc, tb * 512:(tb + 1) * 512], ALU.mult, ALU.add),
                reads=[f"ps{b}", xres(oc, tb), modres], writes=[xres(oc, tb)])
    P.barrier()
    A.reset(m0)


def swiglu_stream(K, hT, hres, groups, mod, modres, g0, comb_fn=None):
    P, A = K.P, K.A
    NF = max(g["nf"] for g in groups)
    Wg = [A.alloc([8, NF * 128], BF16) for _ in range(2)]
    Wu = [A.alloc([8, NF * 128], BF16) for _ in range(2)]
    Wd = [A.alloc([NF, 1024], BF16) for _ in range(2)]
    act = [A.alloc([NF, 512], BF16) for _ in range(2)]
    t1 = [A.alloc([512], BF16) for _ in range(2)]
    t2 = [A.alloc([512], BF16) for _ in range(2)]
    steps = [(gi, tb) for gi in range(len(groups)) for tb in range(4)]
    loaded = set()
    ctr = dict(gu=0, y=0)

    def ensure_loaded(gi):
        if gi in loaded or gi >= len(groups):
            return
        loaded.add(gi)
        par = gi % 2
        groups[gi]["load"](K, Wg[par], Wu[par], Wd[par], par)

    def emit_gu(si):
        gi, tb = steps[si]
        g = groups[gi]
        par = gi % 2
        assert gi in loaded
        ab = act[si % 2]
        ar = f"act{si % 2}"
        comb = comb_fn(g, tb) if comb_fn is not None else None
        for fi in range(g["nf"]):
            k = ctr["gu"]
            ctr["gu"] += 1
            bg = 0 + (k % 2)
            bu = 2 + (k % 2)
            for kc in range(8):
                mm(K, bank(K, bg), Wg[par][:, kc, fi * 128:(fi + 1) * 128], hT[:, kc, tb * 512:(tb + 1) * 512],
                   kc == 0, kc == 7, [f"Wg{par}", hres(kc)], [f"ps{bg}"])
            for kc in range(8):
                mm(K, bank(K, bu), Wu[par][:, kc, fi * 128:(fi + 1) * 128], hT[:, kc, tb * 512:(tb + 1) * 512],
                   kc == 0, kc == 7, [f"Wu{par}", hres(kc)], [f"ps{bu}"])
            tt = t1[k % 2]
            tr = f"t1_{k % 2}"
            P.add("act", lambda e, tt=tt, bg=bg: e.activation(tt, bank(K, bg), AF.Silu), reads=[f"ps{bg}"], writes=[tr])
            src, sr = tt, tr
            if comb is not None:
                cap, cres = comb
                t2b = t2[k % 2]
                t2r = f"t2_{k % 2}"
                P.add("pool", lambda e, t2b=t2b, tt=tt, cap=cap: e.tensor_tensor(t2b, tt, cap, ALU.mult),
                      reads=[tr, cres], writes=[t2r])
                src, sr = t2b, t2r
            P.add("dve", lambda e, ab=ab, fi=fi, src=src, bu=bu: e.tensor_tensor(ab[:, fi, :], src, bank(K, bu), ALU.mult),
                  reads=[sr, f"ps{bu}"], writes=[ar])

    def emit_down(si):
        gi, tb = steps[si]
        g = groups[gi]
        par = gi % 2
        ab = act[si % 2]
        ar = f"act{si % 2}"
        nf = g["nf"]
        for oc in range(8):
            k = ctr["y"]
            ctr["y"] += 1
            b = 4 + (k % 4)
            for fi in range(nf):
                mm(K, bank(K, b), Wd[par][:, fi, oc * 128:(oc + 1) * 128], ab[:, fi, :], fi == 0, fi == nf - 1,
                   [f"Wd{par}", ar], [f"ps{b}"])
            P.add("dve", lambda e, b=b, oc=oc, tb=tb: e.scalar_tensor_tensor(
                K.x[:, oc, tb * 512:(tb + 1) * 512], bank(K, b), mod[:, g0 + oc:g0 + oc + 1],
                K.x[:, oc, tb * 512:(tb + 1) * 512], ALU.mult, ALU.add),
                reads=[f"ps{b}", xres(oc, tb), modres], writes=[xres(oc, tb)])

    ensure_loaded(0)
    ensure_loaded(1)
    emit_gu(0)
    for si in range(len(steps)):
        if si + 1 < len(steps):
            emit_gu(si + 1)
        emit_down(si)
        if steps[si][1] == 3:
            ensure_loaded(steps[si][0] + 2)


def phase_A(K):
    P, A, din = K.P, K.A, K.din
    setup_consts(K)
    adaln(K, [0, 1])
    mod0, mod1 = K.mod
    mA = A.mark()
    hT = A.alloc([8, TE], BF16)
    OT = A.alloc([8, T], BF16)
    m1 = A.mark()
    alloc_norm_scratch(K)
    xs = [A.alloc([8, 512], F32), A.alloc([8, 512], F32)]
    xv = din["xT"].rearrange("(c p) t -> p c t", p=128)

    def hres(c, lo, hi):
        return [f"h{c}_{tb}" for tb in range(lo // 512, (hi - 1) // 512 + 1)]

    for tb in range(5):
        buf = xs[tb % 2]
        P.add("sp", lambda e, buf=buf, tb=tb: e.dma_start(out=buf, in_=xv[:, :, tb * 512:(tb + 1) * 512]),
              writes=[f"xs{tb % 2}"], key=f"xs{tb % 2}")
        modulate_block(K, lambda c, buf=buf: buf[:, c, :], lambda c, tb=tb: f"xs{tb % 2}", 512, mod0, "mod0", 8, 0,
                       lambda c, tb=tb: hT[:, c, tb * 512:(tb + 1) * 512], lambda c, tb=tb: f"h{c}_{tb}", 6)
    P.barrier()
    A.reset(m1)
    wqkv = [A.alloc([3, 8, 128], BF16) for _ in range(2)]
    qT = [A.alloc([T], BF16) for _ in range(2)]
    kT = [A.alloc([TE], BF16) for _ in range(2)]
    Vp = [A.alloc([20, 2, 128], BF16) for _ in range(2)]
    biasb = [A.alloc([NA_NBLK * 64], F32) for _ in range(2)]
    stmp = [A.alloc([384], F32) for _ in range(3)]
    pT = [A.alloc([384], BF16) for _ in range(3)]
    rden = [A.alloc([512], F32) for _ in range(2)]
    for par in range(2):
        P.add("pool", lambda e, par=par: e.memset(Vp[par], 0.0), writes=[f"Vp{par}"])
    wv = din["na_w_qkv"].rearrange("(kc p) n -> p kc n", p=128)
    nm = ["wq", "wk", "wv"]
    PB = 7
    for p in range(8):
        par = p % 2
        for s in range(3):
            P.add("pool", lambda e, par=par, s=s, p=p: e.dma_start(out=wqkv[par][:, s], in_=wv[:, :, s * 1024 + p * 128:s * 1024 + (p + 1) * 128]),
                  writes=[f"{nm[s]}{par}"], key=f"{nm[s]}{par}")
        for h2 in range(2):
            h = 2 * p + h2
            P.add("sp", lambda e, h2=h2, h=h: e.dma_start(out=biasb[h2], in_=din["na_bias"][h]), writes=[f"bias{h2}"], key=f"bias{h2}")
        for tb in range(4):
            lo = 256 + tb * 512
            for kc in range(8):
                mm(K, bank(K, PB), wqkv[par][:, 0, kc, :], hT[:, kc, lo:lo + 512], kc == 0, kc == 7,
                   [f"wq{par}"] + hres(kc, lo, lo + 512), [f"ps{PB}"])
            P.add("act", lambda e, par=par, tb=tb: e.copy(qT[par][:, tb * 512:(tb + 1) * 512], bank(K, PB)),
                  reads=[f"ps{PB}"], writes=[f"qT{par}"])
        for tb in range(5):
            lo = tb * 512
            for kc in range(8):
                mm(K, bank(K, PB), wqkv[par][:, 1, kc, :], hT[:, kc, lo:lo + 512], kc == 0, kc == 7,
                   [f"wk{par}"] + hres(kc, lo, lo + 512), [f"ps{PB}"])
            P.add("act", lambda e, par=par, tb=tb: e.copy(kT[par][:, tb * 512:(tb + 1) * 512], bank(K, PB)),
                  reads=[f"ps{PB}"], writes=[f"kT{par}"])
        for g in range(5):
            for j in range(4):
                t = g * 4 + j
                for kc in range(8):
                    mm(K, bank(K, PB)[:, j * 128:(j + 1) * 128], hT[:, kc, t * 128:(t + 1) * 128], wqkv[par][:, 2, kc, :],
                       kc == 0, kc == 7, [f"wv{par}"] + hres(kc, t * 128, (t + 1) * 128), [f"ps{PB}"])
            pv = bank(K, PB).rearrange("p (t c) -> p t c", c=128)
            P.add("dve", lambda e, par=par, g=g, pv=pv: e.tensor_copy(Vp[par][:, g * 4:(g + 1) * 4, 0, 0:64], pv[:, :, 0:64]),
                  reads=[f"ps{PB}"], writes=[f"Vp{par}"])
            P.add("dve", lambda e, par=par, g=g, pv=pv: e.tensor_copy(Vp[par][:, g * 4:(g + 1) * 4, 1, 64:128], pv[:, :, 64:128]),
                  reads=[f"ps{PB}"], writes=[f"Vp{par}"])
        steps = [(lr, h2) for lr in range(32) for h2 in range(2)]

        def emit_S(si, par=par):
            lr, h2 = steps[si]
            sb = si % 3
            for j, t in enumerate(na_tiles(lr)):
                mm(K, bank(K, sb)[:, j * 64:(j + 1) * 64], kT[par][h2 * 64:(h2 + 1) * 64, t * 128:(t + 1) * 128],
                   qT[par][h2 * 64:(h2 + 1) * 64, lr * 64:(lr + 1) * 64], True, True, [f"kT{par}", f"qT{par}"], [f"ps{sb}"])

        def emit_soft(si):
            lr, h2 = steps[si]
            sb = si % 3
            nt = len(na_tiles(lr))
            bo = na_boff(lr)
            tmp = stmp[si % 3]
            pt = pT[si % 3]
            P.add("dve", lambda e: e.scalar_tensor_tensor(tmp[:, :nt * 64], bank(K, sb)[:, :nt * 64], 0.125,
                                                          biasb[h2][:, bo * 64:(bo + nt) * 64], ALU.mult, ALU.add),
                  reads=[f"ps{sb}", f"bias{h2}"], writes=[f"stmp{si % 3}"])
            P.add("act", lambda e: e.activation(pt[:, :nt * 64], tmp[:, :nt * 64], AF.Exp),
                  reads=[f"stmp{si % 3}"], writes=[f"pT{si % 3}"])

        def emit_PV(si, par=par, p=p):
            lr, h2 = steps[si]
            grp, slot = lr // 8, lr % 8
            ob = 3 + (grp % 2)
            db = 5 + (grp % 2)
            pt = pT[si % 3]
            tiles = na_tiles(lr)
            for j, t in enumerate(tiles):
                first = (h2 == 0 and j == 0)
                last = (h2 == 1 and j == len(tiles) - 1)
                mm(K, bank(K, ob)[:, slot * 64:(slot + 1) * 64], Vp[par][:, t, h2, :], pt[:, j * 64:(j + 1) * 64],
                   first, last, [f"Vp{par}", f"pT{si % 3}"], [f"ps{ob}"])
                mm(K, bank(K, db)[:, slot * 64:(slot + 1) * 64], K.onespad[:, h2, :], pt[:, j * 64:(j + 1) * 64],
                   first, last, ["consts", f"pT{si % 3}"], [f"ps{db}"])
            if slot == 7 and h2 == 1:
                rd = rden[0]
                P.add("dve", lambda e: e.reciprocal(rd, bank(K, db)), reads=[f"ps{db}"], writes=["rden"])
                P.add("dve", lambda e: e.tensor_tensor(OT[:, p, grp * 512:(grp + 1) * 512], bank(K, ob), rd, ALU.mult),
                      reads=[f"ps{ob}", "rden"], writes=[f"OT{p}"])

        emit_S(0)
        emit_S(1)
        for si in range(len(steps)):
            emit_soft(si)
            if si + 2 < len(steps):
                emit_S(si + 2)
            emit_PV(si)
    P.barrier()
    A.reset(mA)
    OT2 = A.alloc([8, TE], BF16)
    OT = A.alloc([8, T], BF16)
    K.x = A.alloc([8, T], F32)
    load_x(K, din["xT"], 256)
    proj_residual(K, din["na_w_o"], OT, lambda kc: f"OT{kc}", mod0, "mod0", 16)
    A.reset(mA)
    hT2 = A.alloc([8, T], BF16)
    skip = A.alloc([8, TE - T], BF16)
    skip2 = A.alloc([8, T], BF16)
    K.x = A.alloc([8, T], F32)
    alloc_norm_scratch(K)
    for tb in range(4):
        modulate_block(K, lambda c, tb=tb: K.x[:, c, tb * 512:(tb + 1) * 512], lambda c, tb=tb: xres(c, tb), 512, mod0, "mod0", 32, 24,
                       lambda c, tb=tb: hT2[:, c, tb * 512:(tb + 1) * 512], lambda c: f"h2_{c}", 6)
    gu = din["ffn_w_gu"].rearrange("(kc p) n -> p kc n", p=128)
    dn = din["ffn_w_down"].rearrange("(f p) n -> p f n", p=128)
    groups = []
    for gi in range(11):
        def load(K, Wg, Wu, Wd, par, gi=gi):
            K.P.add("pool", lambda e: e.dma_start(out=Wg[:, :, 0:256], in_=gu[:, :, gi * 256:(gi + 1) * 256]), writes=[f"Wg{par}"], key=f"Wg{par}")
            K.P.add("pool", lambda e: e.dma_start(out=Wu[:, :, 0:256], in_=gu[:, :, FFN + gi * 256:FFN + (gi + 1) * 256]), writes=[f"Wu{par}"], key=f"Wu{par}")
            K.P.add("pool", lambda e: e.dma_start(out=Wd[:, 0:2, :], in_=dn[:, gi * 2:(gi + 1) * 2, :]), writes=[f"Wd{par}"], key=f"Wd{par}")
        groups.append(dict(load=load, nf=2))
    swiglu_stream(K, hT2, lambda kc: f"h2_{kc}", groups, mod0, "mod0", 40)
    P.barrier()
    A.reset(mA)
    K.mA = mA
    hT3 = A.alloc([8, T], BF16)
    K.hT3 = hT3
    K.cq_region = A.alloc([2, T], BF16)
    K.OTmark = A.mark()
    K.OTreg = A.alloc([8, T], BF16)
    K.xmark = A.mark()
    K.x = A.alloc([8, T], F32)
    alloc_norm_scratch(K)
    for tb in range(4):
        modulate_block(K, lambda c, tb=tb: K.x[:, c, tb * 512:(tb + 1) * 512], lambda c, tb=tb: xres(c, tb), 512, mod1, "mod1", 8, 0,
                       lambda c, tb=tb: hT3[:, c, tb * 512:(tb + 1) * 512], lambda c: f"h3_{c}", 6)
    xo = din["x1"].rearrange("(c p) t -> p c t", p=128)
    for c in range(8):
        P.add("sp", lambda e, c=c: e.dma_start(out=xo[:, c, :], in_=K.x[:, c, :]), reads=[xres(c, tb) for tb in range(4)],
              writes=[f"x1d{c}"], key=f"x{c}")
    mla_latent(K, hT3, lambda kc: f"h3_{kc}")


def mla_latent(K, hT3, hres):
    P, A, din = K.P, K.A, K.din
    wd = A.alloc([8, 160], BF16)
    wsw = A.alloc([8, 32], BF16)
    kvn = A.alloc([1], F32)
    ropeC = A.alloc([T], F32)
    ropeS = A.alloc([T], F32)
    dkv = A.alloc([512], F32)
    sqb = A.alloc([512], F32)
    rsb = A.alloc([512], F32)
    lat = A.alloc([T], F32)
    krot = A.alloc([T], F32)
    ktmp = A.alloc([512], F32)
    wv = din["mla_w_down"].rearrange("(kc p) n -> p kc n", p=128)
    P.add("pool", lambda e: e.dma_start(out=wd, in_=wv[:, :, 256:416]), writes=["wd"], key="wd")
    P.add("sp", lambda e: e.dma_start(out=kvn, in_=din["kv_norm"]), writes=["kvn"], key="kvn")
    P.add("sp", lambda e: e.dma_start(out=ropeC, in_=din["ropeC"]), writes=["ropeC"], key="ropeC")
    P.add("sp", lambda e: e.dma_start(out=ropeS, in_=din["ropeS"]), writes=["ropeS"], key="ropeS")
    wr = wd[:, :, 128:160].rearrange("p k (i two) -> p k i two", two=2)
    ws = wsw.rearrange("p k (i two) -> p k i two", two=2)
    P.add("dve", lambda e: e.tensor_scalar_mul(ws[:, :, :, 0], wr[:, :, :, 1], -1.0), reads=["wd"], writes=["wsw"])
    P.add("dve", lambda e: e.tensor_copy(ws[:, :, :, 1], wr[:, :, :, 0]), reads=["wd"], writes=["wsw"])
    for tb in range(4):
        sl = slice(tb * 512, (tb + 1) * 512)
        for kc in range(8):
            mm(K, bank(K, 0), wd[:, kc, 0:128], hT3[:, kc, sl], kc == 0, kc == 7, ["wd", hres(kc)], ["ps0"])
        for kc in range(8):
            mm(K, bank(K, 1)[0:32, :], wd[:, kc, 128:160], hT3[:, kc, sl], kc == 0, kc == 7, ["wd", hres(kc)], ["ps1"])
        for kc in range(8):
            mm(K, bank(K, 2)[0:32, :], wsw[:, kc, :], hT3[:, kc, sl], kc == 0, kc == 7, ["wsw", hres(kc)], ["ps2"])
        P.add("act", lambda e: e.copy(dkv, bank(K, 0)), reads=["ps0"], writes=["dkv"])
        P.add("pool", lambda e: e.tensor_tensor(sqb, dkv, dkv, ALU.mult), reads=["dkv"], writes=["sqb"])
        mm(K, bank(K, 3), K.ones128, sqb, True, True, ["sqb", "consts"], ["ps3"])
        P.add("act", lambda e: e.activation(rsb, bank(K, 3), AF.Sqrt, bias=EPS, scale=1.0), reads=["ps3"], writes=["rsb"])
        P.add("dve", lambda e: e.reciprocal(rsb, rsb), reads=["rsb"], writes=["rsb"])
        P.add("dve", lambda e, sl=sl: e.scalar_tensor_tensor(lat[:, sl], dkv, kvn[:, 0:1], rsb, ALU.mult, ALU.mult),
              reads=["dkv", "rsb", "kvn"], writes=["lat"])
        P.add("dve", lambda e, sl=sl: e.tensor_tensor(krot[0:32, sl], bank(K, 1)[0:32, :], ropeC[0:32, sl], ALU.mult),
              reads=["ps1", "ropeC"], writes=["krot"])
        P.add("dve", lambda e, sl=sl: e.tensor_tensor(ktmp[0:32, :], bank(K, 2)[0:32, :], ropeS[0:32, sl], ALU.mult),
              reads=["ps2", "ropeS"], writes=["ktmp"])
        P.add("pool", lambda e, sl=sl: e.tensor_tensor(krot[0:32, sl], krot[0:32, sl], ktmp[0:32, :], ALU.add),
              reads=["krot", "ktmp"], writes=["krot"])
    P.add("sp", lambda e: e.dma_start(out=din["lat"][0:128, :], in_=lat), reads=["lat"], writes=["latd0"], key="lat")
    P.add("sp", lambda e: e.dma_start(out=din["lat"][128:160, :], in_=krot[0:32, :]), reads=["krot"], writes=["latd1"], key="krot")


def phase_B_prologue_unfused(K):
    P, A, din = K.P, K.A, K.din
    setup_consts(K)
    adaln(K, [1])
    mA = A.mark()
    K.mA = mA
    K.hT3 = A.alloc([8, T], BF16)
    K.cq_region = A.alloc([2, T], BF16)
    K.OTmark = A.mark()
    K.OTreg = A.alloc([8, T], BF16)
    K.xmark = A.mark()
    K.x = A.alloc([8, T], F32)
    load_x(K, din["x1"], 0)
    alloc_norm_scratch(K)
    for tb in range(4):
        modulate_block(K, lambda c, tb=tb: K.x[:, c, tb * 512:(tb + 1) * 512], lambda c, tb=tb: xres(c, tb), 512, K.mod[1], "mod1", 8, 0,
                       lambda c, tb=tb: K.hT3[:, c, tb * 512:(tb + 1) * 512], lambda c: f"h3_{c}", 6)
    P.barrier()


def phase_B(K):
    P, A, din = K.P, K.A, K.din
    mod1 = K.mod[1]
    mB = K.mA
    oT = K.OTreg
    cqT = K.cq_region
    hT3 = K.hT3
    m1 = K.xmark
    A.reset(m1)
    wdq = A.alloc([8, 256], BF16)
    qn = A.alloc([2], F32)
    dq = [A.alloc([512], F32), A.alloc([512], F32)]
    sqq = [A.alloc([512], F32), A.alloc([512], F32)]
    rsq = A.alloc([512], F32)
    wv = din["mla_w_down"].rearrange("(kc p) n -> p kc n", p=128)
    P.add("pool", lambda e: e.dma_start(out=wdq, in_=wv[:, :, 0:256]), writes=["wdq"], key="wdq")
    P.add("sp", lambda e: e.dma_start(out=qn, in_=din["q_norm"]), writes=["qn"], key="qn")
    for tb in range(4):
        sl = slice(tb * 512, (tb + 1) * 512)
        for j in range(2):
            for kc in range(8):
                mm(K, bank(K, j), wdq[:, kc, j * 128:(j + 1) * 128], hT3[:, kc, sl], kc == 0, kc == 7, ["wdq", f"h3_{kc}"], [f"ps{j}"])
            P.add("act", lambda e, j=j: e.copy(dq[j], bank(K, j)), reads=[f"ps{j}"], writes=[f"dq{j}"])
            P.add("pool", lambda e, j=j: e.tensor_tensor(sqq[j], dq[j], dq[j], ALU.mult), reads=[f"dq{j}"], writes=[f"sqq{j}"])
            mm(K, bank(K, 2), K.ones256, sqq[j], j == 0, j == 1, [f"sqq{j}", "consts"], ["ps2"])
        P.add("act", lambda e: e.activation(rsq, bank(K, 2), AF.Sqrt, bias=EPS, scale=1.0), reads=["ps2"], writes=["rsq"])
        P.add("dve", lambda e: e.reciprocal(rsq, rsq), reads=["rsq"], writes=["rsq"])
        for j in range(2):
            P.add("dve", lambda e, j=j, sl=sl: e.scalar_tensor_tensor(cqT[:, j, sl], dq[j], qn[:, j:j + 1], rsq, ALU.mult, ALU.mult),
                  reads=[f"dq{j}", "rsq", "qn"], writes=["cqT"])
    P.barrier()
    A.reset(m1)
    save_off = A.off
    A.off = K.mA
    ckvT = A.alloc([S], BF16)
    kh0 = A.alloc([S], BF16)
    A.off = save_off
    khT = [kh0, A.alloc([S], BF16)]
    Vh = [A.alloc([64, 128], BF16) for _ in range(2)]
    qhT = [A.alloc([T], BF16) for _ in range(2)]
    wuq = A.alloc([2, 1536], BF16)
    wuqsw = A.alloc([2, 16, 96], BF16)
    wukv = A.alloc([2048], BF16)
    ropeC = A.alloc([T], F32)
    ropeS = A.alloc([T], F32)
    pT = [A.alloc([1024], BF16) for _ in range(3)]
    rt1 = [A.alloc([512], F32) for _ in range(2)]
    rt2 = [A.alloc([512], F32) for _ in range(2)]
    rd = [A.alloc([512], F32) for _ in range(2)]
    otmp = [A.alloc([512], BF16) for _ in range(2)]
    la = din["latall"]
    bsel = A.alloc([2], F32)
    P.add("sp", lambda e: e.dma_start(out=bsel, in_=din["bsel"]), writes=["bsel"], key="bsel")
    CH = 512
    stA = [A.alloc([CH], F32) for _ in range(2)]
    stB = [A.alloc([CH], F32) for _ in range(2)]
    for par in range(2):
        P.add("pool", lambda e, par=par: e.memset(Vh[par][:, :, 64:128], 1.0), writes=[f"Vh1_{par}"])
    nch = T // CH
    it = 0
    for i in range(4):
        for j in range(nch):
            sa, sb_ = stA[it % 2], stB[it % 2]
            ra, rb = f"stA{it % 2}", f"stB{it % 2}"
            it += 1
            r0, r1 = i * 160, (4 + i) * 160
            cs = slice(j * CH, (j + 1) * CH)
            ks = slice(i * T + j * CH, i * T + (j + 1) * CH)
            P.add("sp", lambda e, sa=sa, r0=r0, cs=cs: e.dma_start(out=sa[0:128, :], in_=la[r0:r0 + 128, cs]), reads=["latall"], writes=[ra], key=ra)
            P.add("sp", lambda e, sb_=sb_, r1=r1, cs=cs: e.dma_start(out=sb_[0:128, :], in_=la[r1:r1 + 128, cs]), reads=["latall"], writes=[rb], key=rb)
            P.add("dve", lambda e, sa=sa: e.tensor_scalar_mul(sa, sa, bsel[:, 0:1]), reads=[ra, "bsel"], writes=[ra])
            P.add("dve", lambda e, sa=sa, sb_=sb_, ks=ks: e.scalar_tensor_tensor(ckvT[:, ks], sb_, bsel[:, 1:2], sa, ALU.mult, ALU.add),
                  reads=[ra, rb, "bsel"], writes=[f"ckv{i}"])
    for i in range(4):
        for j in range(nch):
            sa, sb_ = stA[it % 2], stB[it % 2]
            ra, rb = f"stA{it % 2}", f"stB{it % 2}"
            it += 1
            r0, r1 = i * 160 + 128, (4 + i) * 160 + 128
            cs = slice(j * CH, (j + 1) * CH)
            ks = slice(i * T + j * CH, i * T + (j + 1) * CH)
            P.add("sp", lambda e, sa=sa, r0=r0, cs=cs: e.dma_start(out=sa[64:96, :], in_=la[r0:r0 + 32, cs]), reads=["latall"], writes=[ra], key=ra)
            P.add("sp", lambda e, sb_=sb_, r1=r1, cs=cs: e.dma_start(out=sb_[64:96, :], in_=la[r1:r1 + 32, cs]), reads=["latall"], writes=[rb], key=rb)
            P.add("pool", lambda e, sa=sa: e.tensor_scalar_mul(sa[64:96, :], sa[64:96, :], bsel[64:96, 0:1]), reads=[ra, "bsel"], writes=[ra])
            for par in range(2):
                P.add("dve", lambda e, sa=sa, sb_=sb_, ks=ks, par=par: e.scalar_tensor_tensor(
                    khT[par][64:96, ks], sb_[64:96, :], bsel[64:96, 1:2], sa[64:96, :], ALU.mult, ALU.add),
                    reads=[ra, rb, "bsel"], writes=[f"khr{par}"])
    uq = din["mla_w_uq"].rearrange("(kc p) n -> p kc n", p=128)
    P.add("pool", lambda e: e.dma_start(out=wuq, in_=uq), writes=["wuq"], key="wuq")
    P.add("pool", lambda e: e.dma_start(out=wukv, in_=din["mla_w_ukv"]), writes=["wukv"], key="wukv")
    P.add("sp", lambda e: e.dma_start(out=ropeC, in_=din["ropeC"]), writes=["ropeC"], key="ropeC")
    P.add("sp", lambda e: e.dma_start(out=ropeS, in_=din["ropeS"]), writes=["ropeS"], key="ropeS")
    P.add("pool", lambda e: e.memset(wuqsw, 0.0), writes=["wuqsw"])
    wq4 = wuq.rearrange("p k (h f) -> p k h f", h=16)
    src = wq4[:, :, :, 64:96].rearrange("p k h (i two) -> p k h i two", two=2)
    dst = wuqsw[:, :, :, 64:96].rearrange("p k h (i two) -> p k h i two", two=2)
    for kc in range(2):
        P.add("dve", lambda e, kc=kc: e.tensor_scalar_mul(dst[:, kc, :, :, 0], src[:, kc, :, :, 1], -1.0), reads=["wuq", "wuqsw"], writes=["wuqsw"])
        P.add("dve", lambda e, kc=kc: e.tensor_copy(dst[:, kc, :, :, 1], src[:, kc, :, :, 0]), reads=["wuq", "wuqsw"], writes=["wuqsw"])
    SCALE = float(96 ** -0.5)
    PBK = [6, 7]
    pctr = [0]

    def pbank():
        b = PBK[pctr[0] % 2]
        pctr[0] += 1
        return b

    def emit_proj(h):
        par = h % 2
        for kb in range(16):
            b = pbank()
            mm(K, bank(K, b)[0:64, :], wukv[:, h * 128:h * 128 + 64], ckvT[:, kb * 512:(kb + 1) * 512], True, True,
               ["wukv", f"ckv{kb // 4}"], [f"ps{b}"])
            P.add("dve", lambda e, b=b, kb=kb, par=par: e.tensor_copy(khT[par][0:64, kb * 512:(kb + 1) * 512], bank(K, b)[0:64, :]),
                  reads=[f"ps{b}"], writes=[f"kh{par}_{kb}"])
            yield
        for g in range(8):
            b = pbank()
            for j in range(8):
                kt = g * 8 + j
                mm(K, bank(K, b)[:, j * 64:(j + 1) * 64], ckvT[:, kt * 128:(kt + 1) * 128], wukv[:, h * 128 + 64:h * 128 + 128], True, True,
                   ["wukv", f"ckv{kt // 16}"], [f"ps{b}"])
            pv = bank(K, b).rearrange("p (t c) -> p t c", c=64)
            P.add("dve", lambda e, g=g, par=par, pv=pv: e.tensor_copy(Vh[par][:, g * 8:(g + 1) * 8, 0:64], pv),
                  reads=[f"ps{b}"], writes=[f"Vh{par}_{g}"])
            yield
        for tb in range(4):
            sl = slice(tb * 512, (tb + 1) * 512)
            b1 = pbank()
            b2 = pbank()
            for kc in range(2):
                mm(K, bank(K, b1)[0:96, :], wuq[:, kc, h * 96:(h + 1) * 96], cqT[:, kc, sl], kc == 0, kc == 1, ["wuq", "cqT"], [f"ps{b1}"])
            for kc in range(2):
                mm(K, bank(K, b2)[0:96, :], wuqsw[:, kc, h, :], cqT[:, kc, sl], kc == 0, kc == 1, ["wuqsw", "cqT"], [f"ps{b2}"])
            i = tb % 2
            P.add("dve", lambda e, b1=b1, sl=sl, par=par: e.tensor_copy(qhT[par][0:64, sl], bank(K, b1)[0:64, :]),
                  reads=[f"ps{b1}"], writes=[f"qh{par}"])
            P.add("dve", lambda e, b1=b1, sl=sl, i=i: e.tensor_tensor(rt1[i][64:96, :], bank(K, b1)[64:96, :], ropeC[64:96, sl], ALU.mult),
                  reads=[f"ps{b1}", "ropeC"], writes=[f"rt1_{i}"])
            P.add("dve", lambda e, b2=b2, sl=sl, i=i: e.tensor_tensor(rt2[i][64:96, :], bank(K, b2)[64:96, :], ropeS[64:96, sl], ALU.mult),
                  reads=[f"ps{b2}", "ropeS"], writes=[f"rt2_{i}"])
            P.add("pool", lambda e, sl=sl, i=i, par=par: e.tensor_tensor(qhT[par][64:96, sl], rt1[i][64:96, :], rt2[i][64:96, :], ALU.add),
                  reads=[f"rt1_{i}", f"rt2_{i}"], writes=[f"qh{par}"])
            yield

    sctr = [0]

    def emit_S(h, qb, k2):
        par = h % 2
        si = sctr[0]
        sctr[0] += 1
        b0 = (si % 2) * 2
        for j in range(2):
            kt = k2 * 2 + j
            mm(K, bank(K, b0 + j), khT[par][0:96, kt * 128:(kt + 1) * 128], qhT[par][0:96, qb * 512:(qb + 1) * 512], True, True,
               [f"kh{par}_{kt // 4}", f"khr{par}", f"qh{par}"], [f"ps{b0 + j}"])
        return si

    def emit_exp_pv(h, qb, k2, si):
        par = h % 2
        b0 = (si % 2) * 2
        pt = pT[si % 3]
        ob = 4 + ((h * 4 + qb) % 2)
        P.add("act", lambda e: e.activation(pt, K.ps[:, b0 * 512:(b0 + 2) * 512], AF.Exp, scale=SCALE),
              reads=[f"ps{b0}", f"ps{b0 + 1}"], writes=[f"pT{si % 3}"])
        for j in range(2):
            kt = k2 * 2 + j
            mm(K, bank(K, ob), Vh[par][:, kt, :], pt[:, j * 512:(j + 1) * 512], kt == 0, kt == 63,
               [f"Vh{par}_{kt // 8}", f"Vh1_{par}", f"pT{si % 3}"], [f"ps{ob}"])
        if k2 == 31:
            i = (h * 4 + qb) % 2
            c = h // 2
            sl = slice(qb * 512, (qb + 1) * 512)
            P.add("dve", lambda e: e.reciprocal(rd[i][64:128, :], bank(K, ob)[64:128, :]), reads=[f"ps{ob}"], writes=[f"rd{i}"])
            P.add("dve", lambda e: e.tensor_copy(rd[i][0:64, :], rd[i][64:128, :]), reads=[f"rd{i}"], writes=[f"rd{i}"])
            if par == 0:
                P.add("dve", lambda e: e.tensor_tensor(oT[0:64, c, sl], bank(K, ob)[0:64, :], rd[i][0:64, :], ALU.mult),
                      reads=[f"ps{ob}", f"rd{i}"], writes=[f"oT{c}"])
            else:
                P.add("dve", lambda e: e.tensor_tensor(otmp[i][0:64, :], bank(K, ob)[0:64, :], rd[i][0:64, :], ALU.mult),
                      reads=[f"ps{ob}", f"rd{i}"], writes=[f"otmp{i}"])
                P.add("dve", lambda e: e.tensor_copy(oT[64:128, c, sl], otmp[i][0:64, :]), reads=[f"otmp{i}"], writes=[f"oT{c}"])

    for _ in emit_proj(0):
        pass
    seq = [(h, qb, k2) for h in range(16) for qb in range(4) for k2 in range(32)]
    pend = emit_S(*seq[0])
    projgen = None
    for idx, (h, qb, k2) in enumerate(seq):
        nxt = None
        if idx + 1 < len(seq):
            nxt = emit_S(*seq[idx + 1])
        emit_exp_pv(h, qb, k2, pend)
        pend = nxt
        if qb == 1 and k2 == 0 and h + 1 < 16:
            projgen = emit_proj(h + 1)
        if projgen is not None and qb in (1, 2) and k2 % 2 == 1:
            if next(projgen, "done") == "done":
                projgen = None
        if projgen is not None and qb == 3 and k2 == 0:
            for _ in projgen:
                pass
            projgen = None
    P.barrier()
    A.reset(m1)
    K.x = A.alloc([8, T], F32)
    load_x(K, din["x1"], 0, dres="x1d")
    proj_residual(K, din["mla_w_o"], oT, lambda kc: f"oT{kc}", mod1, "mod1", 16)
    A.reset(mB)
    hT4 = A.alloc([8, T], BF16)
    skip = A.alloc([2, T], BF16)
    skip2 = A.alloc([8, T], BF16)
    assert A.mark() == K.xmark
    K.x = A.alloc([8, T], F32)
    mX = A.mark()
    alloc_norm_scratch(K)
    for tb in range(4):
        modulate_block(K, lambda c, tb=tb: K.x[:, c, tb * 512:(tb + 1) * 512], lambda c, tb=tb: xres(c, tb), 512, mod1, "mod1", 32, 24,
                       lambda c, tb=tb: hT4[:, c, tb * 512:(tb + 1) * 512], lambda c: f"h4_{c}", 6)
    save_off = A.off
    A.off = K.OTmark
    wr = A.alloc([8, 8], BF16)
    lg = A.alloc([16, 8], F32)
    eq = A.alloc([16, 8], F32)
    l2 = A.alloc([16, 8], F32)
    ex = A.alloc([16, 8], F32)
    comb = A.alloc([16, 8], F32)
    mx1 = A.alloc([16], F32)
    mx2 = A.alloc([16], F32)
    ssum = A.alloc([16], F32)
    onesF = A.alloc([128], F32)
    cm = [A.alloc([128], F32) for _ in range(2)]
    cbc = [A.alloc([T], F32) for _ in range(2)]
    assert A.off <= K.xmark
    A.off = save_off
    P.add("dve", lambda e: e.memset(onesF, 1.0), writes=["onesF"])
    P.add("pool", lambda e: e.dma_start(out=wr, in_=din["moe_w_router"].rearrange("(kc p) n -> p kc n", p=128)), writes=["wr"], key="wr")
    for tt in range(16):
        for kc in range(8):
            mm(K, bank(K, 6)[:, tt * 8:(tt + 1) * 8], hT4[:, kc, tt * 128:(tt + 1) * 128], wr[:, kc, :], kc == 0, kc == 7,
               ["wr", f"h4_{kc}"], ["ps6"])
    lgf = lg.rearrange("p a b -> p (a b)")
    P.add("dve", lambda e: e.tensor_copy(lgf, bank(K, 6)[:, 0:128]), reads=["ps6"], writes=["lg"])
    X = mybir.AxisListType.X

    def bc(v):
        return v.unsqueeze(2).to_broadcast([128, 16, 8])

    P.add("dve", lambda e: e.tensor_reduce(mx1, lg, X, ALU.max), reads=["lg"], writes=["mx1"])
    P.add("dve", lambda e: e.tensor_tensor(eq, lg, bc(mx1), ALU.is_equal), reads=["lg", "mx1"], writes=["eq"])
    P.add("dve", lambda e: e.scalar_tensor_tensor(l2, eq, -1e30, lg, ALU.mult, ALU.add), reads=["eq", "lg"], writes=["l2"])
    P.add("dve", lambda e: e.tensor_reduce(mx2, l2, X, ALU.max), reads=["l2"], writes=["mx2"])
    P.add("dve", lambda e: e.tensor_tensor(eq, lg, bc(mx2), ALU.is_ge), reads=["lg", "mx2", "eq"], writes=["eq"])
    P.add("dve", lambda e: e.tensor_tensor(l2, lg, bc(mx1), ALU.subtract), reads=["lg", "mx1", "l2"], writes=["l2"])
    P.add("act", lambda e: e.activation(ex, l2, AF.Exp), reads=["l2"], writes=["ex"])
    P.add("dve", lambda e: e.tensor_tensor(ex, ex, eq, ALU.mult), reads=["ex", "eq"], writes=["ex"])
    P.add("dve", lambda e: e.tensor_reduce(ssum, ex, X, ALU.add), reads=["ex"], writes=["ssum"])
    P.add("dve", lambda e: e.reciprocal(ssum, ssum), reads=["ssum"], writes=["ssum"])
    P.add("dve", lambda e: e.tensor_tensor(comb, ex, bc(ssum), ALU.mult), reads=["ex", "ssum"], writes=["comb"])

    def make_cbc(ex_):
        buf = cbc[ex_ % 2]
        res = f"cbc{ex_ % 2}"
        for tt in range(16):
            cmb = cm[tt % 2]
            P.add("dve", lambda e, cmb=cmb, tt=tt: e.tensor_scalar_mul(cmb, onesF, comb[:, tt, ex_:ex_ + 1]),
                  reads=["comb", "onesF"], writes=[f"cm{tt % 2}"])
            mm(K, bank(K, 7)[:, (tt % 4) * 128:(tt % 4 + 1) * 128], cmb, K.ident, True, True, [f"cm{tt % 2}", "ident"], ["ps7"])
            if tt % 4 == 3:
                t0 = tt - 3
                P.add("act", lambda e, t0=t0: e.copy(buf[:, t0 * 128:(t0 + 4) * 128], bank(K, 7)), reads=["ps7"], writes=[res])

    gu = din["moe_w_gu"]
    dn = din["moe_w_down"]
    groups = []
    for ex_ in range(NEXP):
        guv = gu[ex_].rearrange("(kc p) n -> p kc n", p=128)
        dnv = dn[ex_].rearrange("(f p) n -> p f n", p=128)
        for gj in range(7):
            def load(K, Wg, Wu, Wd, par, guv=guv, dnv=dnv, gj=gj, ex_=ex_):
                if gj == 0:
                    make_cbc(ex_)
                K.P.add("pool", lambda e: e.dma_start(out=Wg[:, :, 0:256], in_=guv[:, :, gj * 256:(gj + 1) * 256]), writes=[f"Wg{par}"], key=f"Wg{par}")
                K.P.add("pool", lambda e: e.dma_start(out=Wu[:, :, 0:256], in_=guv[:, :, EDIM + gj * 256:EDIM + (gj + 1) * 256]), writes=[f"Wu{par}"], key=f"Wu{par}")
                K.P.add("pool", lambda e: e.dma_start(out=Wd[:, 0:2, :], in_=dnv[:, gj * 2:(gj + 1) * 2, :]), writes=[f"Wd{par}"], key=f"Wd{par}")
            groups.append(dict(load=load, nf=2, ex=ex_))

    def comb_fn(g, tb):
        e_ = g["ex"]
        return cbc[e_ % 2][:, tb * 512:(tb + 1) * 512], f"cbc{e_ % 2}"

    swiglu_stream(K, hT4, lambda kc: f"h4_{kc}", groups, mod1, "mod1", 40, comb_fn=comb_fn)
    P.barrier()
    A.reset(mX)
    alloc_norm_scratch(K)
    K.fscale = A.alloc([8], F32)
    P.add("sp", lambda e: e.dma_start(out=K.fscale, in_=din["final_norm"]), writes=["fscale"], key="fscale")
    ost = [A.alloc([8, 512], F32) for _ in range(2)]
    ov = din["out"].rearrange("(c p) t -> p c t", p=128)
    for tb in range(4):
        ob_ = ost[tb % 2]
        modulate_block(K, lambda c, tb=tb: K.x[:, c, tb * 512:(tb + 1) * 512], lambda c, tb=tb: xres(c, tb), 512, None, None, 0, 0,
                       lambda c, ob_=ob_: ob_[:, c, :], lambda c, tb=tb: f"ost{tb % 2}", 6)
        P.add("sp", lambda e, ob_=ob_, tb=tb: e.dma_start(out=ov[:, :, tb * 512:(tb + 1) * 512], in_=ob_), reads=[f"ost{tb % 2}"], key=f"ost{tb % 2}")


FUSED = False


def build_A():
    nc = bass.Bass("TRN2", target_bir_lowering=False)
    K = Ctx()
    K.nc = nc
    K.P = Prog(nc)
    K.A = Arena(nc, ARENA_WORDS)
    K.ps = nc.alloc_psum_tensor("ps", [128, 4096], F32)

    def inp(name, shape):
        return nc.dram_tensor(name, list(shape), F32, kind="ExternalInput").ap()

    def outp(name, shape):
        return nc.dram_tensor(name, list(shape), F32, kind="ExternalOutput").ap()

    K.din = dict(
        xT=inp("xT", (D, TE)), cT=inp("cT", (128, 8)), w_ada=inp("w_ada", (2, D, 6 * D)), b_ada=inp("b_ada", (2, 128, 48)),
        ident=inp("ident", (128, 128)), na_w_qkv=inp("na_w_qkv", (D, 3 * D)), na_bias=inp("na_bias", (16, 128, NA_NBLK * 64)),
        na_w_o=inp("na_w_o", (D, D)), ffn_w_gu=inp("ffn_w_gu", (D, 2 * FFN)), ffn_w_down=inp("ffn_w_down", (FFN, D)),
        mla_w_down=inp("mla_w_down", (D, 416)), kv_norm=inp("kv_norm", (128, 1)),
        ropeC=inp("ropeC", (128, T)), ropeS=inp("ropeS", (128, T)),
        x1=outp("x1", (D, T)), lat=outp("lat", (160, T)),
    )
    phase_A(K)
    K.P.emit()
    print("A stats", K.P.stats, "arena peak words", K.A.peak)
    return nc


def build_B():
    nc = bass.Bass("TRN2", target_bir_lowering=False)
    K = Ctx()
    K.nc = nc
    K.P = Prog(nc)
    K.A = Arena(nc, ARENA_WORDS)
    K.ps = nc.alloc_psum_tensor("ps", [128, 4096], F32)

    def inp(name, shape):
        return nc.dram_tensor(name, list(shape), F32, kind="ExternalInput").ap()

    def outp(name, shape):
        return nc.dram_tensor(name, list(shape), F32, kind="ExternalOutput").ap()

    K.din = dict(
        x1=inp("x1", (D, T)), latall=inp("latall", (NC * 160, T)), cT=inp("cT", (128, 8)), w_ada=inp("w_ada", (2, D, 6 * D)),
        b_ada=inp("b_ada", (2, 128, 48)), ident=inp("ident", (128, 128)), mla_w_down=inp("mla_w_down", (D, 416)),
        ropeC=inp("ropeC", (128, T)), ropeS=inp("ropeS", (128, T)), bsel=inp("bsel", (128, 2)),
        q_norm=inp("q_norm", (128, 2)), mla_w_uq=inp("mla_w_uq", (256, 1536)), mla_w_ukv=inp("mla_w_ukv", (128, 2048)),
        mla_w_o=inp("mla_w_o", (D, D)), moe_w_router=inp("moe_w_router", (D, 8)), moe_w_gu=inp("moe_w_gu", (NEXP, D, 2 * EDIM)),
        moe_w_down=inp("moe_w_down", (NEXP, EDIM, D)), final_norm=inp("final_norm", (128, 8)),
        out=outp("out", (D, T)),
    )
    phase_B_prologue_unfused(K)
    phase_B(K)
    K.P.emit()
    print("B stats", K.P.stats, "arena peak words", K.A.peak)
    return nc


def build_AB():
    nc = bass.Bass("TRN2", target_bir_lowering=False)
    K = Ctx()
    K.nc = nc
    K.P = Prog(nc)
    K.A = Arena(nc, ARENA_WORDS)
    K.ps = nc.alloc_psum_tensor("ps", [128, 4096], F32)

    def inp(name, shape):
        return nc.dram_tensor(name, list(shape), F32, kind="ExternalInput").ap()

    def outp(name, shape):
        return nc.dram_tensor(name, list(shape), F32, kind="ExternalOutput").ap()

    x1d = nc.dram_tensor("x1d", [D, T], F32)
    latd = nc.dram_tensor("latd", [160, T], F32)
    latall = nc.dram_tensor("latall", [NC * 160, T], F32)
    K.din = dict(
        xT=inp("xT", (D, TE)), cT=inp("cT", (128, 8)), w_ada=inp("w_ada", (2, D, 6 * D)), b_ada=inp("b_ada", (2, 128, 48)),
        ident=inp("ident", (128, 128)), na_w_qkv=inp("na_w_qkv", (D, 3 * D)), na_bias=inp("na_bias", (16, 128, NA_NBLK * 64)),
        na_w_o=inp("na_w_o", (D, D)), ffn_w_gu=inp("ffn_w_gu", (D, 2 * FFN)), ffn_w_down=inp("ffn_w_down", (FFN, D)),
        mla_w_down=inp("mla_w_down", (D, 416)), kv_norm=inp("kv_norm", (128, 1)),
        ropeC=inp("ropeC", (128, T)), ropeS=inp("ropeS", (128, T)), bsel=inp("bsel", (128, 2)),
        q_norm=inp("q_norm", (128, 2)), mla_w_uq=inp("mla_w_uq", (256, 1536)), mla_w_ukv=inp("mla_w_ukv", (128, 2048)),
        mla_w_o=inp("mla_w_o", (D, D)), moe_w_router=inp("moe_w_router", (D, 8)), moe_w_gu=inp("moe_w_gu", (NEXP, D, 2 * EDIM)),
        moe_w_down=inp("moe_w_down", (NEXP, EDIM, D)), final_norm=inp("final_norm", (128, 8)),
        x1=x1d.ap(), lat=latd.ap(), latall=latall.ap(),
        out=outp("out", (D, T)),
    )
    phase_A(K)
    K.P.barrier()
    K.P.add("pool", lambda e: e.collective_compute("AllGather", ALU.bypass, replica_groups=[list(range(NC))],
                                                   ins=[latd.ap().opt()], outs=[latall.ap().opt()]),
            reads=["latd0", "latd1"], writes=["latall"], key="cc", inc=1)
    phase_B(K)
    K.P.emit()
    print("AB stats", K.P.stats, "arena peak words", K.A.peak)
    return nc


def host_common(inputs):
    x = np.asarray(inputs["x"], np.float32)
    c = np.asarray(inputs["c"], np.float32)
    per_core = []
    for core in range(NC):
        b, q = core // 4, core % 4
        r0 = 32 * q - 4
        xe = np.zeros((TE, D), np.float32)
        lo, hi = max(r0, 0), min(r0 + 40, 128)
        xe[(lo - r0) * 64:(hi - r0) * 64] = x[b, lo * 64:hi * 64]
        Cf, Sf = host_rope_tables(q)
        per_core.append(dict(
            xT=np.ascontiguousarray(xe.T),
            cT=np.ascontiguousarray(c[b].reshape(8, 128).T),
            ropeC=Cf, ropeS=Sf,
        ))
    shared = dict(
        w_ada=np.asarray(inputs["w_ada"], np.float32),
        b_ada=np.ascontiguousarray(np.asarray(inputs["b_ada"], np.float32).reshape(2, 48, 128).transpose(0, 2, 1)),
        ident=np.eye(128, dtype=np.float32),
    )
    return per_core, shared


_CACHE = {}


def kernel(**inputs):
    per_core, shared = host_common(inputs)
    rpb = np.asarray(inputs["na_rpb"], np.float32)[0]
    bias_q = [host_na_bias(rpb, q) for q in range(4)]
    if FUSED and "AB" not in _CACHE:
        _CACHE["AB"] = build_AB()
    if not FUSED and "A" not in _CACHE:
        _CACHE["A"] = build_A()
        _CACHE["B"] = build_B()
    f32 = lambda k: np.asarray(inputs[k], np.float32)[0]
    common = dict(
        na_w_qkv=f32("na_w_qkv"), na_w_o=f32("na_w_o"), ffn_w_gu=f32("ffn_w_gu"), ffn_w_down=f32("ffn_w_down"),
        mla_w_down=f32("mla_w_down"), kv_norm=f32("mla_kv_norm").reshape(128, 1),
        q_norm=np.ascontiguousarray(f32("mla_q_norm").reshape(2, 128).T),
        mla_w_uq=f32("mla_w_uq"), mla_w_ukv=f32("mla_w_ukv"), mla_w_o=f32("mla_w_o"), moe_w_router=f32("moe_w_router"),
        moe_w_gu=f32("moe_w_gu"), moe_w_down=f32("moe_w_down"),
        final_norm=np.ascontiguousarray(np.asarray(inputs["final_norm"], np.float32).reshape(8, 128).T),
    )
    in_maps = []
    for core in range(NC):
        b = core // 4
        m = dict(per_core[core])
        m.update(shared)
        m.update(common)
        m["na_bias"] = bias_q[core % 4]
        bs = np.zeros((128, 2), np.float32)
        bs[:, b] = 1.0
        m["bsel"] = bs
        in_maps.append(m)
    if FUSED:
        res = run_bass_kernel_spmd(_CACHE["AB"], in_maps, core_ids=list(range(NC))).results
    else:
        keysA = ("xT", "cT", "w_ada", "b_ada", "ident", "na_w_qkv", "na_bias", "na_w_o", "ffn_w_gu", "ffn_w_down",
                 "mla_w_down", "kv_norm", "ropeC", "ropeS")
        resA = run_bass_kernel_spmd(_CACHE["A"], [{k: m[k] for k in keysA} for m in in_maps], core_ids=list(range(NC))).results
        if inputs.get("_debugA") is not None:
            return resA
        latall = np.ascontiguousarray(np.concatenate([resA[c]["lat"] for c in range(NC)], axis=0))
        keysB = ("cT", "w_ada", "b_ada", "ident", "mla_w_down", "ropeC", "ropeS", "bsel", "q_norm", "mla_w_uq", "mla_w_ukv",
                 "mla_w_o", "moe_w_router", "moe_w_gu", "moe_w_down", "final_norm")
        mapsB = []
        for c in range(NC):
            mb = {k: in_maps[c][k] for k in keysB}
            mb["x1"] = resA[c]["x1"]
            mb["latall"] = latall
            mapsB.append(mb)
        res = run_bass_kernel_spmd(_CACHE["B"], mapsB, core_ids=list(range(NC))).results
    out = np.empty((2, S, D), np.float32)
    for core in range(NC):
        b, q = core // 4, core % 4
        out[b, q * T:(q + 1) * T, :] = res[core]["out"].T
    return out
```
